# Optimizing a Trainium2 kernel written in Bass

```python
import jax, jax.numpy as jnp
from jax import lax
import numpy as np

D_MODEL = 1024
BATCH = 4
SEQ = 4096
DEPTH = 1

PLE_DIM = 256
EPS = 1e-6
RET_HEADS = 4
RET_DK = 128
RET_DV = 128
RET_CHUNK = 128
SWA_HEADS = 8
SWA_KV_HEADS = 2
SWA_GROUP = SWA_HEADS // SWA_KV_HEADS
SWA_HD = 64
WINDOW = 128
N_GROUPS = 4
EXPERTS_PER_GROUP = 8
N_EXPERTS = N_GROUPS * EXPERTS_PER_GROUP
EXPERT_FF = 256
TOP_K = 2

RET_QK = RET_HEADS * RET_DK
RET_V = RET_HEADS * RET_DV
SWA_Q = SWA_HEADS * SWA_HD
SWA_KV = SWA_KV_HEADS * SWA_HD
IN_SPLITS = (RET_QK, RET_QK, RET_V, RET_V, SWA_Q, SWA_KV, SWA_KV, D_MODEL, D_MODEL)
IN_WIDTH = sum(IN_SPLITS)

kernel_name = "hybrid_retention_swa_hmoe_block"


def rms_norm(x, g):
    xf = x.astype(jnp.float32)
    y = xf * lax.rsqrt(jnp.mean(xf * xf, axis=-1, keepdims=True) + EPS)
    return (y * g.astype(jnp.float32)).astype(x.dtype)


def head_group_norm(y, g, b):
    yf = y.astype(jnp.float32)
    mu = jnp.mean(yf, axis=-1, keepdims=True)
    var = jnp.mean(jnp.square(yf - mu), axis=-1, keepdims=True)
    yn = ((yf - mu) * lax.rsqrt(var + EPS)).reshape(y.shape[0], y.shape[1], -1)
    return (yn * g.astype(jnp.float32) + b.astype(jnp.float32)).astype(y.dtype)


def retention_chunkwise(q, k, v):
    dtype = v.dtype
    q = q.astype(jnp.float32)
    k = k.astype(jnp.float32) * (RET_DK ** -0.5)
    v = v.astype(jnp.float32)
    b, s, h, dk = q.shape
    nc = s // RET_CHUNK
    q = q.reshape(b, nc, RET_CHUNK, h, dk)
    k = k.reshape(b, nc, RET_CHUNK, h, dk)
    v = v.reshape(b, nc, RET_CHUNK, h, RET_DV)
    log_gamma = jnp.log1p(-jnp.exp2(-5.0 - jnp.arange(h, dtype=jnp.float32)))
    pos = jnp.arange(RET_CHUNK, dtype=jnp.float32)
    diff = pos[:, None] - pos[None, :]
    decay_mask = jnp.where(diff[None] >= 0.0,
                           jnp.exp(jnp.maximum(diff, 0.0)[None] * log_gamma[:, None, None]), 0.0)
    scores = jnp.einsum('bnihd,bnjhd->bnhij', q, k) * decay_mask
    inner = jnp.einsum('bnhij,bnjhv->bnihv', scores, v)
    zeta = jnp.exp((RET_CHUNK - 1.0 - pos)[None, :] * log_gamma[:, None])
    chunk_kv = jnp.einsum('bnjhd,bnjhv,hj->nbhdv', k, v, zeta)
    chunk_decay = jnp.exp(RET_CHUNK * log_gamma)[None, :, None, None]

    def step(state, kv):
        return chunk_decay * state + kv, state

    _, prev_states = lax.scan(step, jnp.zeros_like(chunk_kv[0]), chunk_kv)
    xi = jnp.exp((pos + 1.0)[None, :] * log_gamma[:, None])
    cross = jnp.einsum('bnihd,nbhdv,hi->bnihv', q, prev_states, xi)
    return (inner + cross).reshape(b, s, h, RET_DV).astype(dtype)


def sliding_window_gqa(q, k, v, sinks):
    b, s = q.shape[:2]
    nb = s // WINDOW
    qb = q.reshape(b, nb, WINDOW, SWA_KV_HEADS, SWA_GROUP, SWA_HD)
    kb = k.reshape(b, nb, WINDOW, SWA_KV_HEADS, SWA_HD)
    vb = v.reshape(b, nb, WINDOW, SWA_KV_HEADS, SWA_HD)
    k_band = jnp.concatenate([jnp.concatenate([jnp.zeros_like(kb[:, :1]), kb[:, :-1]], axis=1), kb], axis=2)
    v_band = jnp.concatenate([jnp.concatenate([jnp.zeros_like(vb[:, :1]), vb[:, :-1]], axis=1), vb], axis=2)
    scores = jnp.einsum('bnqkgd,bnskd->bnkgqs', qb, k_band).astype(jnp.float32) * (SWA_HD ** -0.5)
    qi = jnp.arange(WINDOW)
    sj = jnp.arange(2 * WINDOW)
    rel = qi[:, None] + WINDOW - sj[None, :]
    in_window = (rel >= 0) & (rel < WINDOW)
    key_exists = (jnp.arange(nb)[:, None] * WINDOW - WINDOW + sj[None, :]) >= 0
    mask = in_window[None] & key_exists[:, None, :]
    slopes = jnp.exp2(-8.0 * jnp.arange(1, SWA_HEADS + 1, dtype=jnp.float32) / SWA_HEADS)
    slopes = slopes.reshape(SWA_KV_HEADS, SWA_GROUP)
    alibi = -slopes[:, :, None, None] * rel.astype(jnp.float32)[None, None]
    scores = jnp.where(mask[None, :, None, None], scores + alibi[None, None], -1e30)
    sink = jnp.broadcast_to(sinks.astype(jnp.float32).reshape(1, 1, SWA_KV_HEADS, SWA_GROUP, 1, 1),
                            scores.shape[:-1] + (1,))
    probs = jax.nn.softmax(jnp.concatenate([scores, sink], axis=-1), axis=-1)[..., :-1]
    out = jnp.einsum('bnkgqs,bnskd->bnqkgd', probs.astype(v.dtype), v_band)
    return out.reshape(b, s, SWA_Q)


def hierarchical_moe(h, w_rg, b_rg, w_re, b_re, w_gate, w_up, w_down):
    b, s, d = h.shape
    hf = h.reshape(-1, d)
    g_prob = jax.nn.softmax((hf @ w_rg + b_rg).astype(jnp.float32), axis=-1)
    g_sel = jnp.argmax(g_prob, axis=-1)
    g_w = jnp.take_along_axis(g_prob, g_sel[:, None], axis=-1)
    e_logits = (hf @ w_re + b_re).astype(jnp.float32).reshape(-1, N_GROUPS, EXPERTS_PER_GROUP)
    e_in_group = jnp.take_along_axis(e_logits, g_sel[:, None, None], axis=1)[:, 0]
    top_p, top_i = lax.top_k(jax.nn.softmax(e_in_group, axis=-1), TOP_K)
    top_p = top_p / jnp.sum(top_p, axis=-1, keepdims=True)
    expert_id = g_sel[:, None] * EXPERTS_PER_GROUP + top_i
    combine = jnp.sum(jax.nn.one_hot(expert_id, N_EXPERTS, dtype=jnp.float32)
                      * (g_w * top_p)[..., None], axis=1)
    y = jnp.zeros_like(hf)
    for grp in range(N_GROUPS):
        sl = slice(grp * EXPERTS_PER_GROUP, (grp + 1) * EXPERTS_PER_GROUP)
        hid = jax.nn.silu(jnp.einsum('td,edf->tef', hf, w_gate[sl])) * jnp.einsum('td,edf->tef', hf, w_up[sl])
        y = y + jnp.einsum('tef,efd->td', hid * combine[:, sl, None].astype(hid.dtype), w_down[sl])
    return y.reshape(b, s, d)


def setup_inputs(seed: int = 0) -> dict:
    key = jax.random.key(seed)
    ks = jax.random.split(key, 26)
    f32 = jnp.float32
    L = DEPTH

    def nrm(k, shape, scale):
        return jax.random.normal(k, shape, f32) * scale

    def gain(k, shape):
        return 1.0 + 0.02 * jax.random.normal(k, shape, f32)

    return dict(
        x=nrm(ks[0], (BATCH, SEQ, D_MODEL), 1.0),
        p=nrm(ks[1], (L, BATCH, SEQ, PLE_DIM), 1.0),
        mix_norm_g=gain(ks[2], (L, D_MODEL)),
        w_in=nrm(ks[3], (L, D_MODEL, IN_WIDTH), D_MODEL ** -0.5),
        ret_gn_g=gain(ks[4], (L, RET_V)),
        ret_gn_b=nrm(ks[5], (L, RET_V), 0.02),
        w_ret_o=nrm(ks[6], (L, RET_V, D_MODEL), RET_V ** -0.5),
        q_norm_g=gain(ks[7], (L, SWA_HD)),
        k_norm_g=gain(ks[8], (L, SWA_HD)),
        attn_sinks=nrm(ks[9], (L, SWA_HEADS), 0.5),
        w_swa_o=nrm(ks[10], (L, SWA_Q, D_MODEL), SWA_Q ** -0.5),
        w_out=nrm(ks[11], (L, D_MODEL, D_MODEL), D_MODEL ** -0.5),
        ffn_norm_g=gain(ks[12], (L, D_MODEL)),
        w_router_group=nrm(ks[13], (L, D_MODEL, N_GROUPS), D_MODEL ** -0.5),
        b_router_group=nrm(ks[14], (L, N_GROUPS), 0.01),
        w_router_expert=nrm(ks[15], (L, D_MODEL, N_EXPERTS), D_MODEL ** -0.5),
        b_router_expert=nrm(ks[16], (L, N_EXPERTS), 0.01),
        w_exp_gate=nrm(ks[17], (L, N_EXPERTS, D_MODEL, EXPERT_FF), D_MODEL ** -0.5),
        w_exp_up=nrm(ks[18], (L, N_EXPERTS, D_MODEL, EXPERT_FF), D_MODEL ** -0.5),
        w_exp_down=nrm(ks[19], (L, N_EXPERTS, EXPERT_FF, D_MODEL), EXPERT_FF ** -0.5),
        ple_gate_norm_g=gain(ks[20], (L, D_MODEL)),
        w_ple_gate=nrm(ks[21], (L, D_MODEL, D_MODEL), D_MODEL ** -0.5),
        w_ple=nrm(ks[22], (L, PLE_DIM, D_MODEL), PLE_DIM ** -0.5),
        ple_norm_g=gain(ks[23], (L, D_MODEL)),
    )


def reference(x, p, mix_norm_g, w_in, ret_gn_g, ret_gn_b, w_ret_o, q_norm_g, k_norm_g, attn_sinks,
              w_swa_o, w_out, ffn_norm_g, w_router_group, b_router_group, w_router_expert,
              b_router_expert, w_exp_gate, w_exp_up, w_exp_down, ple_gate_norm_g, w_ple_gate,
              w_ple, ple_norm_g):
    b, s, _ = x.shape
    split_at = [int(c) for c in np.cumsum(IN_SPLITS)[:-1]]
    for i in range(DEPTH):
        h = rms_norm(x, mix_norm_g[i])
        z = h @ w_in[i]
        rq, rk, rv, rg, sq, sk, sv, gate_r, gate_s = jnp.split(z, split_at, axis=-1)
        y_r = retention_chunkwise(rq.reshape(b, s, RET_HEADS, RET_DK),
                                  rk.reshape(b, s, RET_HEADS, RET_DK),
                                  rv.reshape(b, s, RET_HEADS, RET_DV))
        y_r = (jax.nn.silu(rg) * head_group_norm(y_r, ret_gn_g[i], ret_gn_b[i])) @ w_ret_o[i]
        q = rms_norm(sq.reshape(b, s, SWA_HEADS, SWA_HD), q_norm_g[i])
        k = rms_norm(sk.reshape(b, s, SWA_KV_HEADS, SWA_HD), k_norm_g[i])
        v = sv.reshape(b, s, SWA_KV_HEADS, SWA_HD)
        y_s = sliding_window_gqa(q, k, v, attn_sinks[i]) @ w_swa_o[i]
        merged = jax.nn.sigmoid(gate_r) * y_r + jax.nn.sigmoid(gate_s) * y_s
        x = x + merged @ w_out[i]
        x = x + hierarchical_moe(rms_norm(x, ffn_norm_g[i]), w_router_group[i], b_router_group[i],
                                 w_router_expert[i], b_router_expert[i], w_exp_gate[i],
                                 w_exp_up[i], w_exp_down[i])
        ple = rms_norm(p[i] @ w_ple[i], ple_norm_g[i])
        x = x + jax.nn.sigmoid(rms_norm(x, ple_gate_norm_g[i]) @ w_ple_gate[i]) * ple
    return x
```

```python
import os
import numpy as np
import ml_dtypes
import concourse.bass as bass
import concourse.mybir as mybir
from concourse.bass_utils import run_bass_kernel_spmd

F32 = mybir.dt.float32
BF16 = mybir.dt.bfloat16
AF = mybir.ActivationFunctionType
ALU = mybir.AluOpType
AX = mybir.AxisListType

D = 1024
NCORES = 8
TOK = 2048
NT = 16
EPS = 1e-6
IN_W = 4864
C_RQ, C_RK, C_RV, C_RG, C_SQ, C_SK, C_SV, C_GR, C_GS = 0, 512, 1024, 1536, 2048, 2560, 2688, 2816, 3840
NEG = -30000.0


class Buf:
    __slots__ = ("name", "w", "r", "rd")

    def __init__(self, name):
        self.name = name
        self.w = None
        self.r = {}
        self.rd = []


class Op:
    __slots__ = ("eng", "fn", "deps", "sig", "cnt", "dma", "dsem", "dval", "gidx")


class Sched:
    ENGS = ("pe", "act", "dve", "pool", "sp")

    def __init__(self, nc, n_dma_sems=8):
        self.nc = nc
        self.q = {e: [] for e in self.ENGS}
        self.n = 0
        self.n_dma_sems = n_dma_sems
        self.final_waits = []

    def add(self, eng, fn, reads=(), writes=(), dma=False, force=False):
        import os
        lim = int(os.environ.get("PROG_MAXOPS", "0"))
        if lim and self.n >= lim and not force:
            self.n += 1
            return None
        if os.environ.get("PROG_TRACE"):
            import sys as _s
            fr = _s._getframe(2)
            print("OP", self.n, eng, fr.f_code.co_name, fr.f_lineno, _s._getframe(3).f_code.co_name, _s._getframe(3).f_lineno)
        op = Op()
        op.eng = eng
        op.fn = fn
        op.dma = dma
        op.sig = dma
        op.cnt = 0
        op.gidx = self.n
        self.n += 1
        deps = {}

        def need(p):
            if p is None:
                return
            if (not p.dma) and p.eng == eng and eng == "pe":
                return
            if p.dma:
                deps[("d", p.gidx)] = p
            else:
                k = ("c", p.eng)
                if k not in deps or deps[k].gidx < p.gidx:
                    deps[k] = p

        for b in reads:
            need(b.w)
        for b in writes:
            need(b.w)
            for p in b.r.values():
                need(p)
            for p in b.rd:
                need(p)
        op.deps = list(deps.values())
        for p in op.deps:
            p.sig = True
        for b in reads:
            if dma:
                b.rd.append(op)
            else:
                b.r[eng] = op
        for b in writes:
            b.w = op
            b.r = {}
            b.rd = []
        self.q[eng].append(op)
        return op

    def barrier(self):
        lasts = []
        for e in self.ENGS:
            ops = [o for o in self.q[e] if not o.dma and o.fn is not None]
            if ops:
                lasts.append(ops[-1])
            dops = [o for o in self.q[e] if o.dma]
            lasts.extend(dops[-self.n_dma_sems:])
        for e in self.ENGS:
            op = Op()
            op.eng, op.fn, op.dma, op.sig, op.cnt, op.gidx = e, None, False, False, 0, self.n
            self.n += 1
            op.deps = [p for p in lasts if p.dma or p.eng != e]
            for p in op.deps:
                p.sig = True
            self.q[e].append(op)

    def emit(self):
        nc = self.nc
        from contextlib import ExitStack
        with ExitStack() as es:
            sems = {e: es.enter_context(nc.semaphore("s_" + e)) for e in self.ENGS}
            dsems = {}
            for e in self.ENGS:
                if any(o.dma for o in self.q[e]):
                    dsems[e] = [es.enter_context(nc.semaphore("d_%s%d" % (e, i))) for i in range(self.n_dma_sems)]
            for e in self.ENGS:
                c = 0
                k = 0
                for o in self.q[e]:
                    if o.dma:
                        o.dsem = dsems[e][k % self.n_dma_sems]
                        o.dval = 16 * (k // self.n_dma_sems + 1)
                        k += 1
                    elif o.sig:
                        c += 1
                        o.cnt = c
            block = es.enter_context(nc.Block())
            handles = {"pe": block.tensor, "act": block.scalar, "dve": block.vector, "pool": block.gpsimd, "sp": block.sync}

            def make(e):
                def body(eng):
                    known = {}

                    def wait(sem, val):
                        if known.get(sem.num, 0) >= val:
                            return
                        eng.wait_ge(sem, val)
                        known[sem.num] = val

                    for o in self.q[e]:
                        for p in o.deps:
                            if p.dma:
                                wait(p.dsem, p.dval)
                            else:
                                wait(sems[p.eng], p.cnt)
                        if o.dma and o.dval > 16:
                            wait(o.dsem, o.dval - 16)
                        if o.fn is None:
                            continue
                        ins = o.fn(eng)
                        if o.dma:
                            ins.then_inc(o.dsem, 16)
                        elif o.sig:
                            ins.then_inc(sems[e], 1)
                    if e == "sp":
                        for p in [q_ for q_ in self.final_waits if q_ is not None]:
                            wait(p.dsem, p.dval)
                return body

            for e in self.ENGS:
                if self.q[e] or e == "sp":
                    handles[e](make(e))


def _const_tables():
    t = {}
    t["ident"] = np.eye(128, dtype=np.float32).astype(ml_dtypes.bfloat16)
    h = np.arange(4, dtype=np.float64)
    gam = 1.0 - np.exp2(-5.0 - h)
    lg = np.log(gam)
    pos = np.arange(128, dtype=np.float64)
    qd = np.exp((pos[None, :] - 127.0) * lg[:, None])
    kd = np.exp((127.0 - pos[None, :]) * lg[:, None]) * (128.0 ** -0.5)
    t["qdec"] = np.broadcast_to(qd[None], (128, 4, 128)).astype(np.float32).copy()
    t["kdec"] = np.broadcast_to(kd[None], (128, 4, 128)).astype(np.float32).copy()
    a = np.exp(128.0 * lg)
    t["adec"] = np.broadcast_to(np.repeat(a, 128)[None], (128, 512)).astype(np.float32).copy()
    j = np.arange(128)[:, None]
    i = np.arange(128)[None, :]
    cm = (i >= j).astype(np.float32)
    t["cmask"] = np.broadcast_to(cm[:, None, :], (128, 4, 128)).astype(np.float32).copy()
    slopes = np.exp2(-(np.arange(8, dtype=np.float64) + 1.0))
    s = np.arange(128, dtype=np.float64)[:, None]
    q = np.arange(128, dtype=np.float64)[None, :]
    relp = q + 128.0 - s
    relc = q - s
    bp = np.where((relp >= 0) & (relp < 128), 0.0, 1.0)
    bc = np.where((relc >= 0) & (relc < 128), 0.0, 1.0)
    sbp = np.empty((128, 8, 128), np.float32)
    sbc = np.empty((128, 8, 128), np.float32)
    for hh in range(8):
        sbp[:, hh, :] = np.where(bp > 0, NEG, -slopes[hh] * relp)
        sbc[:, hh, :] = np.where(bc > 0, NEG, -slopes[hh] * relc)
    def hl(a):
        hi = a.astype(ml_dtypes.bfloat16)
        lo = (a - hi.astype(np.float32)).astype(ml_dtypes.bfloat16)
        return np.ascontiguousarray(np.stack([hi, lo], axis=1))
    t["swab_prev"] = hl(sbp)
    t["swab_cur"] = hl(sbc)
    t["swab_neg"] = hl(np.full((128, 8, 128), NEG, np.float32))
    bo = np.zeros((128, 128), np.float32)
    bo[:64, :64] = 1.0 / 64
    bo[64:, 64:] = 1.0 / 64
    t["blockones"] = bo.astype(ml_dtypes.bfloat16)
    sel = np.zeros((64, 32, 128), np.float32)
    for e in range(32):
        sel[e, e, :] = 1.0
        sel[32 + e, e, :] = 1.0
    t["sel"] = sel.astype(ml_dtypes.bfloat16)
    return t


_CT = None


def consts():
    global _CT
    if _CT is None:
        _CT = _const_tables()
    return _CT


class T:
    def __init__(self, h, name):
        self.h = h
        self.b = Buf(name)

    def __getitem__(self, k):
        return self.h[k]


class Builder:
    def __init__(self):
        self.nc = bass.Bass("TRN2", target_bir_lowering=False)
        self.S = Sched(self.nc)
        self.dbg = []

    def din(self, name, shape, dt=F32):
        return T(self.nc.dram_tensor(name, list(shape), dt, kind="ExternalInput").ap(), name)

    def dout(self, name, shape, dt=F32):
        return T(self.nc.dram_tensor(name, list(shape), dt, kind="ExternalOutput").ap(), name)

    def dscratch(self, name, shape, dt=F32):
        return T(self.nc.dram_tensor(name, list(shape), dt).ap(), name)

    def sb(self, name, shape, dt):
        esz = 2 if dt == BF16 else 4
        n = 1
        for v in shape[1:]:
            n *= v
        nbytes = (n * esz + 31) // 32 * 32
        if not hasattr(self, "sp"):
            self.sp = (self.nc.sbuf_base + 63) // 64 * 64
            self.uid = 0
        off = self.sp
        self.sp += nbytes
        assert self.sp <= self.nc.sbuf_top, "SBUF overflow at %s: %d > %d" % (name, self.sp, self.nc.sbuf_top)
        self.hw = max(getattr(self, "hw", 0), self.sp)
        self.uid += 1
        return T(self.nc.alloc_sbuf_tensor_at("%s_%d" % (name, self.uid), list(shape), dt, offset=off), name)

    def mark(self):
        return self.sp

    def release(self, mk):
        if os.environ.get("PROG_SBUF"):
            print("SBUF high-water before release: %d of %d" % (self.hw, self.nc.sbuf_top))
        self.hw = 0
        self.sp = mk

    def ps(self, name):
        t = T(self.nc.alloc_psum_tensor(name, [128, 512], F32), name)
        t.psum = True
        return t

    def _bufs(self, ts):
        return [t.b for t in ts]

    def _rw(self, rd, wr):
        wr = list(wr) + [t for t in rd if getattr(t, "psum", False)]
        return [t.b for t in rd], [t.b for t in wr]

    def dma(self, out, in_, rd, wr, q="sp"):
        return self.S.add(q, lambda e: e.dma_start(out=out, in_=in_), *self._rw(rd, wr), dma=True)

    def mm(self, out, lhsT, rhs, start, stop, rd, wr):
        return self.S.add("pe", lambda e: e.matmul(out, lhsT, rhs, start=start, stop=stop), *self._rw(rd, wr))

    def tr(self, out, in_, ident, rd, wr):
        return self.S.add("pe", lambda e: e.transpose(out, in_, ident), *self._rw(rd, wr))

    def act(self, out, in_, func, rd, wr, bias=None, scale=None, accum_out=None):
        kw = {}
        if bias is not None:
            kw["bias"] = bias
        if scale is not None:
            kw["scale"] = scale
        if accum_out is not None:
            kw["accum_out"] = accum_out
        return self.S.add("act", lambda e: e.activation(out, in_, func, **kw), *self._rw(rd, wr))

    def tt(self, eng, out, in0, in1, op, rd, wr):
        return self.S.add(eng, lambda e: e.tensor_tensor(out, in0, in1, op), *self._rw(rd, wr))

    def ts(self, eng, out, in0, s1, s2, op0, op1, rd, wr):
        if op1 is None:
            return self.S.add(eng, lambda e: e.tensor_scalar(out, in0, s1, None, op0), *self._rw(rd, wr))
        return self.S.add(eng, lambda e: e.tensor_scalar(out, in0, s1, s2, op0, op1), *self._rw(rd, wr))

    def stt(self, eng, out, in0, scalar, in1, op0, op1, rd, wr):
        return self.S.add(eng, lambda e: e.scalar_tensor_tensor(out, in0, scalar, in1, op0, op1), *self._rw(rd, wr))

    def cp(self, eng, out, in_, rd, wr):
        if eng == "act":
            return self.S.add("act", lambda e: e.copy(out, in_), *self._rw(rd, wr))
        return self.S.add(eng, lambda e: e.tensor_copy(out, in_), *self._rw(rd, wr))

    def memset(self, eng, ap, val, wr):
        return self.S.add(eng, lambda e: e.memset(ap, val), [], self._bufs(wr))

    def dump(self, name, t, ap, shape, dt):
        o = self.dout("dbg_" + name, shape, dt)
        op = self.S.add("sp", lambda e: e.dma_start(out=o[:], in_=ap), [t.b], [o.b], dma=True, force=True)
        self.S.final_waits.append(op)
        self.dbg.append("dbg_" + name)


def interleave(*gens):
    gens = [g for g in gens if g is not None]
    while gens:
        for g in list(gens):
            try:
                next(g)
            except StopIteration:
                gens.remove(g)


class Prog:
    def __init__(self, n_prev=16, n_own=16, do_moe=True, do_ple=True, n_groups=16, dbg=None):
        self.B = B = Builder()
        self.n_prev, self.n_own = n_prev, n_own
        self.dbg = dbg or set()
        d = self.d = {}
        for nm, shp, dt in [
            ("x", [TOK, D], F32), ("xprev", [TOK, D], F32), ("p", [TOK, 256], F32),
            ("w_in", [D, IN_W], F32), ("w_ret_o", [512, D], F32), ("w_swa_o", [512, D], F32),
            ("w_out", [D, D], F32), ("w_rt", [D, 36], F32),
            ("w_gate", [32, D, 256], F32), ("w_up", [32, D, 256], F32), ("w_down", [32, 256, D], F32),
            ("w_pg", [D, D], F32), ("w_ple", [256, D], F32),
            ("gmix", [128, 8], F32), ("gffn", [128, 8], F32), ("gpg", [128, 8], F32),
            ("gng", [128, 4], F32), ("gnb", [128, 4], F32), ("gqd", [128, 1], F32), ("gkd", [128, 1], F32),
            ("sinks", [1, 8], F32), ("brt", [1, 36], F32), ("gple", [1, D], F32),
            ("gffn_row", [1, D], F32), ("gpg_row", [1, D], F32),
            ("ident", [128, 128], BF16), ("qdec", [128, 4, 128], F32), ("kdec", [128, 4, 128], F32),
            ("adec", [128, 512], F32), ("cmask", [128, 4, 128], F32),
            ("swab_prev", [128, 2, 8, 128], BF16), ("swab_cur", [128, 2, 8, 128], BF16), ("swab_first", [128, 2, 8, 128], BF16),
            ("blockones", [128, 128], BF16), ("sel", [64, 32, 128], BF16),
        ]:
            d[nm] = B.din(nm, shp, dt)
        self.out = B.dout("out", [TOK, D], F32)
        self.x1s = B.dscratch("x1s", [TOK, D], F32)
        self.psb = [B.ps("bank%d" % i) for i in range(8)]
        self.c = {}
        self.ncast = 0
        self.cast_engs = ("act", "dve")
        self.zbank = 0
        self.lc(["ident", "gmix", "gffn", "gpg"])
        self.mhalf = B.sb("mhalf", [128, 256], F32)
        B.memset("pool", self.mhalf[:], -0.5, [self.mhalf])
        self.h2T = B.sb("h2T", [128, 8, TOK], BF16)
        self.lg_all = B.sb("lg_all", [128, 16, 36], F32)
        mk0 = B.mark()
        self.grT = B.sb("grT", [128, 16, 8, 128], BF16)
        mk = B.mark()
        self.phaseA1()
        if "dumpA1" in self.dbg:
            m = self.m
            for nm, t, shp, dt in [("qT", m["qT"][1], [128, 4, 128], BF16), ("kT", m["kT"][1], [128, 4, 128], BF16),
                                   ("vtok", m["vtok"][1], [128, 512], BF16), ("scT", m["scT"], [128, 4, 128], BF16),
                                   ("yn", m["yn"], [128, 512], BF16), ("YrT", m["YrT"][1], [128, 4, 128], BF16),
                                   ("sgT", m["sgT"][1], [128, 4, 128], BF16), ("sigr", m["sigr"][1], [128, 8, 128], BF16),
                                   ("S", m["S"], [128, 512], F32), ("hT", m["hT"][1], [128, 1024], BF16),
                                   ("mean", m["mean"], [128, 4], F32), ("rstd", m["rstd"], [128, 4], F32),
                                   ("ktok", m["ktok"], [128, 512], BF16), ("t1", m["t1"], [128, 4, 128], BF16)]:
                B.dump(nm, t, t[:], shp, dt)
        if "stopA1" in self.dbg:
            B.dump("grT", self.grT, self.grT[:, 0:n_own], [128, n_own, 8, 128], BF16)
            B.S.emit()
            return
        B.S.barrier()
        B.release(mk)
        self.phaseA2()
        B.S.barrier()
        B.release(mk0)
        if do_moe:
            self.moe(n_groups)
        if do_ple:
            self.ple()
        B.S.emit()

    SHAPES = {"ident": ([128, 128], BF16), "qdec": ([128, 4, 128], F32), "kdec": ([128, 4, 128], F32),
              "adec": ([128, 512], F32), "cmask": ([128, 4, 128], F32), "swab_prev": ([128, 2, 8, 128], BF16),
              "swab_cur": ([128, 2, 8, 128], BF16), "swab_first": ([128, 2, 8, 128], BF16), "blockones": ([128, 128], BF16),
              "gmix": ([128, 8], F32), "gffn": ([128, 8], F32), "gpg": ([128, 8], F32), "gng": ([128, 4], F32),
              "gnb": ([128, 4], F32), "gqd": ([128, 1], F32), "gkd": ([128, 1], F32), "sel": ([64, 32, 128], BF16)}

    def lc(self, names):
        B, d, c = self.B, self.d, self.c
        for nm in names:
            shp, dt = self.SHAPES[nm]
            c[nm] = B.sb("c_" + nm, shp, dt)
            B.dma(c[nm][:], d[nm][:], [d[nm]], [c[nm]])

    def cast(self, out, in_, rd, wr, scale=None):
        B = self.B
        eng = self.cast_engs[self.ncast % len(self.cast_engs)]
        self.ncast += 1
        if scale is None:
            B.cp(eng, out, in_, rd, wr)
        elif eng == "act":
            B.act(out, in_, AF.Copy, rd, wr, scale=scale)
        else:
            B.ts(eng, out, in_, scale, None, ALU.mult, None, rd, wr)

    def rsqrt(self, dst, src, n, scale, rd_t, wr_t):
        B = self.B
        B.ts("pool", dst, src, scale, EPS, ALU.mult, ALU.add, rd_t, wr_t)
        for c0 in range(0, n, 256):
            c1 = min(n, c0 + 256)
            B.tt("pool", dst[:, c0:c1], dst[:, c0:c1], self.mhalf[:, 0:c1 - c0], ALU.pow, wr_t + [self.mhalf], wr_t)

    def new_stage(self, n=6, cols=1024):
        self.stage = [self.B.sb("stage%d" % i, [128, cols], F32) for i in range(n)]
        self.stage_cols = cols
        self.nstage = 0

    def next_stage(self):
        st = self.stage[self.nstage % len(self.stage)]
        self.nstage += 1
        return st

    def load_rows(self, dst, src_name, nk, scale_name=None, const_scale=None):
        B, d, c = self.B, self.d, self.c
        v = d[src_name][:].rearrange("(c p) n -> p c n", p=128)
        n = v.shape[2]
        per = max(1, self.stage_cols // n)
        for kc in range(0, nk, per):
            st = self.next_stage()
            k1 = min(nk, kc + per)
            stv = st[:, 0:(k1 - kc) * n].rearrange("p (c n) -> p c n", c=k1 - kc)
            B.dma(stv, v[:, kc:k1, :], [d[src_name]], [st], q=("sp", "pool")[self.nstage % 2])
            if scale_name is None and const_scale is not None:
                self.B.ts("dve", dst[:, kc:k1, :], stv, const_scale, None, ALU.mult, None, [st], [dst])
            elif scale_name is None:
                self.cast(dst[:, kc:k1, :], stv, [st], [dst])
            else:
                for k in range(kc, k1):
                    self.cast(dst[:, k, :], stv[:, k - kc, :], [st, c[scale_name]], [dst], scale=c[scale_name][:, k:k + 1])

    def load_win(self, dst, pieces):
        B, d, c = self.B, self.d, self.c
        win_v = d["w_in"][:].rearrange("(c p) n -> p c n", p=128)
        SC = self.stage_cols
        for kc in range(8):
            sc = c["gmix"][:, kc:kc + 1]
            for (c0, c1, dsts) in pieces:
                for p0 in range(c0, c1, SC):
                    p1 = min(c1, p0 + SC)
                    st = self.next_stage()
                    B.dma(st[:, 0:p1 - p0], win_v[:, kc, p0:p1], [d["w_in"]], [st], q=("sp", "pool")[self.nstage % 2])
                    for (dc0, r0, r1) in dsts:
                        a0, a1 = max(r0, p0 - c0), min(r1, p1 - c0)
                        if a0 >= a1:
                            continue
                        self.cast(dst[:, kc, dc0 + (a0 - r0):dc0 + (a1 - r0)], st[:, a0 - (p0 - c0):a1 - (p0 - c0)], [st, c["gmix"]], [dst], scale=sc)

    def common_bufs(self, nx=2):
        B, m = self.B, self.m
        sb = B.sb
        m["x"] = [sb("xr%d" % i, [128, D], F32) for i in range(nx)]
        m["junk"] = sb("junk", [128, D], BF16)
        m["ss"] = [sb("ss%d" % i, [128, 1], F32) for i in range(2)]
        m["rs"] = [sb("rs%d" % i, [128, 1], F32) for i in range(2)]
        m["hb"] = sb("hb", [128, D], BF16)
        m["hT"] = [sb("hT%d" % i, [128, D], BF16) for i in range(2)]

    def phaseA1(self):
        B, d, c = self.B, self.d, self.c
        sb = B.sb
        self.lc(["qdec", "kdec", "adec", "cmask", "gng", "gnb"])
        W = self.W = {}
        W["in"] = sb("w1_bf", [128, 8, 3072], BF16)
        W["ret_o"] = sb("w_ret_o_bf", [128, 4, D], BF16)
        mk_st = B.mark()
        self.new_stage()
        self.load_win(W["in"], [(0, 2048, [(0, 0, 2048)]), (C_GR, C_GR + 1024, [(2048, 0, 1024)])])
        self.load_rows(W["ret_o"], "w_ret_o", 4, const_scale=0.25)
        B.S.barrier()
        B.release(mk_st)
        m = self.m = {}
        self.common_bufs()
        R2 = lambda nm, shp, dt, n=2: [sb("%s%d" % (nm, i), shp, dt) for i in range(n)]
        m["qT"] = R2("qT", [128, 4, 128], BF16)
        m["kT"] = R2("kT", [128, 4, 128], BF16)
        m["sgT"] = R2("sgT", [128, 4, 128], BF16, 3)
        m["th"] = sb("th", [128, 4, 128], BF16)
        m["sigr"] = R2("sigr", [128, 8, 128], BF16, 4)
        m["vtok"] = R2("vtok", [128, 512], BF16)
        m["ktok"] = sb("ktok", [128, 512], BF16)
        m["S"] = sb("S", [128, 512], F32)
        m["Shat"] = sb("Shat", [128, 512], BF16)
        m["scT"] = sb("scT", [128, 4, 128], BF16)
        m["ysq"] = sb("ysq", [128, 512], F32)
        for nm in ("s1", "s2", "mean", "msq", "var", "rstd"):
            m[nm] = sb("gn_" + nm, [128, 4], F32)
        m["yn"] = sb("yn", [128, 512], BF16)
        m["t1"] = sb("t1", [128, 4, 128], BF16)
        m["YrT"] = R2("YrT", [128, 4, 128], BF16)
        B.memset("pool", m["S"][:], 0.0, [m["S"]])
        tiles = [("prev", 16 - self.n_prev + i) for i in range(self.n_prev)] + [("own", i) for i in range(self.n_own)]
        self.first_state = True

        def front_n(u):
            if u < len(tiles):
                yield from self.gen_N(u, tiles[u])

        def front_z(u):
            if u < len(tiles):
                yield from self.gen_Z1(u, tiles[u])

        def mid(u):
            if 0 <= u < len(tiles):
                yield from self.gen_R(u, tiles[u])

        def mid2(u):
            if 0 <= u < len(tiles) and tiles[u][0] == "own":
                yield from self.gen_R2(u, tiles[u])

        def back(u):
            if 0 <= u < len(tiles) and tiles[u][0] == "own":
                yield from self.gen_O1(u, tiles[u])

        self.cur_tiles = tiles
        self.load_x(0)
        interleave(front_n(0))
        interleave(front_n(1), front_z(0))
        for u in range(len(tiles) + 2):
            interleave(front_n(u + 2), front_z(u + 1), mid(u), mid2(u - 1), back(u - 2))

    def load_x(self, u):
        tiles = self.cur_tiles
        if (u in tiles) if isinstance(tiles, dict) else (0 <= u < len(tiles)):
            kind, ti = tiles[u]
            src = self.d["xprev"] if kind == "prev" else self.d["x"]
            xt = self.m["x"][u % len(self.m["x"])]
            self.B.dma(xt[:], src[ti * 128:(ti + 1) * 128, :], [src], [xt])

    def gen_N(self, u, tile):
        B, d, c, m = self.B, self.d, self.c, self.m
        kind, ti = tile
        xt = m["x"][u % len(m["x"])]
        ss, rs, hb, hT = m["ss"][u % 2], m["rs"][u % 2], m["hb"], m["hT"][u % 2]
        self.load_x(u + 1)
        B.act(m["junk"][:], xt[:], AF.Square, [xt, ss], [m["junk"], ss], accum_out=ss[:])
        self.rsqrt(rs[:], ss[:], 1, 1.0 / D, [ss], [rs])
        B.act(hb[:], xt[:], AF.Copy, [xt, rs], [hb], scale=rs[:, 0:1])
        yield
        pb = self.psb[2]
        pbb = pb[:].bitcast(BF16)
        for cc in range(8):
            B.tr(pbb[:, cc * 128:(cc + 1) * 128], hb[:, cc * 128:(cc + 1) * 128], c["ident"][:], [hb, c["ident"]], [pb])
        B.cp("dve", hT[:], pbb[:, 0:D], [pb], [hT])
        yield

    def zgroup(self, u, c0, nchunk):
        B, W, m = self.B, self.W, self.m
        hT = m["hT"][u % 2]
        pb = self.psb[self.zbank % 2]
        self.zbank += 1
        pv = pb[:].rearrange("p (a q) -> p a q", a=4)
        for a in range(nchunk):
            for kc in range(8):
                B.mm(pv[:, a, :], W["in"][:, kc, c0 + a * 128:c0 + (a + 1) * 128], hT[:, kc * 128:(kc + 1) * 128],
                     kc == 0, kc == 7, [W["in"], hT], [pb])
        return pb, pv

    def ztok(self, u, c0, n):
        B, W, m = self.B, self.W, self.m
        hT = m["hT"][u % 2]
        pb = self.psb[self.zbank % 2]
        self.zbank += 1
        for kc in range(8):
            B.mm(pb[:, 0:n], hT[:, kc * 128:(kc + 1) * 128], W["in"][:, kc, c0:c0 + n], kc == 0, kc == 7, [hT, W["in"]], [pb])
        return pb

    def gen_Z1(self, u, tile):
        B, d, c, m, W = self.B, self.d, self.c, self.m, self.W
        kind, ti = tile
        own = kind == "own"
        s = u % 2
        pb, pv = self.zgroup(u, 512, 4)
        B.tt("dve", m["kT"][s][:], pv, c["kdec"][:], ALU.mult, [pb, c["kdec"]], [m["kT"][s]])
        yield
        pb = self.ztok(u, 1024, 512)
        B.cp("act", m["vtok"][s][:], pb[:], [pb], [m["vtok"][s]])
        yield
        if not own:
            return
        pb, pv = self.zgroup(u, 0, 4)
        B.tt("dve", m["qT"][s][:], pv, c["qdec"][:], ALU.mult, [pb, c["qdec"]], [m["qT"][s]])
        yield
        pb, pv = self.zgroup(u, 1536, 4)
        B.act(m["th"][:], pv, AF.Tanh, [pb], [m["th"]], scale=0.5)
        B.stt("dve", m["sgT"][u % 3][:], m["th"][:], 1.0, pv, ALU.add, ALU.mult, [pb, m["th"]], [m["sgT"][u % 3]])
        yield
        for g2 in range(2):
            pb, pv = self.zgroup(u, 2048 + g2 * 512, 4)
            B.act(m["sigr"][u % 4][:, g2 * 4:(g2 + 1) * 4, :], pv, AF.Tanh, [pb], [m["sigr"][u % 4]], scale=0.5)
            yield

    def gen_R(self, u, tile):
        B, d, c, m, W = self.B, self.d, self.c, self.m, self.W
        kind, ti = tile
        own = kind == "own"
        s = u % 2
        kT, qT, vtok, ktok = m["kT"][s], m["qT"][s], m["vtok"][s], m["ktok"]
        S, Shat = m["S"], m["Shat"]
        if not self.first_state:
            B.tt("dve", S[:], S[:], c["adec"][:], ALU.mult, [S, c["adec"]], [S])
        self.first_state = False
        if own:
            B.cp("act", Shat[:], S[:], [S], [Shat])
        p3 = self.psb[3]
        p3b = p3[:].bitcast(BF16)
        for h in range(4):
            B.tr(p3b[:, h * 128:(h + 1) * 128], kT[:, h, :], c["ident"][:], [kT, c["ident"]], [p3])
        B.cp("act", ktok[:], p3b[:, 0:512], [p3], [ktok])
        yield
        p4 = self.psb[4]
        if own:
            p4v = p4[:].rearrange("p (a q) -> p a q", a=4)
            for h in range(4):
                B.mm(p4v[:, h, :], kT[:, h, :], qT[:, h, :], True, True, [kT, qT], [p4])
            B.tt("dve", m["scT"][:], p4v, c["cmask"][:], ALU.mult, [p4, c["cmask"]], [m["scT"]])
            yield
            p5 = self.psb[5 + s]
            for h in range(4):
                B.mm(p5[:, h * 128:(h + 1) * 128], m["scT"][:, h, :], vtok[:, h * 128:(h + 1) * 128], True, False, [m["scT"], vtok], [p5])
                B.mm(p5[:, h * 128:(h + 1) * 128], qT[:, h, :], Shat[:, h * 128:(h + 1) * 128], False, True, [qT, Shat], [p5])
            yield
        for h in range(4):
            B.mm(p4[:, h * 128:(h + 1) * 128], ktok[:, h * 128:(h + 1) * 128], vtok[:, h * 128:(h + 1) * 128], True, True, [ktok, vtok], [p4])
        B.tt("dve", S[:], S[:], p4[:], ALU.add, [S, p4], [S])
        yield

    def gen_R2(self, u, tile):
        B, d, c, m, W = self.B, self.d, self.c, self.m, self.W
        kind, ti = tile
        s = u % 2
        p5 = self.psb[5 + s]
        p5v = p5[:].rearrange("p (a q) -> p a q", a=4)
        B.act(m["ysq"][:], p5[:], AF.Square, [p5], [m["ysq"]])
        B.S.add("dve", lambda e: e.tensor_reduce(m["s1"][:], p5v, AX.X, ALU.add), [p5.b], [m["s1"].b, p5.b])
        B.S.add("dve", lambda e: e.tensor_reduce(m["s2"][:], m["ysq"][:].rearrange("p (a q) -> p a q", a=4), AX.X, ALU.add), [m["ysq"].b], [m["s2"].b])
        B.ts("dve", m["mean"][:], m["s1"][:], 1.0 / 128, None, ALU.mult, None, [m["s1"]], [m["mean"]])
        B.tt("dve", m["msq"][:], m["mean"][:], m["mean"][:], ALU.mult, [m["mean"]], [m["msq"]])
        B.stt("dve", m["var"][:], m["s2"][:], 1.0 / 128, m["msq"][:], ALU.mult, ALU.subtract, [m["s2"], m["msq"]], [m["var"]])
        self.rsqrt(m["rstd"][:], m["var"][:], 4, 1.0, [m["var"]], [m["rstd"]])
        yield
        ynv = m["yn"][:].rearrange("p (a q) -> p a q", a=4)
        B.tt("dve", m["ysq"][:].rearrange("p (a q) -> p a q", a=4), p5v, m["mean"][:].unsqueeze(2).to_broadcast([128, 4, 128]), ALU.subtract,
             [p5, m["mean"]], [m["ysq"]])
        B.tt("dve", ynv, m["ysq"][:].rearrange("p (a q) -> p a q", a=4), m["rstd"][:].unsqueeze(2).to_broadcast([128, 4, 128]), ALU.mult,
             [m["ysq"], m["rstd"]], [m["yn"]])
        yield
        p3 = self.psb[3]
        p3b = p3[:].bitcast(BF16)
        for h in range(4):
            B.tr(p3b[:, h * 128:(h + 1) * 128], m["yn"][:, h * 128:(h + 1) * 128], c["ident"][:], [m["yn"], c["ident"]], [p3])
        for h in range(4):
            B.act(m["t1"][:, h, :], p3b[:, h * 128:(h + 1) * 128], AF.Identity, [p3, c["gng"], c["gnb"]], [m["t1"]],
                  bias=c["gnb"][:, h:h + 1], scale=c["gng"][:, h:h + 1])
        yield
        sgT = m["sgT"][u % 3]
        B.tt("pool", m["YrT"][s][:], m["t1"][:], sgT[:], ALU.mult, [m["t1"], sgT], [m["YrT"][s]])
        yield

    def gen_O1(self, u, tile):
        B, d, c, m, W = self.B, self.d, self.c, self.m, self.W
        kind, ti = tile
        s = u % 2
        sigr = m["sigr"][u % 4]
        YrT = m["YrT"][s]
        for g2 in range(2):
            pb = self.psb[7]
            pv = pb[:].rearrange("p (a q) -> p a q", a=4)
            for a in range(4):
                oc = g2 * 4 + a
                for kc in range(4):
                    B.mm(pv[:, a, :], W["ret_o"][:, kc, oc * 128:(oc + 1) * 128], YrT[:, kc, :], kc == 0, kc == 3, [W["ret_o"], YrT], [pb])
            B.stt("dve", self.grT[:, ti, g2 * 4:(g2 + 1) * 4, :], sigr[:, g2 * 4:(g2 + 1) * 4, :], 1.0, pv, ALU.add, ALU.mult, [pb, sigr], [self.grT])
            yield

    def phaseA2(self):
        B, d, c = self.B, self.d, self.c
        sb = B.sb
        self.lc(["swab_prev", "swab_cur", "swab_first", "blockones", "gqd", "gkd"])
        c["esink"] = sb("c_esink", [128, 8], F32)
        B.dma(c["esink"][:], d["sinks"][:].partition_broadcast(128), [d["sinks"]], [c["esink"]])
        B.act(c["esink"][:], c["esink"][:], AF.Exp, [c["esink"]], [c["esink"]])
        c["gqk"] = sb("c_gqk", [128, 1], F32)
        B.stt("dve", c["gqk"][:], c["gqd"][:], 0.125, c["gkd"][:], ALU.mult, ALU.mult, [c["gqd"], c["gkd"]], [c["gqk"]])
        W = self.W = {}
        W["in"] = sb("w2_bf", [128, 8, 1920], BF16)
        W["swa_o"] = sb("w_swa_o_bf", [128, 4, D], BF16)
        W["out"] = sb("w_out_bf", [128, 8, D], BF16)
        W["rt"] = sb("wrt_bf", [128, 8, 36], BF16)
        W["rt_lo"] = sb("wrt_lo_bf", [128, 8, 36], BF16)
        wrt_f = sb("wrt_f", [128, 8, 36], F32)
        mk_st = B.mark()
        self.new_stage()
        self.load_win(W["in"], [
            (C_SQ, C_SV + 128, [(0, 0, 512), (512, 512, 576), (576, 512, 576), (640, 576, 640), (704, 576, 640), (768, 640, 768)]),
            (C_GS, C_GS + 1024, [(896, 0, 1024)])])
        self.load_rows(W["swa_o"], "w_swa_o", 4, const_scale=0.5)
        self.load_rows(W["out"], "w_out", 8)
        st = self.next_stage()
        stv = st[:, 0:288].rearrange("p (c n) -> p c n", c=8)
        B.dma(stv, d["w_rt"][:].rearrange("(c p) n -> p c n", p=128), [d["w_rt"]], [st])
        B.cp("dve", W["rt"][:], stv, [st], [W["rt"]])
        B.cp("dve", wrt_f[:], W["rt"][:], [W["rt"]], [wrt_f])
        B.tt("dve", W["rt_lo"][:], stv, wrt_f[:], ALU.subtract, [st, wrt_f], [W["rt_lo"]])
        B.S.barrier()
        B.release(mk_st)
        c["gffn_row"] = sb("gffn_row", [128, D], F32)
        B.dma(c["gffn_row"][:], d["gffn_row"][:].partition_broadcast(128), [d["gffn_row"]], [c["gffn_row"]])
        c["brt"] = sb("brt_b", [128, 36], F32)
        B.dma(c["brt"][:], d["brt"][:].partition_broadcast(128), [d["brt"]], [c["brt"]])
        m = self.m = {}
        self.common_bufs(2)
        m["hb2"] = sb("hb2", [128, D], BF16)
        m["hf2"] = sb("hf2", [128, D], F32)
        m["hl2"] = sb("hl2", [128, D], BF16)
        m["loT"] = sb("loT", [128, 8, 128], BF16)
        m["ss2"] = sb("ss2", [128, 1], F32)
        m["rs2"] = sb("rs2", [128, 1], F32)
        R2 = lambda nm, shp, dt, n=2: [sb("%s%d" % (nm, i), shp, dt) for i in range(n)]
        m["sigs"] = R2("sigs", [128, 8, 128], BF16, 3)
        m["sqT"] = R2("sqT", [128, 4, 128], BF16)
        m["skT"] = R2("skT", [128, 2, 128], BF16, 3)
        m["vaug"] = R2("vaug", [128, 2, 65], BF16, 3)
        m["sqf"] = sb("sqf", [128, 512], F32)
        m["sq2"] = sb("sq2", [128, 512], BF16)
        m["rq"] = sb("rq", [128, 512], F32)
        m["skf"] = sb("skf", [128, 256], F32)
        m["sk2"] = sb("sk2", [128, 256], BF16)
        m["rk"] = sb("rk", [128, 256], F32)
        m["pT"] = R2("pT", [128, 4, 128], BF16, 4)
        m["den"] = sb("den", [128, 8], F32)
        m["rden"] = sb("rden", [128, 8], F32)
        m["ystok"] = sb("ystok", [128, 512], BF16)
        m["ysT"] = R2("ysT", [128, 4, 128], BF16)
        m["gs"] = R2("gs", [128, 4, 128], BF16)
        m["mrg"] = sb("mrg", [128, 8, 128], BF16)
        m["x1t"] = R2("x1t", [128, D], F32, 2)
        for i in range(3):
            B.memset("pool", m["vaug"][i][:], 1.0, [m["vaug"][i]])
        tiles = {15: ("prev", 15)}
        for i in range(self.n_own):
            tiles[16 + i] = ("own", i)
        us = sorted(tiles)

        def front_n(u):
            if u in tiles:
                yield from self.gen_N(u, tiles[u])

        def front_z(u):
            if u in tiles:
                yield from self.gen_Z2(u, tiles[u])

        def mid(u):
            if u in tiles and tiles[u][0] == "own":
                yield from self.gen_W(u, tiles[u])

        def back(u):
            if u in tiles and tiles[u][0] == "own":
                yield from self.gen_O2(u, tiles[u])

        def back2(u):
            if u in tiles and tiles[u][0] == "own":
                yield from self.gen_O2b(u, tiles[u])

        self.cur_tiles = tiles
        self.load_x(us[0])
        interleave(front_n(us[0]))
        interleave(front_n(us[0] + 1), front_z(us[0]))
        for u in range(us[0], us[-1] + 3):
            interleave(front_n(u + 2), front_z(u + 1), mid(u), back(u - 1), back2(u - 2))

    def gen_Z2(self, u, tile):
        B, d, c, m, W = self.B, self.d, self.c, self.m, self.W
        kind, ti = tile
        own = kind == "own"
        s = u % 2
        pb, pv = self.zgroup(u, 512, 2)
        B.cp("dve", m["skf"][:], pb[:, 0:256], [pb], [m["skf"]])
        B.act(m["sk2"][:], pb[:, 0:256], AF.Square, [pb], [m["sk2"]])
        p7 = self.psb[2]
        B.mm(p7[:, 0:256], c["blockones"][:], m["sk2"][:], True, True, [c["blockones"], m["sk2"]], [p7])
        B.act(m["rk"][:], p7[:, 0:256], AF.Sqrt, [p7], [m["rk"]], bias=EPS, scale=1.0)
        B.S.add("dve", lambda e: e.reciprocal(m["rk"][:], m["rk"][:]), [m["rk"].b], [m["rk"].b])
        B.stt("dve", m["skT"][u % 3][:], m["skf"][:].rearrange("p (a q) -> p a q", a=2), c["gqk"][:, 0:1],
              m["rk"][:].rearrange("p (a q) -> p a q", a=2), ALU.mult, ALU.mult, [m["skf"], m["rk"], c["gqk"]], [m["skT"][u % 3]])
        yield
        pb = self.ztok(u, 768, 128)
        B.cp("dve", m["vaug"][u % 3][:, :, 0:64], pb[:, 0:128].rearrange("p (a q) -> p a q", a=2), [pb], [m["vaug"][u % 3]])
        yield
        if not own:
            return
        pb, pv = self.zgroup(u, 0, 4)
        B.cp("dve", m["sqf"][:], pb[:], [pb], [m["sqf"]])
        B.act(m["sq2"][:], pb[:], AF.Square, [pb], [m["sq2"]])
        p7 = self.psb[2]
        B.mm(p7[:], c["blockones"][:], m["sq2"][:], True, True, [c["blockones"], m["sq2"]], [p7])
        B.act(m["rq"][:], p7[:], AF.Sqrt, [p7], [m["rq"]], bias=EPS, scale=1.0)
        B.S.add("dve", lambda e: e.reciprocal(m["rq"][:], m["rq"][:]), [m["rq"].b], [m["rq"].b])
        B.tt("pool", m["sqT"][s][:], m["sqf"][:].rearrange("p (a q) -> p a q", a=4),
             m["rq"][:].rearrange("p (a q) -> p a q", a=4), ALU.mult, [m["sqf"], m["rq"]], [m["sqT"][s]])
        yield
        for g2 in range(2):
            pb, pv = self.zgroup(u, 896 + g2 * 512, 4)
            B.act(m["sigs"][u % 3][:, g2 * 4:(g2 + 1) * 4, :], pv, AF.Tanh, [pb], [m["sigs"][u % 3]], scale=0.5)
            yield

    def gen_W(self, u, tile):
        B, d, c, m, W = self.B, self.d, self.c, self.m, self.W
        kind, ti = tile
        s = u % 2
        sp_ = (u - 1) % 2
        sqT = m["sqT"][s]
        first = (ti == 0)
        k = 0
        pvb = [self.psb[6], self.psb[6]]
        for kh in range(2):
            pts = []
            for kt in range(2):
                slot = (u - 1) % 3 if kt == 0 else u % 3
                skT = m["skT"][slot]
                bias = c["swab_cur"] if kt == 1 else (c["swab_first"] if first else c["swab_prev"])
                pT = m["pT"][kh * 2 + kt]
                pT4 = pT[:].rearrange("p (a b) q -> p a b q", a=2)
                for par in range(2):
                    po = par * 64
                    pb = self.psb[4 + par]
                    pv2 = pb[:, 0:256].rearrange("p (a q) -> p a q", a=2)
                    B.mm(pv2, skT[po:po + 64, kh, :], sqT[po:po + 64, kh * 2:kh * 2 + 2, :], True, False, [skT, sqT], [pb])
                    for hl_ in range(2):
                        b4 = bias[:, hl_, kh * 4:(kh + 1) * 4, :].rearrange("p (a b) q -> p a b q", a=2)
                        B.mm(pv2, c["ident"][:], b4[:, :, par, :], False, hl_ == 1, [c["ident"], bias], [pb])
                    B.act(pT4[:, :, par, :], pv2, AF.Exp, [pb], [pT])
                pts.append((pT, m["vaug"][slot]))
                yield
            po_ = pvb[kh]
            pov = po_[:, 0:260].rearrange("p (a q) -> p a q", a=4)
            for g in range(4):
                for kt in range(2):
                    pT, va = pts[kt]
                    B.mm(pov[:, g, :], pT[:, g, :], va[:, kh, :], kt == 0, kt == 1, [pT, va], [po_])
            B.tt("dve", m["den"][:, kh * 4:(kh + 1) * 4], pov[:, :, 64], c["esink"][:, kh * 4:(kh + 1) * 4], ALU.add, [po_, c["esink"]], [m["den"]])
            B.S.add("dve", lambda e, kh=kh: e.reciprocal(m["rden"][:, kh * 4:(kh + 1) * 4], m["den"][:, kh * 4:(kh + 1) * 4]), [m["den"].b], [m["rden"].b])
            B.tt("dve", m["ystok"][:, kh * 256:(kh + 1) * 256].rearrange("p (a q) -> p a q", a=4), pov[:, :, 0:64],
                 m["rden"][:, kh * 4:(kh + 1) * 4].unsqueeze(2).to_broadcast([128, 4, 64]), ALU.mult, [po_, m["rden"]], [m["ystok"]])
            yield
        p3 = self.psb[3]
        p3b = p3[:].bitcast(BF16)
        for h in range(4):
            B.tr(p3b[:, h * 128:(h + 1) * 128], m["ystok"][:, h * 128:(h + 1) * 128], c["ident"][:], [m["ystok"], c["ident"]], [p3])
        B.cp("dve", m["ysT"][s][:], p3b[:, 0:512].rearrange("p (a q) -> p a q", a=4), [p3], [m["ysT"][s]])
        yield

    def gen_O2(self, u, tile):
        B, d, c, m, W = self.B, self.d, self.c, self.m, self.W
        kind, ti = tile
        s = u % 2
        sigs = m["sigs"][u % 3]
        ysT = m["ysT"][s]
        x1t = m["x1t"][s]
        B.dma(x1t[:], d["x"][ti * 128:(ti + 1) * 128, :], [d["x"]], [x1t])
        for g2 in range(2):
            pb = self.psb[7]
            pv = pb[:].rearrange("p (a q) -> p a q", a=4)
            for a in range(4):
                oc = g2 * 4 + a
                for kc in range(4):
                    B.mm(pv[:, a, :], W["swa_o"][:, kc, oc * 128:(oc + 1) * 128], ysT[:, kc, :], kc == 0, kc == 3, [W["swa_o"], ysT], [pb])
            B.stt("dve", m["gs"][g2][:], sigs[:, g2 * 4:(g2 + 1) * 4, :], 1.0, pv, ALU.add, ALU.mult, [pb, sigs], [m["gs"][g2]])
            B.tt("pool", m["mrg"][:, g2 * 4:(g2 + 1) * 4, :], self.grT[:, ti, g2 * 4:(g2 + 1) * 4, :], m["gs"][g2][:], ALU.add,
                 [self.grT, m["gs"][g2]], [m["mrg"]])
            yield
        for half in range(2):
            pb = self.psb[7]
            for kc in range(8):
                B.mm(pb[:], m["mrg"][:, kc, :], W["out"][:, kc, half * 512:(half + 1) * 512], kc == 0, kc == 7, [m["mrg"], W["out"]], [pb])
            B.tt("dve", x1t[:, half * 512:(half + 1) * 512], pb[:], x1t[:, half * 512:(half + 1) * 512], ALU.add, [pb, x1t], [x1t])
            yield
        if "x1" in self.dbg:
            op = B.dma(self.out[ti * 128:(ti + 1) * 128, :], x1t[:], [x1t], [self.out])
            B.S.final_waits.append(op)
        else:
            B.dma(self.x1s[ti * 128:(ti + 1) * 128, :], x1t[:], [x1t], [self.x1s])
        yield

    def gen_O2b(self, u, tile):
        B, d, c, m, W = self.B, self.d, self.c, self.m, self.W
        kind, ti = tile
        x1t = m["x1t"][u % 2]
        ss, rs, hb = m["ss2"], m["rs2"], m["hb2"]
        B.act(m["junk"][:], x1t[:], AF.Square, [x1t, ss], [m["junk"], ss], accum_out=ss[:])
        self.rsqrt(rs[:], ss[:], 1, 1.0 / D, [ss], [rs])
        hf, hl, loT = m["hf2"], m["hl2"], m["loT"]
        B.stt("dve", hf[:], x1t[:], rs[:, 0:1], c["gffn_row"][:], ALU.mult, ALU.mult, [x1t, rs, c["gffn_row"]], [hf])
        B.cp("act", hb[:], hf[:], [hf], [hb])
        B.tt("pool", hl[:], hf[:], hb[:], ALU.subtract, [hf, hb], [hl])
        yield
        p3 = self.psb[3]
        p3b = p3[:].bitcast(BF16)
        for cc in range(8):
            B.tr(p3b[:, cc * 128:(cc + 1) * 128], hb[:, cc * 128:(cc + 1) * 128], c["ident"][:], [hb, c["ident"]], [p3])
        h2v = self.h2T[:, :, ti * 128:(ti + 1) * 128]
        B.cp("act", h2v, p3b[:, 0:D].rearrange("p (a q) -> p a q", a=8), [p3], [self.h2T])
        yield
        for cc in range(8):
            B.tr(p3b[:, cc * 128:(cc + 1) * 128], hl[:, cc * 128:(cc + 1) * 128], c["ident"][:], [hl, c["ident"]], [p3])
        B.cp("dve", loT[:], p3b[:, 0:D].rearrange("p (a q) -> p a q", a=8), [p3], [loT])
        yield
        p6 = self.psb[3]
        k = 0
        for (lhs_t, lhs_ap, wt) in [(self.h2T, None, W["rt"]), (loT, loT, W["rt"]), (self.h2T, None, W["rt_lo"])]:
            for kc in range(8):
                lhsT = self.h2T[:, kc, ti * 128:(ti + 1) * 128] if lhs_ap is None else loT[:, kc, :]
                B.mm(p6[:, 0:36], lhsT, wt[:, kc, :], k == 0, k == 23, [lhs_t, wt], [p6])
                k += 1
        B.tt("dve", self.lg_all[:, ti, :], p6[:, 0:36], c["brt"][:], ALU.add, [p6, c["brt"]], [self.lg_all])
        yield

    def moe(self, n_groups):
        B, d, c = self.B, self.d, self.c
        sb = B.sb
        m = self.m = {}
        self.acc = sb("acc", [128, 16, D], F32)
        self.mk_after_acc = B.mark()
        self.lc(["sel"])
        self.new_stage(3, 2048)
        h2T = self.h2T
        NTL = self.n_own
        cT = sb("cT", [64, TOK], BF16)
        F = lambda nm, shp: sb("r_" + nm, shp, F32)
        gmax, gd, ge, gsum, gw = F("gmax", [128, 16]), F("gd", [128, 16, 4]), F("ge", [128, 16, 4]), F("gsum", [128, 16]), F("gw", [128, 16])
        oh, pen = F("oh", [128, 16, 4]), F("pen", [128, 16, 4])
        elm, m1, mask1 = F("elm", [128, 16, 32]), F("m1", [128, 16]), F("mask1", [128, 16, 32])
        elm2, m2, mask2 = F("elm2", [128, 16, 32]), F("m2", [128, 16]), F("mask2", [128, 16, 32])
        dm, ed, w1, w2 = F("dm", [128, 16]), F("ed", [128, 16]), F("w1", [128, 16]), F("w2", [128, 16])
        c1, cc, chf, clo = mask1, mask2, elm, elm2
        chl = sb("chl", [128, 16, 64], BF16)
        lg = self.lg_all
        L = lambda fn, rd, wr: B.S.add("dve", fn, [t.b for t in rd], [t.b for t in wr])
        bc = lambda t, shp: t[:].unsqueeze(2).to_broadcast(shp)
        L(lambda e: e.tensor_reduce(gmax[:], lg[:, :, 0:4], AX.X, ALU.max), [lg], [gmax])
        B.tt("dve", gd[:], lg[:, :, 0:4], bc(gmax, [128, 16, 4]), ALU.subtract, [lg, gmax], [gd])
        B.act(ge[:], gd[:], AF.Exp, [gd], [ge])
        L(lambda e: e.tensor_reduce(gsum[:], ge[:], AX.X, ALU.add), [ge], [gsum])
        L(lambda e: e.reciprocal(gw[:], gsum[:]), [gsum], [gw])
        B.tt("dve", oh[:], lg[:, :, 0:4], bc(gmax, [128, 16, 4]), ALU.is_equal, [lg, gmax], [oh])
        B.ts("dve", pen[:], oh[:], 1e30, -1e30, ALU.mult, ALU.add, [oh], [pen])
        B.tt("dve", elm[:].rearrange("p t (a q) -> p t a q", a=4), lg[:, :, 4:36].rearrange("p t (a q) -> p t a q", a=4),
             pen[:].unsqueeze(3).to_broadcast([128, 16, 4, 8]), ALU.add, [lg, pen], [elm])
        L(lambda e: e.tensor_reduce(m1[:], elm[:], AX.X, ALU.max), [elm], [m1])
        B.tt("dve", mask1[:], elm[:], bc(m1, [128, 16, 32]), ALU.is_equal, [elm, m1], [mask1])
        B.stt("dve", elm2[:], mask1[:], -1e30, elm[:], ALU.mult, ALU.add, [mask1, elm], [elm2])
        L(lambda e: e.tensor_reduce(m2[:], elm2[:], AX.X, ALU.max), [elm2], [m2])
        B.tt("dve", mask2[:], elm2[:], bc(m2, [128, 16, 32]), ALU.is_equal, [elm2, m2], [mask2])
        B.tt("dve", dm[:], m2[:], m1[:], ALU.subtract, [m2, m1], [dm])
        B.act(ed[:], dm[:], AF.Exp, [dm], [ed])
        B.ts("dve", w1[:], ed[:], 1.0, None, ALU.add, None, [ed], [w1])
        L(lambda e: e.reciprocal(w1[:], w1[:]), [w1], [w1])
        B.tt("dve", w1[:], w1[:], gw[:], ALU.mult, [w1, gw], [w1])
        B.tt("dve", w2[:], w1[:], ed[:], ALU.mult, [w1, ed], [w2])
        B.tt("dve", c1[:], mask1[:], bc(w1, [128, 16, 32]), ALU.mult, [mask1, w1], [c1])
        B.tt("dve", cc[:], mask2[:], bc(w2, [128, 16, 32]), ALU.mult, [mask2, w2], [cc])
        B.tt("dve", cc[:], cc[:], c1[:], ALU.add, [cc, c1], [cc])
        B.cp("dve", chl[:, :, 0:32], cc[:], [cc], [chl])
        B.cp("dve", chf[:], chl[:, :, 0:32], [chl], [chf])
        B.tt("dve", clo[:], cc[:], chf[:], ALU.subtract, [cc, chf], [clo])
        B.cp("dve", chl[:, :, 32:64], clo[:], [clo], [chl])
        for hf in range(2):
            p5 = self.psb[5 + hf]
            p5b = p5[:].bitcast(BF16)
            nt = min(8, NTL - hf * 8)
            if nt <= 0:
                break
            for k in range(nt):
                B.tr(p5b[0:64, k * 128:(k + 1) * 128], chl[:, hf * 8 + k, :], c["ident"][:], [chl, c["ident"]], [p5])
            B.cp("act", cT[:, hf * 1024:hf * 1024 + nt * 128], p5b[0:64, 0:nt * 128], [p5], [cT])
        if "comb" in self.dbg:
            B.dump("cT", cT, cT[:], [64, TOK], BF16)
        NG = n_groups
        slots = [[{"g": sb("wg%d%d" % (i, j), [128, 8, 256], BF16), "u": sb("wu%d%d" % (i, j), [128, 8, 256], BF16),
                   "d": sb("wd%d%d" % (i, j), [128, 2, D], BF16)} for j in range(2)] for i in range(2)]
        sg = [sb("sg%d" % i, [128, 512], BF16) for i in range(2)]
        tu = [sb("tu%d" % i, [128, 512], BF16) for i in range(2)]
        hid = [[[sb("hid%d%d%d" % (i, j, k), [128, 512], BF16) for k in range(2)] for j in range(2)] for i in range(2)]

        def gen_load(gi):
            work = []
            for j in range(2):
                e = gi * 2 + j
                sl = slots[gi % 2][j]
                work.append((sl["g"], d["w_gate"], e, 8))
                work.append((sl["u"], d["w_up"], e, 8))
                work.append((sl["d"], d["w_down"], e, 2))

            def issue(k):
                dst, src, e, cdim = work[k]
                st = self.next_stage()
                stv = st[:].rearrange("p (c n) -> p c n", c=cdim)
                B.dma(stv, src[e].rearrange("(c p) n -> p c n", p=128), [src], [st])
                return st, stv

            q = [issue(k) for k in range(min(3, len(work)))]
            for k in range(len(work)):
                st, stv = q[k]
                self.cast(work[k][0][:], stv, [st], [work[k][0]])
                if k + 3 < len(work):
                    q.append(issue(k + 3))
                yield

        def load_group(gi):
            interleave(gen_load(gi))

        self.cast_engs = ("act",)
        load_group(0)
        for t in range(NTL):
            B.dma(self.acc[:, t, :], self.x1s[t * 128:(t + 1) * 128, :], [self.x1s], [self.acc])
        cnt = {"nb": 0, "nd": 0, "npc": 0}
        nchunk = self.n_own // 4

        def gen_gu(gi, ch):
            cs = slice(ch * 512, (ch + 1) * 512)
            hs = hid[ch % 2]
            for j in range(2):
                e = gi * 2 + j
                sl = slots[gi % 2][j]
                pc = self.psb[4 + 3 * (cnt["npc"] % 2)]
                cnt["npc"] += 1
                for fc in range(2):
                    nb = cnt["nb"]
                    pa, pu = self.psb[nb % 2], self.psb[2 + nb % 2]
                    cnt["nb"] += 1
                    nb += 1
                    for kc in range(8):
                        B.mm(pa[:], sl["g"][:, kc, fc * 128:(fc + 1) * 128], h2T[:, kc, cs], kc == 0, kc == 7, [sl["g"], h2T], [pa])
                    B.act(sg[nb % 2][:], pa[:], AF.Silu, [pa], [sg[nb % 2]])
                    yield
                    for kc in range(8):
                        B.mm(pu[:], sl["u"][:, kc, fc * 128:(fc + 1) * 128], h2T[:, kc, cs], kc == 0, kc == 7, [sl["u"], h2T], [pu])
                    if fc == 0:
                        B.mm(pc[:], c["sel"][:, e, :], cT[:, cs], True, True, [c["sel"], cT], [pc])
                    B.tt("dve", tu[nb % 2][:], pu[:], sg[nb % 2][:], ALU.mult, [pu, sg[nb % 2]], [tu[nb % 2]])
                    B.tt("dve", hs[j][fc][:], pc[:], tu[nb % 2][:], ALU.mult, [pc, tu[nb % 2]], [hs[j][fc]])
                    yield

        def gen_down(gi, ch):
            hs = hid[ch % 2]
            for tl in range(4):
                t = ch * 4 + tl
                for half in range(2):
                    pd = self.psb[5 + cnt["nd"] % 2]
                    cnt["nd"] += 1
                    k = 0
                    for j in range(2):
                        sl = slots[gi % 2][j]
                        for fc in range(2):
                            B.mm(pd[:], hs[j][fc][:, tl * 128:(tl + 1) * 128], sl["d"][:, fc, half * 512:(half + 1) * 512],
                                 k == 0, k == 3, [hs[j][fc], sl["d"]], [pd])
                            k += 1
                    a = self.acc[:, t, half * 512:(half + 1) * 512]
                    B.tt("dve", a, a, pd[:], ALU.add, [self.acc, pd], [self.acc])
                    yield

        items = [(gi, ch) for gi in range(NG) for ch in range(nchunk)]
        prev = None
        pending = None
        for (gi, ch) in items:
            interleave(gen_gu(gi, ch), gen_down(*prev) if prev is not None else None, pending)
            pending = None
            prev = (gi, ch)
            if ch == 0 and gi + 1 < NG:
                if nchunk >= 2:
                    pending = gen_load(gi + 1)
                else:
                    load_group(gi + 1)
        interleave(gen_down(*prev))

    def ple(self):
        B, d, c = self.B, self.d, self.c
        sb = B.sb
        B.S.barrier()
        B.release(self.mk_after_acc)
        m = self.m = {}
        self.cast_engs = ("act", "dve")
        self.new_stage()
        wpg = sb("wpg_bf", [128, 8, D], BF16)
        wple = sb("wple_bf", [128, 2, D], BF16)
        self.load_rows(wpg, "w_pg", 8)
        self.load_rows(wple, "w_ple", 2)
        grow = sb("gpg_row", [128, D], F32)
        B.dma(grow[:], d["gpg_row"][:].partition_broadcast(128), [d["gpg_row"]], [grow])
        gple = sb("gple_b", [128, D], F32)
        B.dma(gple[:], d["gple"][:].partition_broadcast(128), [d["gple"]], [gple])
        B.ts("pool", gple[:], gple[:], 0.5, None, ALU.mult, None, [gple], [gple])
        m["junk"] = sb("junk", [128, D], BF16)
        junk2 = sb("junk2", [128, 512], BF16)
        R2 = lambda nm, shp, dt, n=2: [sb("%s%d" % (nm, i), shp, dt) for i in range(n)]
        hb = R2("hb3", [128, D], BF16)
        h3T = R2("h3T", [128, 8, 128], BF16)
        ss, rs = R2("ss", [128, 1], F32), R2("rs", [128, 1], F32)
        ssp, rsp = R2("ssp", [128, 2], F32), R2("rsp", [128, 1], F32)
        pt = R2("pt", [128, 256], F32)
        pbf = R2("pbf", [128, 256], BF16)
        pT = R2("pT", [128, 2, 128], BF16)
        sgate = R2("sgate", [128, D], F32)
        ple = R2("ple", [128, D], F32)
        ot = R2("ot", [128, D], F32)
        NTL = self.n_own

        def front(t):
            if t >= NTL:
                return
            i = t % 2
            xa = self.acc[:, t, :]
            if t + 1 < NTL:
                B.dma(pt[(t + 1) % 2][:], d["p"][(t + 1) * 128:(t + 2) * 128, :], [d["p"]], [pt[(t + 1) % 2]])
            B.act(m["junk"][:], xa, AF.Square, [self.acc, ss[i]], [m["junk"], ss[i]], accum_out=ss[i][:])
            self.rsqrt(rs[i][:], ss[i][:], 1, 1.0 / D, [ss[i]], [rs[i]])
            B.stt("dve", hb[i][:], xa, rs[i][:, 0:1], grow[:], ALU.mult, ALU.mult, [self.acc, rs[i], grow], [hb[i]])
            B.cp("dve", pbf[i][:], pt[i][:], [pt[i]], [pbf[i]])
            yield

        def front_b(t):
            if t >= NTL:
                return
            i = t % 2
            pb = self.psb[7]
            pbb = pb[:].bitcast(BF16)
            for cc in range(8):
                B.tr(pbb[:, cc * 128:(cc + 1) * 128], hb[i][:, cc * 128:(cc + 1) * 128], c["ident"][:], [hb[i], c["ident"]], [pb])
            B.cp("act", h3T[i][:], pbb[:, 0:D].rearrange("p (a q) -> p a q", a=8), [pb], [h3T[i]])
            yield
            p4 = self.psb[6]
            p4b = p4[:].bitcast(BF16)
            for kc in range(2):
                B.tr(p4b[:, kc * 128:(kc + 1) * 128], pbf[i][:, kc * 128:(kc + 1) * 128], c["ident"][:], [pbf[i], c["ident"]], [p4])
            B.cp("dve", pT[i][:], p4b[:, 0:256].rearrange("p (a q) -> p a q", a=2), [p4], [pT[i]])
            yield

        def mid(t):
            if not (0 <= t < NTL):
                return
            i = t % 2
            for half in range(2):
                pb = self.psb[2 + 2 * i + half]
                for kc in range(2):
                    B.mm(pb[:], pT[i][:, kc, :], wple[:, kc, half * 512:(half + 1) * 512], kc == 0, kc == 1, [pT[i], wple], [pb])
                B.act(junk2[:], pb[:], AF.Square, [pb, ssp[i]], [junk2, ssp[i]], accum_out=ssp[i][:, half:half + 1])
            yield
            for half in range(2):
                pb = self.psb[half]
                for kc in range(8):
                    B.mm(pb[:], h3T[i][:, kc, :], wpg[:, kc, half * 512:(half + 1) * 512], kc == 0, kc == 7, [h3T[i], wpg], [pb])
                B.act(sgate[i][:, half * 512:(half + 1) * 512], pb[:], AF.Tanh, [pb], [sgate[i]], scale=0.5)
                yield

        def back(t):
            if not (0 <= t < NTL):
                return
            i = t % 2
            xa = self.acc[:, t, :]
            B.tt("dve", rsp[i][:], ssp[i][:, 0:1], ssp[i][:, 1:2], ALU.add, [ssp[i]], [rsp[i]])
            self.rsqrt(rsp[i][:], rsp[i][:], 1, 1.0 / D, [rsp[i]], [rsp[i]])
            yield
            for half in range(2):
                pb = self.psb[2 + 2 * i + half]
                hsl = slice(half * 512, (half + 1) * 512)
                B.stt("dve", ple[i][:, hsl], pb[:], rsp[i][:, 0:1], gple[:, hsl], ALU.mult, ALU.mult, [pb, rsp[i], gple], [ple[i]])
            yield
            o = ot[i]
            B.stt("dve", o[:], sgate[i][:], 1.0, ple[i][:], ALU.add, ALU.mult, [sgate[i], ple[i]], [o])
            B.tt("pool", o[:], o[:], xa, ALU.add, [o, self.acc], [o])
            op = B.dma(self.out[t * 128:(t + 1) * 128, :], o[:], [o], [self.out])
            B.S.final_waits.append(op)
            yield

        B.dma(pt[0][:], d["p"][0:128, :], [d["p"]], [pt[0]])
        interleave(front(0))
        interleave(front(1), front_b(0))
        interleave(front(2), front_b(1), mid(0))
        for t in range(NTL):
            interleave(front(t + 3), front_b(t + 2), mid(t + 1), back(t))


def _pc(v, n):
    return np.ascontiguousarray(np.asarray(v, np.float32).reshape(n, 128).T)


def prep_inputs(inp):
    ct = consts()
    f = lambda a: np.ascontiguousarray(np.asarray(a, np.float32))
    x = f(inp["x"])
    p = f(inp["p"])[0]
    shared = dict(
        w_in=f(inp["w_in"])[0], w_ret_o=f(inp["w_ret_o"])[0], w_swa_o=f(inp["w_swa_o"])[0], w_out=f(inp["w_out"])[0],
        w_rt=np.ascontiguousarray(np.concatenate([f(inp["w_router_group"])[0], f(inp["w_router_expert"])[0]], axis=1)),
        w_gate=f(inp["w_exp_gate"])[0], w_up=f(inp["w_exp_up"])[0], w_down=f(inp["w_exp_down"])[0],
        w_pg=f(inp["w_ple_gate"])[0], w_ple=f(inp["w_ple"])[0],
        gmix=_pc(inp["mix_norm_g"][0], 8), gffn=_pc(inp["ffn_norm_g"][0], 8), gpg=_pc(inp["ple_gate_norm_g"][0], 8),
        gng=_pc(inp["ret_gn_g"][0], 4), gnb=_pc(inp["ret_gn_b"][0], 4),
        gqd=np.ascontiguousarray(np.tile(f(inp["q_norm_g"])[0], 2).reshape(128, 1)),
        gkd=np.ascontiguousarray(np.tile(f(inp["k_norm_g"])[0], 2).reshape(128, 1)),
        sinks=f(inp["attn_sinks"]).reshape(1, 8),
        brt=np.ascontiguousarray(np.concatenate([f(inp["b_router_group"])[0], f(inp["b_router_expert"])[0]]).reshape(1, 36)),
        gple=f(inp["ple_norm_g"]).reshape(1, D),
        gffn_row=f(inp["ffn_norm_g"]).reshape(1, D), gpg_row=f(inp["ple_gate_norm_g"]).reshape(1, D),
        ident=ct["ident"], qdec=ct["qdec"], kdec=ct["kdec"], adec=ct["adec"], cmask=ct["cmask"],
        swab_prev=ct["swab_prev"], swab_cur=ct["swab_cur"], blockones=ct["blockones"], sel=ct["sel"],
    )
    maps = []
    zeros = np.zeros((TOK, D), np.float32)
    allneg = ct["swab_neg"]
    for cid in range(NCORES):
        b, half = cid // 2, cid % 2
        mp = dict(shared)
        mp["x"] = np.ascontiguousarray(x[b, half * TOK:(half + 1) * TOK])
        mp["xprev"] = np.ascontiguousarray(x[b, 0:TOK]) if half == 1 else zeros
        mp["p"] = np.ascontiguousarray(p[b, half * TOK:(half + 1) * TOK])
        mp["swab_first"] = ct["swab_prev"] if half == 1 else allneg
        maps.append(mp)
    return maps


_PROG = None


def kernel(**inputs):
    global _PROG
    if _PROG is None:
        _PROG = Prog()
    maps = prep_inputs(inputs)
    res = run_bass_kernel_spmd(_PROG.B.nc, maps, core_ids=list(range(NCORES)))
    out = np.empty((4, 4096, D), np.float32)
    for cid in range(NCORES):
        b, half = cid // 2, cid % 2
        out[b, half * TOK:(half + 1) * TOK] = res.results[cid]["out"]
    return out
```

```python
import os
import numpy as np
import ml_dtypes
import concourse.bass as bass
import concourse.mybir as mybir
from concourse.bass_utils import run_bass_kernel_spmd

F32 = mybir.dt.float32
BF16 = mybir.dt.bfloat16
AF = mybir.ActivationFunctionType
ALU = mybir.AluOpType
AX = mybir.AxisListType

D = 1024
NCORES = 8
TOK = 2048
NT = 16
EPS = 1e-6
IN_W = 4864
C_RQ, C_RK, C_RV, C_RG, C_SQ, C_SK, C_SV, C_GR, C_GS = 0, 512, 1024, 1536, 2048, 2560, 2688, 2816, 3840
NEG = -30000.0


class Buf:
    __slots__ = ("name", "w", "r", "rd")

    def __init__(self, name):
        self.name = name
        self.w = None
        self.r = {}
        self.rd = []


class Op:
    __slots__ = ("eng", "fn", "deps", "sig", "cnt", "dma", "dsem", "dval", "gidx")


class Sched:
    ENGS = ("pe", "act", "dve", "pool", "sp")

    def __init__(self, nc, n_dma_sems=8):
        self.nc = nc
        self.q = {e: [] for e in self.ENGS}
        self.n = 0
        self.n_dma_sems = n_dma_sems
        self.final_waits = []

    def add(self, eng, fn, reads=(), writes=(), dma=False, force=False):
        import os
        lim = int(os.environ.get("PROG_MAXOPS", "0"))
        if lim and self.n >= lim and not force:
            self.n += 1
            return None
        if os.environ.get("PROG_TRACE"):
            import sys as _s
            fr = _s._getframe(2)
            print("OP", self.n, eng, fr.f_code.co_name, fr.f_lineno, _s._getframe(3).f_code.co_name, _s._getframe(3).f_lineno)
        op = Op()
        op.eng = eng
        op.fn = fn
        op.dma = dma
        op.sig = dma
        op.cnt = 0
        op.gidx = self.n
        self.n += 1
        deps = {}

        def need(p):
            if p is None:
                return
            if (not p.dma) and p.eng == eng and eng == "pe":
                return
            if p.dma:
                deps[("d", p.gidx)] = p
            else:
                k = ("c", p.eng)
                if k not in deps or deps[k].gidx < p.gidx:
                    deps[k] = p

        for b in reads:
            need(b.w)
        for b in writes:
            need(b.w)
            for p in b.r.values():
                need(p)
            for p in b.rd:
                need(p)
        op.deps = list(deps.values())
        for p in op.deps:
            p.sig = True
        for b in reads:
            if dma:
                b.rd.append(op)
            else:
                b.r[eng] = op
        for b in writes:
            b.w = op
            b.r = {}
            b.rd = []
        self.q[eng].append(op)
        return op

    def barrier(self):
        lasts = []
        for e in self.ENGS:
            ops = [o for o in self.q[e] if not o.dma and o.fn is not None]
            if ops:
                lasts.append(ops[-1])
            dops = [o for o in self.q[e] if o.dma]
            lasts.extend(dops[-self.n_dma_sems:])
        for e in self.ENGS:
            op = Op()
            op.eng, op.fn, op.dma, op.sig, op.cnt, op.gidx = e, None, False, False, 0, self.n
            self.n += 1
            op.deps = [p for p in lasts if p.dma or p.eng != e]
            for p in op.deps:
                p.sig = True
            self.q[e].append(op)

    def emit(self):
        nc = self.nc
        from contextlib import ExitStack
        with ExitStack() as es:
            sems = {e: es.enter_context(nc.semaphore("s_" + e)) for e in self.ENGS}
            dsems = {}
            for e in self.ENGS:
                if any(o.dma for o in self.q[e]):
                    dsems[e] = [es.enter_context(nc.semaphore("d_%s%d" % (e, i))) for i in range(self.n_dma_sems)]
            for e in self.ENGS:
                c = 0
                k = 0
                for o in self.q[e]:
                    if o.dma:
                        o.dsem = dsems[e][k % self.n_dma_sems]
                        o.dval = 16 * (k // self.n_dma_sems + 1)
                        k += 1
                    elif o.sig:
                        c += 1
                        o.cnt = c
            block = es.enter_context(nc.Block())
            handles = {"pe": block.tensor, "act": block.scalar, "dve": block.vector, "pool": block.gpsimd, "sp": block.sync}

            def make(e):
                def body(eng):
                    known = {}

                    def wait(sem, val):
                        if known.get(sem.num, 0) >= val:
                            return
                        eng.wait_ge(sem, val)
                        known[sem.num] = val

                    for o in self.q[e]:
                        for p in o.deps:
                            if p.dma:
                                wait(p.dsem, p.dval)
                            else:
                                wait(sems[p.eng], p.cnt)
                        if o.dma and o.dval > 16:
                            wait(o.dsem, o.dval - 16)
                        if o.fn is None:
                            continue
                        ins = o.fn(eng)
                        if o.dma:
                            ins.then_inc(o.dsem, 16)
                        elif o.sig:
                            ins.then_inc(sems[e], 1)
                    if e == "sp":
                        for p in [q_ for q_ in self.final_waits if q_ is not None]:
                            wait(p.dsem, p.dval)
                return body

            for e in self.ENGS:
                if self.q[e] or e == "sp":
                    handles[e](make(e))


def _const_tables():
    t = {}
    t["ident"] = np.eye(128, dtype=np.float32).astype(ml_dtypes.bfloat16)
    h = np.arange(4, dtype=np.float64)
    gam = 1.0 - np.exp2(-5.0 - h)
    lg = np.log(gam)
    pos = np.arange(128, dtype=np.float64)
    qd = np.exp((pos[None, :] - 127.0) * lg[:, None])
    kd = np.exp((127.0 - pos[None, :]) * lg[:, None]) * (128.0 ** -0.5)
    t["qdec"] = np.broadcast_to(qd[None], (128, 4, 128)).astype(np.float32).copy()
    t["kdec"] = np.broadcast_to(kd[None], (128, 4, 128)).astype(np.float32).copy()
    a = np.exp(128.0 * lg)
    t["adec"] = np.broadcast_to(np.repeat(a, 128)[None], (128, 512)).astype(np.float32).copy()
    j = np.arange(128)[:, None]
    i = np.arange(128)[None, :]
    cm = (i >= j).astype(np.float32)
    t["cmask"] = np.broadcast_to(cm[:, None, :], (128, 4, 128)).astype(np.float32).copy()
    slopes = np.exp2(-(np.arange(8, dtype=np.float64) + 1.0))
    s = np.arange(128, dtype=np.float64)[:, None]
    q = np.arange(128, dtype=np.float64)[None, :]
    relp = q + 128.0 - s
    relc = q - s
    bp = np.where((relp >= 0) & (relp < 128), 0.0, 1.0)
    bc = np.where((relc >= 0) & (relc < 128), 0.0, 1.0)
    sbp = np.empty((128, 8, 128), np.float32)
    sbc = np.empty((128, 8, 128), np.float32)
    for hh in range(8):
        sbp[:, hh, :] = np.where(bp > 0, NEG, -slopes[hh] * relp)
        sbc[:, hh, :] = np.where(bc > 0, NEG, -slopes[hh] * relc)
    def hl(a):
        hi = a.astype(ml_dtypes.bfloat16)
        lo = (a - hi.astype(np.float32)).astype(ml_dtypes.bfloat16)
        return np.ascontiguousarray(np.stack([hi, lo], axis=1))
    t["swab_prev"] = hl(sbp)
    t["swab_cur"] = hl(sbc)
    t["swab_neg"] = hl(np.full((128, 8, 128), NEG, np.float32))
    bo = np.zeros((128, 128), np.float32)
    bo[:64, :64] = 1.0 / 64
    bo[64:, 64:] = 1.0 / 64
    t["blockones"] = bo.astype(ml_dtypes.bfloat16)
    sel = np.zeros((64, 32, 128), np.float32)
    for e in range(32):
        sel[e, e, :] = 1.0
        sel[32 + e, e, :] = 1.0
    t["sel"] = sel.astype(ml_dtypes.bfloat16)
    return t


_CT = None


def consts():
    global _CT
    if _CT is None:
        _CT = _const_tables()
    return _CT


class T:
    def __init__(self, h, name):
        self.h = h
        self.b = Buf(name)

    def __getitem__(self, k):
        return self.h[k]


class Builder:
    def __init__(self):
        self.nc = bass.Bass("TRN2", target_bir_lowering=False)
        self.S = Sched(self.nc)
        self.dbg = []

    def din(self, name, shape, dt=F32):
        return T(self.nc.dram_tensor(name, list(shape), dt, kind="ExternalInput").ap(), name)

    def dout(self, name, shape, dt=F32):
        return T(self.nc.dram_tensor(name, list(shape), dt, kind="ExternalOutput").ap(), name)

    def dscratch(self, name, shape, dt=F32):
        return T(self.nc.dram_tensor(name, list(shape), dt).ap(), name)

    def sb(self, name, shape, dt):
        esz = 2 if dt == BF16 else 4
        n = 1
        for v in shape[1:]:
            n *= v
        nbytes = (n * esz + 31) // 32 * 32
        if not hasattr(self, "sp"):
            self.sp = (self.nc.sbuf_base + 63) // 64 * 64
            self.uid = 0
        off = self.sp
        self.sp += nbytes
        assert self.sp <= self.nc.sbuf_top, "SBUF overflow at %s: %d > %d" % (name, self.sp, self.nc.sbuf_top)
        self.hw = max(getattr(self, "hw", 0), self.sp)
        self.uid += 1
        return T(self.nc.alloc_sbuf_tensor_at("%s_%d" % (name, self.uid), list(shape), dt, offset=off), name)

    def mark(self):
        return self.sp

    def release(self, mk):
        if os.environ.get("PROG_SBUF"):
            print("SBUF high-water before release: %d of %d" % (self.hw, self.nc.sbuf_top))
        self.hw = 0
        self.sp = mk

    def ps(self, name):
        t = T(self.nc.alloc_psum_tensor(name, [128, 512], F32), name)
        t.psum = True
        return t

    def _bufs(self, ts):
        return [t.b for t in ts]

    def _rw(self, rd, wr):
        wr = list(wr) + [t for t in rd if getattr(t, "psum", False)]
        return [t.b for t in rd], [t.b for t in wr]

    def dma(self, out, in_, rd, wr, q="sp"):
        return self.S.add(q, lambda e: e.dma_start(out=out, in_=in_), *self._rw(rd, wr), dma=True)

    def mm(self, out, lhsT, rhs, start, stop, rd, wr):
        return self.S.add("pe", lambda e: e.matmul(out, lhsT, rhs, start=start, stop=stop), *self._rw(rd, wr))

    def tr(self, out, in_, ident, rd, wr):
        return self.S.add("pe", lambda e: e.transpose(out, in_, ident), *self._rw(rd, wr))

    def act(self, out, in_, func, rd, wr, bias=None, scale=None, accum_out=None):
        kw = {}
        if bias is not None:
            kw["bias"] = bias
        if scale is not None:
            kw["scale"] = scale
        if accum_out is not None:
            kw["accum_out"] = accum_out
        return self.S.add("act", lambda e: e.activation(out, in_, func, **kw), *self._rw(rd, wr))

    def tt(self, eng, out, in0, in1, op, rd, wr):
        return self.S.add(eng, lambda e: e.tensor_tensor(out, in0, in1, op), *self._rw(rd, wr))

    def ts(self, eng, out, in0, s1, s2, op0, op1, rd, wr):
        if op1 is None:
            return self.S.add(eng, lambda e: e.tensor_scalar(out, in0, s1, None, op0), *self._rw(rd, wr))
        return self.S.add(eng, lambda e: e.tensor_scalar(out, in0, s1, s2, op0, op1), *self._rw(rd, wr))

    def stt(self, eng, out, in0, scalar, in1, op0, op1, rd, wr):
        return self.S.add(eng, lambda e: e.scalar_tensor_tensor(out, in0, scalar, in1, op0, op1), *self._rw(rd, wr))

    def cp(self, eng, out, in_, rd, wr):
        if eng == "act":
            return self.S.add("act", lambda e: e.copy(out, in_), *self._rw(rd, wr))
        return self.S.add(eng, lambda e: e.tensor_copy(out, in_), *self._rw(rd, wr))

    def memset(self, eng, ap, val, wr):
        return self.S.add(eng, lambda e: e.memset(ap, val), [], self._bufs(wr))

    def dump(self, name, t, ap, shape, dt):
        o = self.dout("dbg_" + name, shape, dt)
        op = self.S.add("sp", lambda e: e.dma_start(out=o[:], in_=ap), [t.b], [o.b], dma=True, force=True)
        self.S.final_waits.append(op)
        self.dbg.append("dbg_" + name)


def interleave(*gens):
    gens = [g for g in gens if g is not None]
    while gens:
        for g in list(gens):
            try:
                next(g)
            except StopIteration:
                gens.remove(g)


class Prog:
    def __init__(self, n_prev=16, n_own=16, do_moe=True, do_ple=True, n_groups=16, dbg=None):
        self.B = B = Builder()
        self.n_prev, self.n_own = n_prev, n_own
        self.dbg = dbg or set()
        d = self.d = {}
        for nm, shp, dt in [
            ("x", [TOK, D], F32), ("xprev", [TOK, D], F32), ("p", [TOK, 256], F32),
            ("w_in", [D, IN_W], F32), ("w_ret_o", [512, D], F32), ("w_swa_o", [512, D], F32),
            ("w_out", [D, D], F32), ("w_rt", [D, 36], F32),
            ("w_gate", [32, D, 256], F32), ("w_up", [32, D, 256], F32), ("w_down", [32, 256, D], F32),
            ("w_pg", [D, D], F32), ("w_ple", [256, D], F32),
            ("gmix", [128, 8], F32), ("gffn", [128, 8], F32), ("gpg", [128, 8], F32),
            ("gng", [128, 4], F32), ("gnb", [128, 4], F32), ("gqd", [128, 1], F32), ("gkd", [128, 1], F32),
            ("sinks", [1, 8], F32), ("brt", [1, 36], F32), ("gple", [1, D], F32),
            ("gffn_row", [1, D], F32), ("gpg_row", [1, D], F32),
            ("ident", [128, 128], BF16), ("qdec", [128, 4, 128], F32), ("kdec", [128, 4, 128], F32),
            ("adec", [128, 512], F32), ("cmask", [128, 4, 128], F32),
            ("swab_prev", [128, 2, 8, 128], BF16), ("swab_cur", [128, 2, 8, 128], BF16), ("swab_first", [128, 2, 8, 128], BF16),
            ("blockones", [128, 128], BF16), ("sel", [64, 32, 128], BF16),
        ]:
            d[nm] = B.din(nm, shp, dt)
        self.out = B.dout("out", [TOK, D], F32)
        self.x1s = B.dscratch("x1s", [TOK, D], F32)
        self.psb = [B.ps("bank%d" % i) for i in range(8)]
        self.c = {}
        self.ncast = 0
        self.cast_engs = ("act", "dve")
        self.zbank = 0
        self.lc(["ident", "gmix", "gffn", "gpg"])
        self.mhalf = B.sb("mhalf", [128, 256], F32)
        B.memset("pool", self.mhalf[:], -0.5, [self.mhalf])
        self.h2T = B.sb("h2T", [128, 8, TOK], BF16)
        self.lg_all = B.sb("lg_all", [128, 16, 36], F32)
        mk0 = B.mark()
        self.grT = B.sb("grT", [128, 16, 8, 128], BF16)
        mk = B.mark()
        self.phaseA1()
        if "dumpA1" in self.dbg:
            m = self.m
            for nm, t, shp, dt in [("qT", m["qT"][1], [128, 4, 128], BF16), ("kT", m["kT"][1], [128, 4, 128], BF16),
                                   ("vtok", m["vtok"][1], [128, 512], BF16), ("scT", m["scT"], [128, 4, 128], BF16),
                                   ("yn", m["yn"], [128, 512], BF16), ("YrT", m["YrT"][1], [128, 4, 128], BF16),
                                   ("sgT", m["sgT"][1], [128, 4, 128], BF16), ("sigr", m["sigr"][1], [128, 8, 128], BF16),
                                   ("S", m["S"], [128, 512], F32), ("hT", m["hT"][1], [128, 1024], BF16),
                                   ("mean", m["mean"], [128, 4], F32), ("rstd", m["rstd"], [128, 4], F32),
                                   ("ktok", m["ktok"], [128, 512], BF16), ("t1", m["t1"], [128, 4, 128], BF16)]:
                B.dump(nm, t, t[:], shp, dt)
        if "stopA1" in self.dbg:
            B.dump("grT", self.grT, self.grT[:, 0:n_own], [128, n_own, 8, 128], BF16)
            B.S.emit()
            return
        B.S.barrier()
        B.release(mk)
        self.phaseA2()
        B.S.barrier()
        B.release(mk0)
        if do_moe:
            self.moe(n_groups)
        if do_ple:
            self.ple()
        B.S.emit()

    SHAPES = {"ident": ([128, 128], BF16), "qdec": ([128, 4, 128], F32), "kdec": ([128, 4, 128], F32),
              "adec": ([128, 512], F32), "cmask": ([128, 4, 128], F32), "swab_prev": ([128, 2, 8, 128], BF16),
              "swab_cur": ([128, 2, 8, 128], BF16), "swab_first": ([128, 2, 8, 128], BF16), "blockones": ([128, 128], BF16),
              "gmix": ([128, 8], F32), "gffn": ([128, 8], F32), "gpg": ([128, 8], F32), "gng": ([128, 4], F32),
              "gnb": ([128, 4], F32), "gqd": ([128, 1], F32), "gkd": ([128, 1], F32), "sel": ([64, 32, 128], BF16)}

    def lc(self, names):
        B, d, c = self.B, self.d, self.c
        for nm in names:
            shp, dt = self.SHAPES[nm]
            c[nm] = B.sb("c_" + nm, shp, dt)
            B.dma(c[nm][:], d[nm][:], [d[nm]], [c[nm]])

    def cast(self, out, in_, rd, wr, scale=None):
        B = self.B
        eng = self.cast_engs[self.ncast % len(self.cast_engs)]
        self.ncast += 1
        if scale is None:
            B.cp(eng, out, in_, rd, wr)
        elif eng == "act":
            B.act(out, in_, AF.Copy, rd, wr, scale=scale)
        else:
            B.ts(eng, out, in_, scale, None, ALU.mult, None, rd, wr)

    def rsqrt(self, dst, src, n, scale, rd_t, wr_t):
        B = self.B
        B.ts("pool", dst, src, scale, EPS, ALU.mult, ALU.add, rd_t, wr_t)
        for c0 in range(0, n, 256):
            c1 = min(n, c0 + 256)
            B.tt("pool", dst[:, c0:c1], dst[:, c0:c1], self.mhalf[:, 0:c1 - c0], ALU.pow, wr_t + [self.mhalf], wr_t)

    def new_stage(self, n=6, cols=1024):
        self.stage = [self.B.sb("stage%d" % i, [128, cols], F32) for i in range(n)]
        self.stage_cols = cols
        self.nstage = 0

    def next_stage(self):
        st = self.stage[self.nstage % len(self.stage)]
        self.nstage += 1
        return st

    def load_rows(self, dst, src_name, nk, scale_name=None, const_scale=None):
        B, d, c = self.B, self.d, self.c
        v = d[src_name][:].rearrange("(c p) n -> p c n", p=128)
        n = v.shape[2]
        per = max(1, self.stage_cols // n)
        for kc in range(0, nk, per):
            st = self.next_stage()
            k1 = min(nk, kc + per)
            stv = st[:, 0:(k1 - kc) * n].rearrange("p (c n) -> p c n", c=k1 - kc)
            B.dma(stv, v[:, kc:k1, :], [d[src_name]], [st])
            if scale_name is None and const_scale is not None:
                self.B.ts("dve", dst[:, kc:k1, :], stv, const_scale, None, ALU.mult, None, [st], [dst])
            elif scale_name is None:
                self.cast(dst[:, kc:k1, :], stv, [st], [dst])
            else:
                for k in range(kc, k1):
                    self.cast(dst[:, k, :], stv[:, k - kc, :], [st, c[scale_name]], [dst], scale=c[scale_name][:, k:k + 1])

    def load_win(self, dst, pieces):
        B, d, c = self.B, self.d, self.c
        win_v = d["w_in"][:].rearrange("(c p) n -> p c n", p=128)
        SC = self.stage_cols
        for kc in range(8):
            sc = c["gmix"][:, kc:kc + 1]
            for (c0, c1, dsts) in pieces:
                for p0 in range(c0, c1, SC):
                    p1 = min(c1, p0 + SC)
                    st = self.next_stage()
                    B.dma(st[:, 0:p1 - p0], win_v[:, kc, p0:p1], [d["w_in"]], [st])
                    for (dc0, r0, r1) in dsts:
                        a0, a1 = max(r0, p0 - c0), min(r1, p1 - c0)
                        if a0 >= a1:
                            continue
                        self.cast(dst[:, kc, dc0 + (a0 - r0):dc0 + (a1 - r0)], st[:, a0 - (p0 - c0):a1 - (p0 - c0)], [st, c["gmix"]], [dst], scale=sc)

    def common_bufs(self, nx=2):
        B, m = self.B, self.m
        sb = B.sb
        m["x"] = [sb("xr%d" % i, [128, D], F32) for i in range(nx)]
        m["junk"] = sb("junk", [128, D], BF16)
        m["ss"] = [sb("ss%d" % i, [128, 1], F32) for i in range(2)]
        m["rs"] = [sb("rs%d" % i, [128, 1], F32) for i in range(2)]
        m["hb"] = sb("hb", [128, D], BF16)
        m["hT"] = [sb("hT%d" % i, [128, D], BF16) for i in range(2)]

    def phaseA1(self):
        B, d, c = self.B, self.d, self.c
        sb = B.sb
        self.lc(["qdec", "kdec", "adec", "cmask", "gng", "gnb"])
        W = self.W = {}
        W["in"] = sb("w1_bf", [128, 8, 3072], BF16)
        W["ret_o"] = sb("w_ret_o_bf", [128, 4, D], BF16)
        mk_st = B.mark()
        self.new_stage()
        self.load_win(W["in"], [(0, 2048, [(0, 0, 2048)]), (C_GR, C_GR + 1024, [(2048, 0, 1024)])])
        self.load_rows(W["ret_o"], "w_ret_o", 4, const_scale=0.25)
        B.S.barrier()
        B.release(mk_st)
        m = self.m = {}
        self.common_bufs()
        R2 = lambda nm, shp, dt, n=2: [sb("%s%d" % (nm, i), shp, dt) for i in range(n)]
        m["qT"] = R2("qT", [128, 4, 128], BF16)
        m["kT"] = R2("kT", [128, 4, 128], BF16)
        m["sgT"] = R2("sgT", [128, 4, 128], BF16, 3)
        m["th"] = sb("th", [128, 4, 128], BF16)
        m["sigr"] = R2("sigr", [128, 8, 128], BF16, 4)
        m["vtok"] = R2("vtok", [128, 512], BF16)
        m["ktok"] = sb("ktok", [128, 512], BF16)
        m["S"] = sb("S", [128, 512], F32)
        m["Shat"] = sb("Shat", [128, 512], BF16)
        m["scT"] = sb("scT", [128, 4, 128], BF16)
        m["ysq"] = sb("ysq", [128, 512], F32)
        for nm in ("s1", "s2", "mean", "msq", "var", "rstd"):
            m[nm] = sb("gn_" + nm, [128, 4], F32)
        m["yn"] = sb("yn", [128, 512], BF16)
        m["t1"] = sb("t1", [128, 4, 128], BF16)
        m["YrT"] = R2("YrT", [128, 4, 128], BF16)
        B.memset("pool", m["S"][:], 0.0, [m["S"]])
        tiles = [("prev", 16 - self.n_prev + i) for i in range(self.n_prev)] + [("own", i) for i in range(self.n_own)]
        self.first_state = True

        def front_n(u):
            if u < len(tiles):
                yield from self.gen_N(u, tiles[u])

        def front_z(u):
            if u < len(tiles):
                yield from self.gen_Z1(u, tiles[u])

        def mid(u):
            if 0 <= u < len(tiles):
                yield from self.gen_R(u, tiles[u])

        def mid2(u):
            if 0 <= u < len(tiles) and tiles[u][0] == "own":
                yield from self.gen_R2(u, tiles[u])

        def back(u):
            if 0 <= u < len(tiles) and tiles[u][0] == "own":
                yield from self.gen_O1(u, tiles[u])

        self.cur_tiles = tiles
        self.load_x(0)
        interleave(front_n(0))
        interleave(front_n(1), front_z(0))
        for u in range(len(tiles) + 2):
            interleave(front_n(u + 2), front_z(u + 1), mid(u), mid2(u - 1), back(u - 2))

    def load_x(self, u):
        tiles = self.cur_tiles
        if (u in tiles) if isinstance(tiles, dict) else (0 <= u < len(tiles)):
            kind, ti = tiles[u]
            src = self.d["xprev"] if kind == "prev" else self.d["x"]
            xt = self.m["x"][u % len(self.m["x"])]
            self.B.dma(xt[:], src[ti * 128:(ti + 1) * 128, :], [src], [xt])

    def gen_N(self, u, tile):
        B, d, c, m = self.B, self.d, self.c, self.m
        kind, ti = tile
        xt = m["x"][u % len(m["x"])]
        ss, rs, hb, hT = m["ss"][u % 2], m["rs"][u % 2], m["hb"], m["hT"][u % 2]
        self.load_x(u + 1)
        B.act(m["junk"][:], xt[:], AF.Square, [xt, ss], [m["junk"], ss], accum_out=ss[:])
        self.rsqrt(rs[:], ss[:], 1, 1.0 / D, [ss], [rs])
        if getattr(self, "n_cast_eng", "act") == "dve":
            B.ts("dve", hb[:], xt[:], rs[:, 0:1], None, ALU.mult, None, [xt, rs], [hb])
        else:
            B.act(hb[:], xt[:], AF.Copy, [xt, rs], [hb], scale=rs[:, 0:1])
        yield
        pb = self.psb[2]
        pbb = pb[:].bitcast(BF16)
        for cc in range(8):
            B.tr(pbb[:, cc * 128:(cc + 1) * 128], hb[:, cc * 128:(cc + 1) * 128], c["ident"][:], [hb, c["ident"]], [pb])
        B.cp("dve", hT[:], pbb[:, 0:D], [pb], [hT])
        yield

    def zgroup(self, u, c0, nchunk):
        B, W, m = self.B, self.W, self.m
        hT = m["hT"][u % 2]
        pb = self.psb[self.zbank % 2]
        self.zbank += 1
        pv = pb[:].rearrange("p (a q) -> p a q", a=4)
        for a in range(nchunk):
            for kc in range(8):
                B.mm(pv[:, a, :], W["in"][:, kc, c0 + a * 128:c0 + (a + 1) * 128], hT[:, kc * 128:(kc + 1) * 128],
                     kc == 0, kc == 7, [W["in"], hT], [pb])
        return pb, pv

    def ztok(self, u, c0, n):
        B, W, m = self.B, self.W, self.m
        hT = m["hT"][u % 2]
        pb = self.psb[self.zbank % 2]
        self.zbank += 1
        for kc in range(8):
            B.mm(pb[:, 0:n], hT[:, kc * 128:(kc + 1) * 128], W["in"][:, kc, c0:c0 + n], kc == 0, kc == 7, [hT, W["in"]], [pb])
        return pb

    def gen_Z1(self, u, tile):
        B, d, c, m, W = self.B, self.d, self.c, self.m, self.W
        kind, ti = tile
        own = kind == "own"
        s = u % 2
        pb, pv = self.zgroup(u, 512, 4)
        B.tt("dve", m["kT"][s][:], pv, c["kdec"][:], ALU.mult, [pb, c["kdec"]], [m["kT"][s]])
        yield
        pb = self.ztok(u, 1024, 512)
        B.cp("act", m["vtok"][s][:], pb[:], [pb], [m["vtok"][s]])
        yield
        if not own:
            return
        pb, pv = self.zgroup(u, 0, 4)
        B.tt("dve", m["qT"][s][:], pv, c["qdec"][:], ALU.mult, [pb, c["qdec"]], [m["qT"][s]])
        yield
        pb, pv = self.zgroup(u, 1536, 4)
        B.act(m["th"][:], pv, AF.Tanh, [pb], [m["th"]], scale=0.5)
        B.stt("dve", m["sgT"][u % 3][:], m["th"][:], 1.0, pv, ALU.add, ALU.mult, [pb, m["th"]], [m["sgT"][u % 3]])
        yield
        for g2 in range(2):
            pb, pv = self.zgroup(u, 2048 + g2 * 512, 4)
            B.act(m["sigr"][u % 4][:, g2 * 4:(g2 + 1) * 4, :], pv, AF.Tanh, [pb], [m["sigr"][u % 4]], scale=0.5)
            yield

    def gen_R(self, u, tile):
        B, d, c, m, W = self.B, self.d, self.c, self.m, self.W
        kind, ti = tile
        own = kind == "own"
        s = u % 2
        kT, qT, vtok, ktok = m["kT"][s], m["qT"][s], m["vtok"][s], m["ktok"]
        S, Shat = m["S"], m["Shat"]
        if not self.first_state:
            B.tt("dve", S[:], S[:], c["adec"][:], ALU.mult, [S, c["adec"]], [S])
        self.first_state = False
        if own:
            B.cp("act", Shat[:], S[:], [S], [Shat])
        p3 = self.psb[3]
        p3b = p3[:].bitcast(BF16)
        for h in range(4):
            B.tr(p3b[:, h * 128:(h + 1) * 128], kT[:, h, :], c["ident"][:], [kT, c["ident"]], [p3])
        B.cp("act", ktok[:], p3b[:, 0:512], [p3], [ktok])
        yield
        p4 = self.psb[4]
        if own:
            p4v = p4[:].rearrange("p (a q) -> p a q", a=4)
            for h in range(4):
                B.mm(p4v[:, h, :], kT[:, h, :], qT[:, h, :], True, True, [kT, qT], [p4])
            B.tt("dve", m["scT"][:], p4v, c["cmask"][:], ALU.mult, [p4, c["cmask"]], [m["scT"]])
            yield
            p5 = self.psb[5 + s]
            for h in range(4):
                B.mm(p5[:, h * 128:(h + 1) * 128], m["scT"][:, h, :], vtok[:, h * 128:(h + 1) * 128], True, False, [m["scT"], vtok], [p5])
                B.mm(p5[:, h * 128:(h + 1) * 128], qT[:, h, :], Shat[:, h * 128:(h + 1) * 128], False, True, [qT, Shat], [p5])
            yield
        for h in range(4):
            B.mm(p4[:, h * 128:(h + 1) * 128], ktok[:, h * 128:(h + 1) * 128], vtok[:, h * 128:(h + 1) * 128], True, True, [ktok, vtok], [p4])
        B.tt("dve", S[:], S[:], p4[:], ALU.add, [S, p4], [S])
        yield

    def gen_R2(self, u, tile):
        B, d, c, m, W = self.B, self.d, self.c, self.m, self.W
        kind, ti = tile
        s = u % 2
        p5 = self.psb[5 + s]
        p5v = p5[:].rearrange("p (a q) -> p a q", a=4)
        B.act(m["ysq"][:], p5[:], AF.Square, [p5], [m["ysq"]])
        B.S.add("dve", lambda e: e.tensor_reduce(m["s1"][:], p5v, AX.X, ALU.add), [p5.b], [m["s1"].b, p5.b])
        B.S.add("dve", lambda e: e.tensor_reduce(m["s2"][:], m["ysq"][:].rearrange("p (a q) -> p a q", a=4), AX.X, ALU.add), [m["ysq"].b], [m["s2"].b])
        B.ts("dve", m["mean"][:], m["s1"][:], 1.0 / 128, None, ALU.mult, None, [m["s1"]], [m["mean"]])
        B.tt("dve", m["msq"][:], m["mean"][:], m["mean"][:], ALU.mult, [m["mean"]], [m["msq"]])
        B.stt("dve", m["var"][:], m["s2"][:], 1.0 / 128, m["msq"][:], ALU.mult, ALU.subtract, [m["s2"], m["msq"]], [m["var"]])
        self.rsqrt(m["rstd"][:], m["var"][:], 4, 1.0, [m["var"]], [m["rstd"]])
        yield
        ynv = m["yn"][:].rearrange("p (a q) -> p a q", a=4)
        B.tt("dve", m["ysq"][:].rearrange("p (a q) -> p a q", a=4), p5v, m["mean"][:].unsqueeze(2).to_broadcast([128, 4, 128]), ALU.subtract,
             [p5, m["mean"]], [m["ysq"]])
        B.tt("dve", ynv, m["ysq"][:].rearrange("p (a q) -> p a q", a=4), m["rstd"][:].unsqueeze(2).to_broadcast([128, 4, 128]), ALU.mult,
             [m["ysq"], m["rstd"]], [m["yn"]])
        yield
        p3 = self.psb[3]
        p3b = p3[:].bitcast(BF16)
        for h in range(4):
            B.tr(p3b[:, h * 128:(h + 1) * 128], m["yn"][:, h * 128:(h + 1) * 128], c["ident"][:], [m["yn"], c["ident"]], [p3])
        for h in range(4):
            B.act(m["t1"][:, h, :], p3b[:, h * 128:(h + 1) * 128], AF.Identity, [p3, c["gng"], c["gnb"]], [m["t1"]],
                  bias=c["gnb"][:, h:h + 1], scale=c["gng"][:, h:h + 1])
        yield
        sgT = m["sgT"][u % 3]
        B.tt("pool", m["YrT"][s][:], m["t1"][:], sgT[:], ALU.mult, [m["t1"], sgT], [m["YrT"][s]])
        yield

    def gen_O1(self, u, tile):
        B, d, c, m, W = self.B, self.d, self.c, self.m, self.W
        kind, ti = tile
        s = u % 2
        sigr = m["sigr"][u % 4]
        YrT = m["YrT"][s]
        for g2 in range(2):
            pb = self.psb[7]
            pv = pb[:].rearrange("p (a q) -> p a q", a=4)
            for a in range(4):
                oc = g2 * 4 + a
                for kc in range(4):
                    B.mm(pv[:, a, :], W["ret_o"][:, kc, oc * 128:(oc + 1) * 128], YrT[:, kc, :], kc == 0, kc == 3, [W["ret_o"], YrT], [pb])
            B.stt("dve", self.grT[:, ti, g2 * 4:(g2 + 1) * 4, :], sigr[:, g2 * 4:(g2 + 1) * 4, :], 1.0, pv, ALU.add, ALU.mult, [pb, sigr], [self.grT])
            yield

    def phaseA2(self):
        B, d, c = self.B, self.d, self.c
        sb = B.sb
        self.lc(["swab_prev", "swab_cur", "swab_first", "blockones", "gqd", "gkd"])
        c["esink"] = sb("c_esink", [128, 8], F32)
        B.dma(c["esink"][:], d["sinks"][:].partition_broadcast(128), [d["sinks"]], [c["esink"]])
        B.act(c["esink"][:], c["esink"][:], AF.Exp, [c["esink"]], [c["esink"]])
        c["gqk"] = sb("c_gqk", [128, 1], F32)
        B.stt("dve", c["gqk"][:], c["gqd"][:], 0.125, c["gkd"][:], ALU.mult, ALU.mult, [c["gqd"], c["gkd"]], [c["gqk"]])
        W = self.W = {}
        W["in"] = sb("w2_bf", [128, 8, 1920], BF16)
        W["swa_o"] = sb("w_swa_o_bf", [128, 4, D], BF16)
        W["out"] = sb("w_out_bf", [128, 8, D], BF16)
        W["rt"] = sb("wrt_bf", [128, 8, 36], BF16)
        W["rt_lo"] = sb("wrt_lo_bf", [128, 8, 36], BF16)
        wrt_f = sb("wrt_f", [128, 8, 36], F32)
        mk_st = B.mark()
        self.new_stage()
        self.load_win(W["in"], [
            (C_SQ, C_SV + 128, [(0, 0, 512), (512, 512, 576), (576, 512, 576), (640, 576, 640), (704, 576, 640), (768, 640, 768)]),
            (C_GS, C_GS + 1024, [(896, 0, 1024)])])
        self.load_rows(W["swa_o"], "w_swa_o", 4, const_scale=0.5)
        self.load_rows(W["out"], "w_out", 8)
        st = self.next_stage()
        stv = st[:, 0:288].rearrange("p (c n) -> p c n", c=8)
        B.dma(stv, d["w_rt"][:].rearrange("(c p) n -> p c n", p=128), [d["w_rt"]], [st])
        B.cp("dve", W["rt"][:], stv, [st], [W["rt"]])
        B.cp("dve", wrt_f[:], W["rt"][:], [W["rt"]], [wrt_f])
        B.tt("dve", W["rt_lo"][:], stv, wrt_f[:], ALU.subtract, [st, wrt_f], [W["rt_lo"]])
        B.S.barrier()
        B.release(mk_st)
        c["gffn_row"] = sb("gffn_row", [128, D], F32)
        B.dma(c["gffn_row"][:], d["gffn_row"][:].partition_broadcast(128), [d["gffn_row"]], [c["gffn_row"]])
        c["brt"] = sb("brt_b", [128, 36], F32)
        B.dma(c["brt"][:], d["brt"][:].partition_broadcast(128), [d["brt"]], [c["brt"]])
        m = self.m = {}
        self.common_bufs(2)
        m["hb2"] = sb("hb2", [128, D], BF16)
        m["hf2"] = sb("hf2", [128, D], F32)
        m["hl2"] = sb("hl2", [128, D], BF16)
        m["loT"] = sb("loT", [128, 8, 128], BF16)
        m["ss2"] = sb("ss2", [128, 1], F32)
        m["rs2"] = sb("rs2", [128, 1], F32)
        R2 = lambda nm, shp, dt, n=2: [sb("%s%d" % (nm, i), shp, dt) for i in range(n)]
        m["sigs"] = R2("sigs", [128, 8, 128], BF16, 3)
        m["sqT"] = R2("sqT", [128, 4, 128], BF16)
        m["skT"] = R2("skT", [128, 2, 128], BF16, 3)
        m["vaug"] = R2("vaug", [128, 2, 65], BF16, 3)
        m["sqf"] = sb("sqf", [128, 512], F32)
        m["sq2"] = sb("sq2", [128, 512], BF16)
        m["rq"] = sb("rq", [128, 512], F32)
        m["skf"] = sb("skf", [128, 256], F32)
        m["sk2"] = sb("sk2", [128, 256], BF16)
        m["rk"] = sb("rk", [128, 256], F32)
        m["pT"] = R2("pT", [128, 4, 128], BF16, 4)
        m["den"] = sb("den", [128, 8], F32)
        m["rden"] = sb("rden", [128, 8], F32)
        m["ystok"] = sb("ystok", [128, 512], BF16)
        m["ysT"] = R2("ysT", [128, 4, 128], BF16)
        m["gs"] = R2("gs", [128, 4, 128], BF16)
        m["mrg"] = sb("mrg", [128, 8, 128], BF16)
        m["x1t"] = R2("x1t", [128, D], F32, 2)
        for i in range(3):
            B.memset("pool", m["vaug"][i][:], 1.0, [m["vaug"][i]])
        tiles = {15: ("prev", 15)}
        for i in range(self.n_own):
            tiles[16 + i] = ("own", i)
        us = sorted(tiles)

        def front_n(u):
            if u in tiles:
                yield from self.gen_N(u, tiles[u])

        def front_z(u):
            if u in tiles:
                yield from self.gen_Z2(u, tiles[u])

        def mid(u):
            if u in tiles and tiles[u][0] == "own":
                yield from self.gen_W(u, tiles[u])

        def back(u):
            if u in tiles and tiles[u][0] == "own":
                yield from self.gen_O2(u, tiles[u])

        def back2(u):
            if u in tiles and tiles[u][0] == "own":
                yield from self.gen_O2b(u, tiles[u])

        self.cur_tiles = tiles
        self.n_cast_eng = "dve"
        self.load_x(us[0])
        interleave(front_n(us[0]))
        interleave(front_n(us[0] + 1), front_z(us[0]))
        for u in range(us[0], us[-1] + 3):
            interleave(front_n(u + 2), front_z(u + 1), mid(u), back(u - 1), back2(u - 2))

    def gen_Z2(self, u, tile):
        B, d, c, m, W = self.B, self.d, self.c, self.m, self.W
        kind, ti = tile
        own = kind == "own"
        s = u % 2
        pb, pv = self.zgroup(u, 512, 2)
        B.cp("dve", m["skf"][:], pb[:, 0:256], [pb], [m["skf"]])
        B.act(m["sk2"][:], pb[:, 0:256], AF.Square, [pb], [m["sk2"]])
        p7 = self.psb[2]
        B.mm(p7[:, 0:256], c["blockones"][:], m["sk2"][:], True, True, [c["blockones"], m["sk2"]], [p7])
        B.act(m["rk"][:], p7[:, 0:256], AF.Sqrt, [p7], [m["rk"]], bias=EPS, scale=1.0)
        B.S.add("dve", lambda e: e.reciprocal(m["rk"][:], m["rk"][:]), [m["rk"].b], [m["rk"].b])
        B.stt("dve", m["skT"][u % 3][:], m["skf"][:].rearrange("p (a q) -> p a q", a=2), c["gqk"][:, 0:1],
              m["rk"][:].rearrange("p (a q) -> p a q", a=2), ALU.mult, ALU.mult, [m["skf"], m["rk"], c["gqk"]], [m["skT"][u % 3]])
        yield
        pb = self.ztok(u, 768, 128)
        B.cp("dve", m["vaug"][u % 3][:, :, 0:64], pb[:, 0:128].rearrange("p (a q) -> p a q", a=2), [pb], [m["vaug"][u % 3]])
        yield
        if not own:
            return
        pb, pv = self.zgroup(u, 0, 4)
        B.cp("dve", m["sqf"][:], pb[:], [pb], [m["sqf"]])
        B.act(m["sq2"][:], pb[:], AF.Square, [pb], [m["sq2"]])
        p7 = self.psb[2]
        B.mm(p7[:], c["blockones"][:], m["sq2"][:], True, True, [c["blockones"], m["sq2"]], [p7])
        B.act(m["rq"][:], p7[:], AF.Sqrt, [p7], [m["rq"]], bias=EPS, scale=1.0)
        B.S.add("dve", lambda e: e.reciprocal(m["rq"][:], m["rq"][:]), [m["rq"].b], [m["rq"].b])
        B.tt("pool", m["sqT"][s][:], m["sqf"][:].rearrange("p (a q) -> p a q", a=4),
             m["rq"][:].rearrange("p (a q) -> p a q", a=4), ALU.mult, [m["sqf"], m["rq"]], [m["sqT"][s]])
        yield
        for g2 in range(2):
            pb, pv = self.zgroup(u, 896 + g2 * 512, 4)
            B.act(m["sigs"][u % 3][:, g2 * 4:(g2 + 1) * 4, :], pv, AF.Tanh, [pb], [m["sigs"][u % 3]], scale=0.5)
            yield

    def gen_W(self, u, tile):
        B, d, c, m, W = self.B, self.d, self.c, self.m, self.W
        kind, ti = tile
        s = u % 2
        sp_ = (u - 1) % 2
        sqT = m["sqT"][s]
        first = (ti == 0)
        k = 0
        pvb = [self.psb[6], self.psb[6]]
        for kh in range(2):
            pts = []
            for kt in range(2):
                slot = (u - 1) % 3 if kt == 0 else u % 3
                skT = m["skT"][slot]
                bias = c["swab_cur"] if kt == 1 else (c["swab_first"] if first else c["swab_prev"])
                pT = m["pT"][kh * 2 + kt]
                pT4 = pT[:].rearrange("p (a b) q -> p a b q", a=2)
                for par in range(2):
                    po = par * 64
                    pb = self.psb[4 + par]
                    pv2 = pb[:, 0:256].rearrange("p (a q) -> p a q", a=2)
                    B.mm(pv2, skT[po:po + 64, kh, :], sqT[po:po + 64, kh * 2:kh * 2 + 2, :], True, False, [skT, sqT], [pb])
                    for hl_ in range(2):
                        b4 = bias[:, hl_, kh * 4:(kh + 1) * 4, :].rearrange("p (a b) q -> p a b q", a=2)
                        B.mm(pv2, c["ident"][:], b4[:, :, par, :], False, hl_ == 1, [c["ident"], bias], [pb])
                    B.act(pT4[:, :, par, :], pv2, AF.Exp, [pb], [pT])
                pts.append((pT, m["vaug"][slot]))
                yield
            po_ = pvb[kh]
            pov = po_[:, 0:260].rearrange("p (a q) -> p a q", a=4)
            for g in range(4):
                for kt in range(2):
                    pT, va = pts[kt]
                    B.mm(pov[:, g, :], pT[:, g, :], va[:, kh, :], kt == 0, kt == 1, [pT, va], [po_])
            B.tt("dve", m["den"][:, kh * 4:(kh + 1) * 4], pov[:, :, 64], c["esink"][:, kh * 4:(kh + 1) * 4], ALU.add, [po_, c["esink"]], [m["den"]])
            B.S.add("dve", lambda e, kh=kh: e.reciprocal(m["rden"][:, kh * 4:(kh + 1) * 4], m["den"][:, kh * 4:(kh + 1) * 4]), [m["den"].b], [m["rden"].b])
            B.tt("dve", m["ystok"][:, kh * 256:(kh + 1) * 256].rearrange("p (a q) -> p a q", a=4), pov[:, :, 0:64],
                 m["rden"][:, kh * 4:(kh + 1) * 4].unsqueeze(2).to_broadcast([128, 4, 64]), ALU.mult, [po_, m["rden"]], [m["ystok"]])
            yield
        p3 = self.psb[3]
        p3b = p3[:].bitcast(BF16)
        for h in range(4):
            B.tr(p3b[:, h * 128:(h + 1) * 128], m["ystok"][:, h * 128:(h + 1) * 128], c["ident"][:], [m["ystok"], c["ident"]], [p3])
        B.cp("dve", m["ysT"][s][:], p3b[:, 0:512].rearrange("p (a q) -> p a q", a=4), [p3], [m["ysT"][s]])
        yield

    def gen_O2(self, u, tile):
        B, d, c, m, W = self.B, self.d, self.c, self.m, self.W
        kind, ti = tile
        s = u % 2
        sigs = m["sigs"][u % 3]
        ysT = m["ysT"][s]
        x1t = m["x1t"][s]
        B.dma(x1t[:], d["x"][ti * 128:(ti + 1) * 128, :], [d["x"]], [x1t])
        for g2 in range(2):
            pb = self.psb[7]
            pv = pb[:].rearrange("p (a q) -> p a q", a=4)
            for a in range(4):
                oc = g2 * 4 + a
                for kc in range(4):
                    B.mm(pv[:, a, :], W["swa_o"][:, kc, oc * 128:(oc + 1) * 128], ysT[:, kc, :], kc == 0, kc == 3, [W["swa_o"], ysT], [pb])
            B.stt("dve", m["gs"][g2][:], sigs[:, g2 * 4:(g2 + 1) * 4, :], 1.0, pv, ALU.add, ALU.mult, [pb, sigs], [m["gs"][g2]])
            B.tt("pool", m["mrg"][:, g2 * 4:(g2 + 1) * 4, :], self.grT[:, ti, g2 * 4:(g2 + 1) * 4, :], m["gs"][g2][:], ALU.add,
                 [self.grT, m["gs"][g2]], [m["mrg"]])
            yield
        for half in range(2):
            pb = self.psb[7]
            for kc in range(8):
                B.mm(pb[:], m["mrg"][:, kc, :], W["out"][:, kc, half * 512:(half + 1) * 512], kc == 0, kc == 7, [m["mrg"], W["out"]], [pb])
            B.tt("dve", x1t[:, half * 512:(half + 1) * 512], pb[:], x1t[:, half * 512:(half + 1) * 512], ALU.add, [pb, x1t], [x1t])
            yield
        if "x1" in self.dbg:
            op = B.dma(self.out[ti * 128:(ti + 1) * 128, :], x1t[:], [x1t], [self.out])
            B.S.final_waits.append(op)
        else:
            B.dma(self.x1s[ti * 128:(ti + 1) * 128, :], x1t[:], [x1t], [self.x1s])
        yield

    def gen_O2b(self, u, tile):
        B, d, c, m, W = self.B, self.d, self.c, self.m, self.W
        kind, ti = tile
        x1t = m["x1t"][u % 2]
        ss, rs, hb = m["ss2"], m["rs2"], m["hb2"]
        B.act(m["junk"][:], x1t[:], AF.Square, [x1t, ss], [m["junk"], ss], accum_out=ss[:])
        self.rsqrt(rs[:], ss[:], 1, 1.0 / D, [ss], [rs])
        hf, hl, loT = m["hf2"], m["hl2"], m["loT"]
        B.stt("dve", hf[:], x1t[:], rs[:, 0:1], c["gffn_row"][:], ALU.mult, ALU.mult, [x1t, rs, c["gffn_row"]], [hf])
        B.cp("act", hb[:], hf[:], [hf], [hb])
        B.tt("pool", hl[:], hf[:], hb[:], ALU.subtract, [hf, hb], [hl])
        yield
        p3 = self.psb[3]
        p3b = p3[:].bitcast(BF16)
        for cc in range(8):
            B.tr(p3b[:, cc * 128:(cc + 1) * 128], hb[:, cc * 128:(cc + 1) * 128], c["ident"][:], [hb, c["ident"]], [p3])
        h2v = self.h2T[:, :, ti * 128:(ti + 1) * 128]
        B.cp("act", h2v, p3b[:, 0:D].rearrange("p (a q) -> p a q", a=8), [p3], [self.h2T])
        yield
        for cc in range(8):
            B.tr(p3b[:, cc * 128:(cc + 1) * 128], hl[:, cc * 128:(cc + 1) * 128], c["ident"][:], [hl, c["ident"]], [p3])
        B.cp("dve", loT[:], p3b[:, 0:D].rearrange("p (a q) -> p a q", a=8), [p3], [loT])
        yield
        p6 = self.psb[3]
        k = 0
        for (lhs_t, lhs_ap, wt) in [(self.h2T, None, W["rt"]), (loT, loT, W["rt"]), (self.h2T, None, W["rt_lo"])]:
            for kc in range(8):
                lhsT = self.h2T[:, kc, ti * 128:(ti + 1) * 128] if lhs_ap is None else loT[:, kc, :]
                B.mm(p6[:, 0:36], lhsT, wt[:, kc, :], k == 0, k == 23, [lhs_t, wt], [p6])
                k += 1
        B.tt("dve", self.lg_all[:, ti, :], p6[:, 0:36], c["brt"][:], ALU.add, [p6, c["brt"]], [self.lg_all])
        yield

    def moe(self, n_groups):
        B, d, c = self.B, self.d, self.c
        sb = B.sb
        m = self.m = {}
        self.acc = sb("acc", [128, 16, D], F32)
        self.mk_after_acc = B.mark()
        self.lc(["sel"])
        self.new_stage(3, 2048)
        h2T = self.h2T
        NTL = self.n_own
        cT = sb("cT", [64, TOK], BF16)
        F = lambda nm, shp: sb("r_" + nm, shp, F32)
        gmax, gd, ge, gsum, gw = F("gmax", [128, 16]), F("gd", [128, 16, 4]), F("ge", [128, 16, 4]), F("gsum", [128, 16]), F("gw", [128, 16])
        oh, pen = F("oh", [128, 16, 4]), F("pen", [128, 16, 4])
        elm, m1, mask1 = F("elm", [128, 16, 32]), F("m1", [128, 16]), F("mask1", [128, 16, 32])
        elm2, m2, mask2 = F("elm2", [128, 16, 32]), F("m2", [128, 16]), F("mask2", [128, 16, 32])
        dm, ed, w1, w2 = F("dm", [128, 16]), F("ed", [128, 16]), F("w1", [128, 16]), F("w2", [128, 16])
        c1, cc, chf, clo = mask1, mask2, elm, elm2
        chl = sb("chl", [128, 16, 64], BF16)
        lg = self.lg_all
        L = lambda fn, rd, wr: B.S.add("dve", fn, [t.b for t in rd], [t.b for t in wr])
        bc = lambda t, shp: t[:].unsqueeze(2).to_broadcast(shp)
        L(lambda e: e.tensor_reduce(gmax[:], lg[:, :, 0:4], AX.X, ALU.max), [lg], [gmax])
        B.tt("dve", gd[:], lg[:, :, 0:4], bc(gmax, [128, 16, 4]), ALU.subtract, [lg, gmax], [gd])
        B.act(ge[:], gd[:], AF.Exp, [gd], [ge])
        L(lambda e: e.tensor_reduce(gsum[:], ge[:], AX.X, ALU.add), [ge], [gsum])
        L(lambda e: e.reciprocal(gw[:], gsum[:]), [gsum], [gw])
        B.tt("dve", oh[:], lg[:, :, 0:4], bc(gmax, [128, 16, 4]), ALU.is_equal, [lg, gmax], [oh])
        B.ts("dve", pen[:], oh[:], 1e30, -1e30, ALU.mult, ALU.add, [oh], [pen])
        B.tt("dve", elm[:].rearrange("p t (a q) -> p t a q", a=4), lg[:, :, 4:36].rearrange("p t (a q) -> p t a q", a=4),
             pen[:].unsqueeze(3).to_broadcast([128, 16, 4, 8]), ALU.add, [lg, pen], [elm])
        L(lambda e: e.tensor_reduce(m1[:], elm[:], AX.X, ALU.max), [elm], [m1])
        B.tt("dve", mask1[:], elm[:], bc(m1, [128, 16, 32]), ALU.is_equal, [elm, m1], [mask1])
        B.stt("dve", elm2[:], mask1[:], -1e30, elm[:], ALU.mult, ALU.add, [mask1, elm], [elm2])
        L(lambda e: e.tensor_reduce(m2[:], elm2[:], AX.X, ALU.max), [elm2], [m2])
        B.tt("dve", mask2[:], elm2[:], bc(m2, [128, 16, 32]), ALU.is_equal, [elm2, m2], [mask2])
        B.tt("dve", dm[:], m2[:], m1[:], ALU.subtract, [m2, m1], [dm])
        B.act(ed[:], dm[:], AF.Exp, [dm], [ed])
        B.ts("dve", w1[:], ed[:], 1.0, None, ALU.add, None, [ed], [w1])
        L(lambda e: e.reciprocal(w1[:], w1[:]), [w1], [w1])
        B.tt("dve", w1[:], w1[:], gw[:], ALU.mult, [w1, gw], [w1])
        B.tt("dve", w2[:], w1[:], ed[:], ALU.mult, [w1, ed], [w2])
        B.tt("dve", c1[:], mask1[:], bc(w1, [128, 16, 32]), ALU.mult, [mask1, w1], [c1])
        B.tt("dve", cc[:], mask2[:], bc(w2, [128, 16, 32]), ALU.mult, [mask2, w2], [cc])
        B.tt("dve", cc[:], cc[:], c1[:], ALU.add, [cc, c1], [cc])
        B.cp("dve", chl[:, :, 0:32], cc[:], [cc], [chl])
        B.cp("dve", chf[:], chl[:, :, 0:32], [chl], [chf])
        B.tt("dve", clo[:], cc[:], chf[:], ALU.subtract, [cc, chf], [clo])
        B.cp("dve", chl[:, :, 32:64], clo[:], [clo], [chl])
        for hf in range(2):
            p5 = self.psb[5 + hf]
            p5b = p5[:].bitcast(BF16)
            nt = min(8, NTL - hf * 8)
            if nt <= 0:
                break
            for k in range(nt):
                B.tr(p5b[0:64, k * 128:(k + 1) * 128], chl[:, hf * 8 + k, :], c["ident"][:], [chl, c["ident"]], [p5])
            B.cp("act", cT[:, hf * 1024:hf * 1024 + nt * 128], p5b[0:64, 0:nt * 128], [p5], [cT])
        if "comb" in self.dbg:
            B.dump("cT", cT, cT[:], [64, TOK], BF16)
        NG = n_groups
        slots = [[{"g": sb("wg%d%d" % (i, j), [128, 8, 256], BF16), "u": sb("wu%d%d" % (i, j), [128, 8, 256], BF16),
                   "d": sb("wd%d%d" % (i, j), [128, 2, D], BF16)} for j in range(2)] for i in range(2)]
        sg = [sb("sg%d" % i, [128, 512], BF16) for i in range(2)]
        tu = [sb("tu%d" % i, [128, 512], BF16) for i in range(2)]
        hid = [[[sb("hid%d%d%d" % (i, j, k), [128, 512], BF16) for k in range(2)] for j in range(2)] for i in range(2)]

        def gen_load(gi):
            work = []
            for j in range(2):
                e = gi * 2 + j
                sl = slots[gi % 2][j]
                work.append((sl["g"], d["w_gate"], e, 8))
                work.append((sl["u"], d["w_up"], e, 8))
                work.append((sl["d"], d["w_down"], e, 2))

            def issue(k):
                dst, src, e, cdim = work[k]
                st = self.next_stage()
                stv = st[:].rearrange("p (c n) -> p c n", c=cdim)
                B.dma(stv, src[e].rearrange("(c p) n -> p c n", p=128), [src], [st])
                return st, stv

            q = [issue(k) for k in range(min(3, len(work)))]
            for k in range(len(work)):
                st, stv = q[k]
                self.cast(work[k][0][:], stv, [st], [work[k][0]])
                if k + 3 < len(work):
                    q.append(issue(k + 3))
                yield

        def load_group(gi):
            interleave(gen_load(gi))

        self.cast_engs = ("act",)
        load_group(0)
        for t in range(NTL):
            B.dma(self.acc[:, t, :], self.x1s[t * 128:(t + 1) * 128, :], [self.x1s], [self.acc])
        cnt = {"nb": 0, "nd": 0, "npc": 0}
        nchunk = self.n_own // 4

        def gen_gu(gi, ch):
            cs = slice(ch * 512, (ch + 1) * 512)
            hs = hid[ch % 2]
            for j in range(2):
                e = gi * 2 + j
                sl = slots[gi % 2][j]
                pc = self.psb[4 + 3 * (cnt["npc"] % 2)]
                cnt["npc"] += 1
                for fc in range(2):
                    nb = cnt["nb"]
                    pa, pu = self.psb[nb % 2], self.psb[2 + nb % 2]
                    cnt["nb"] += 1
                    nb += 1
                    for kc in range(8):
                        B.mm(pa[:], sl["g"][:, kc, fc * 128:(fc + 1) * 128], h2T[:, kc, cs], kc == 0, kc == 7, [sl["g"], h2T], [pa])
                    B.act(sg[nb % 2][:], pa[:], AF.Silu, [pa], [sg[nb % 2]])
                    yield
                    for kc in range(8):
                        B.mm(pu[:], sl["u"][:, kc, fc * 128:(fc + 1) * 128], h2T[:, kc, cs], kc == 0, kc == 7, [sl["u"], h2T], [pu])
                    if fc == 0:
                        B.mm(pc[:], c["sel"][:, e, :], cT[:, cs], True, True, [c["sel"], cT], [pc])
                    B.tt("dve", tu[nb % 2][:], pu[:], sg[nb % 2][:], ALU.mult, [pu, sg[nb % 2]], [tu[nb % 2]])
                    B.tt("dve", hs[j][fc][:], pc[:], tu[nb % 2][:], ALU.mult, [pc, tu[nb % 2]], [hs[j][fc]])
                    yield

        def gen_down(gi, ch):
            hs = hid[ch % 2]
            for tl in range(4):
                t = ch * 4 + tl
                for half in range(2):
                    pd = self.psb[5 + cnt["nd"] % 2]
                    cnt["nd"] += 1
                    k = 0
                    for j in range(2):
                        sl = slots[gi % 2][j]
                        for fc in range(2):
                            B.mm(pd[:], hs[j][fc][:, tl * 128:(tl + 1) * 128], sl["d"][:, fc, half * 512:(half + 1) * 512],
                                 k == 0, k == 3, [hs[j][fc], sl["d"]], [pd])
                            k += 1
                    a = self.acc[:, t, half * 512:(half + 1) * 512]
                    B.tt("dve", a, a, pd[:], ALU.add, [self.acc, pd], [self.acc])
                    yield

        items = [(gi, ch) for gi in range(NG) for ch in range(nchunk)]
        prev = None
        pending = None
        for (gi, ch) in items:
            interleave(gen_gu(gi, ch), gen_down(*prev) if prev is not None else None, pending)
            pending = None
            prev = (gi, ch)
            if ch == 0 and gi + 1 < NG:
                if nchunk >= 2:
                    pending = gen_load(gi + 1)
                else:
                    load_group(gi + 1)
        interleave(gen_down(*prev))

    def ple(self):
        B, d, c = self.B, self.d, self.c
        sb = B.sb
        B.S.barrier()
        B.release(self.mk_after_acc)
        m = self.m = {}
        self.cast_engs = ("act", "dve")
        self.new_stage()
        wpg = sb("wpg_bf", [128, 8, D], BF16)
        wple = sb("wple_bf", [128, 2, D], BF16)
        self.load_rows(wpg, "w_pg", 8)
        self.load_rows(wple, "w_ple", 2)
        grow = sb("gpg_row", [128, D], F32)
        B.dma(grow[:], d["gpg_row"][:].partition_broadcast(128), [d["gpg_row"]], [grow])
        gple = sb("gple_b", [128, D], F32)
        B.dma(gple[:], d["gple"][:].partition_broadcast(128), [d["gple"]], [gple])
        B.ts("pool", gple[:], gple[:], 0.5, None, ALU.mult, None, [gple], [gple])
        m["junk"] = sb("junk", [128, D], BF16)
        junk2 = sb("junk2", [128, 512], BF16)
        R2 = lambda nm, shp, dt, n=2: [sb("%s%d" % (nm, i), shp, dt) for i in range(n)]
        hb = R2("hb3", [128, D], BF16)
        h3T = R2("h3T", [128, 8, 128], BF16)
        ss, rs = R2("ss", [128, 1], F32), R2("rs", [128, 1], F32)
        ssp, rsp = R2("ssp", [128, 2], F32), R2("rsp", [128, 1], F32)
        pt = R2("pt", [128, 256], F32)
        pbf = R2("pbf", [128, 256], BF16)
        pT = R2("pT", [128, 2, 128], BF16)
        sgate = R2("sgate", [128, D], F32)
        ple = R2("ple", [128, D], F32)
        ot = R2("ot", [128, D], F32)
        NTL = self.n_own

        def front(t):
            if t >= NTL:
                return
            i = t % 2
            xa = self.acc[:, t, :]
            if t + 1 < NTL:
                B.dma(pt[(t + 1) % 2][:], d["p"][(t + 1) * 128:(t + 2) * 128, :], [d["p"]], [pt[(t + 1) % 2]])
            B.act(m["junk"][:], xa, AF.Square, [self.acc, ss[i]], [m["junk"], ss[i]], accum_out=ss[i][:])
            self.rsqrt(rs[i][:], ss[i][:], 1, 1.0 / D, [ss[i]], [rs[i]])
            B.stt("dve", hb[i][:], xa, rs[i][:, 0:1], grow[:], ALU.mult, ALU.mult, [self.acc, rs[i], grow], [hb[i]])
            B.cp("dve", pbf[i][:], pt[i][:], [pt[i]], [pbf[i]])
            yield

        def front_b(t):
            if t >= NTL:
                return
            i = t % 2
            pb = self.psb[7]
            pbb = pb[:].bitcast(BF16)
            for cc in range(8):
                B.tr(pbb[:, cc * 128:(cc + 1) * 128], hb[i][:, cc * 128:(cc + 1) * 128], c["ident"][:], [hb[i], c["ident"]], [pb])
            B.cp("act", h3T[i][:], pbb[:, 0:D].rearrange("p (a q) -> p a q", a=8), [pb], [h3T[i]])
            yield
            p4 = self.psb[6]
            p4b = p4[:].bitcast(BF16)
            for kc in range(2):
                B.tr(p4b[:, kc * 128:(kc + 1) * 128], pbf[i][:, kc * 128:(kc + 1) * 128], c["ident"][:], [pbf[i], c["ident"]], [p4])
            B.cp("dve", pT[i][:], p4b[:, 0:256].rearrange("p (a q) -> p a q", a=2), [p4], [pT[i]])
            yield

        def mid(t):
            if not (0 <= t < NTL):
                return
            i = t % 2
            for half in range(2):
                pb = self.psb[2 + 2 * i + half]
                for kc in range(2):
                    B.mm(pb[:], pT[i][:, kc, :], wple[:, kc, half * 512:(half + 1) * 512], kc == 0, kc == 1, [pT[i], wple], [pb])
                B.act(junk2[:], pb[:], AF.Square, [pb, ssp[i]], [junk2, ssp[i]], accum_out=ssp[i][:, half:half + 1])
            yield
            for half in range(2):
                pb = self.psb[half]
                for kc in range(8):
                    B.mm(pb[:], h3T[i][:, kc, :], wpg[:, kc, half * 512:(half + 1) * 512], kc == 0, kc == 7, [h3T[i], wpg], [pb])
                B.act(sgate[i][:, half * 512:(half + 1) * 512], pb[:], AF.Tanh, [pb], [sgate[i]], scale=0.5)
                yield

        def back(t):
            if not (0 <= t < NTL):
                return
            i = t % 2
            xa = self.acc[:, t, :]
            B.tt("dve", rsp[i][:], ssp[i][:, 0:1], ssp[i][:, 1:2], ALU.add, [ssp[i]], [rsp[i]])
            self.rsqrt(rsp[i][:], rsp[i][:], 1, 1.0 / D, [rsp[i]], [rsp[i]])
            yield
            for half in range(2):
                pb = self.psb[2 + 2 * i + half]
                hsl = slice(half * 512, (half + 1) * 512)
                B.stt("dve", ple[i][:, hsl], pb[:], rsp[i][:, 0:1], gple[:, hsl], ALU.mult, ALU.mult, [pb, rsp[i], gple], [ple[i]])
            yield
            o = ot[i]
            B.stt("dve", o[:], sgate[i][:], 1.0, ple[i][:], ALU.add, ALU.mult, [sgate[i], ple[i]], [o])
            B.tt("pool", o[:], o[:], xa, ALU.add, [o, self.acc], [o])
            op = B.dma(self.out[t * 128:(t + 1) * 128, :], o[:], [o], [self.out])
            B.S.final_waits.append(op)
            yield

        B.dma(pt[0][:], d["p"][0:128, :], [d["p"]], [pt[0]])
        interleave(front(0))
        interleave(front(1), front_b(0))
        interleave(front(2), front_b(1), mid(0))
        for t in range(NTL):
            interleave(front(t + 3), front_b(t + 2), mid(t + 1), back(t))


def _pc(v, n):
    return np.ascontiguousarray(np.asarray(v, np.float32).reshape(n, 128).T)


def prep_inputs(inp):
    ct = consts()
    f = lambda a: np.ascontiguousarray(np.asarray(a, np.float32))
    x = f(inp["x"])
    p = f(inp["p"])[0]
    shared = dict(
        w_in=f(inp["w_in"])[0], w_ret_o=f(inp["w_ret_o"])[0], w_swa_o=f(inp["w_swa_o"])[0], w_out=f(inp["w_out"])[0],
        w_rt=np.ascontiguousarray(np.concatenate([f(inp["w_router_group"])[0], f(inp["w_router_expert"])[0]], axis=1)),
        w_gate=f(inp["w_exp_gate"])[0], w_up=f(inp["w_exp_up"])[0], w_down=f(inp["w_exp_down"])[0],
        w_pg=f(inp["w_ple_gate"])[0], w_ple=f(inp["w_ple"])[0],
        gmix=_pc(inp["mix_norm_g"][0], 8), gffn=_pc(inp["ffn_norm_g"][0], 8), gpg=_pc(inp["ple_gate_norm_g"][0], 8),
        gng=_pc(inp["ret_gn_g"][0], 4), gnb=_pc(inp["ret_gn_b"][0], 4),
        gqd=np.ascontiguousarray(np.tile(f(inp["q_norm_g"])[0], 2).reshape(128, 1)),
        gkd=np.ascontiguousarray(np.tile(f(inp["k_norm_g"])[0], 2).reshape(128, 1)),
        sinks=f(inp["attn_sinks"]).reshape(1, 8),
        brt=np.ascontiguousarray(np.concatenate([f(inp["b_router_group"])[0], f(inp["b_router_expert"])[0]]).reshape(1, 36)),
        gple=f(inp["ple_norm_g"]).reshape(1, D),
        gffn_row=f(inp["ffn_norm_g"]).reshape(1, D), gpg_row=f(inp["ple_gate_norm_g"]).reshape(1, D),
        ident=ct["ident"], qdec=ct["qdec"], kdec=ct["kdec"], adec=ct["adec"], cmask=ct["cmask"],
        swab_prev=ct["swab_prev"], swab_cur=ct["swab_cur"], blockones=ct["blockones"], sel=ct["sel"],
    )
    maps = []
    zeros = np.zeros((TOK, D), np.float32)
    allneg = ct["swab_neg"]
    for cid in range(NCORES):
        b, half = cid // 2, cid % 2
        mp = dict(shared)
        mp["x"] = np.ascontiguousarray(x[b, half * TOK:(half + 1) * TOK])
        mp["xprev"] = np.ascontiguousarray(x[b, 0:TOK]) if half == 1 else zeros
        mp["p"] = np.ascontiguousarray(p[b, half * TOK:(half + 1) * TOK])
        mp["swab_first"] = ct["swab_prev"] if half == 1 else allneg
        maps.append(mp)
    return maps


_PROG = None


def kernel(**inputs):
    global _PROG
    if _PROG is None:
        _PROG = Prog()
    maps = prep_inputs(inputs)
    res = run_bass_kernel_spmd(_PROG.B.nc, maps, core_ids=list(range(NCORES)))
    out = np.empty((4, 4096, D), np.float32)
    for cid in range(NCORES):
        b, half = cid // 2, cid % 2
        out[b, half * TOK:(half + 1) * TOK] = res.results[cid]["out"]
    return out
```

```python
import os
import numpy as np
import ml_dtypes
import concourse.bass as bass
import concourse.mybir as mybir
from concourse.bass_utils import run_bass_kernel_spmd

F32 = mybir.dt.float32
BF16 = mybir.dt.bfloat16
AF = mybir.ActivationFunctionType
ALU = mybir.AluOpType
AX = mybir.AxisListType

D = 1024
NCORES = 8
TOK = 2048
NT = 16
EPS = 1e-6
IN_W = 4864
C_RQ, C_RK, C_RV, C_RG, C_SQ, C_SK, C_SV, C_GR, C_GS = 0, 512, 1024, 1536, 2048, 2560, 2688, 2816, 3840
NEG = -30000.0


class Buf:
    __slots__ = ("name", "w", "r", "rd")

    def __init__(self, name):
        self.name = name
        self.w = None
        self.r = {}
        self.rd = []


class Op:
    __slots__ = ("eng", "fn", "deps", "sig", "cnt", "dma", "dsem", "dval", "gidx")


class Sched:
    ENGS = ("pe", "act", "dve", "pool", "sp")

    def __init__(self, nc, n_dma_sems=8):
        self.nc = nc
        self.q = {e: [] for e in self.ENGS}
        self.n = 0
        self.n_dma_sems = n_dma_sems
        self.final_waits = []

    def add(self, eng, fn, reads=(), writes=(), dma=False, force=False):
        import os
        lim = int(os.environ.get("PROG_MAXOPS", "0"))
        if lim and self.n >= lim and not force:
            self.n += 1
            return None
        if os.environ.get("PROG_TRACE"):
            import sys as _s
            fr = _s._getframe(2)
            print("OP", self.n, eng, fr.f_code.co_name, fr.f_lineno, _s._getframe(3).f_code.co_name, _s._getframe(3).f_lineno)
        op = Op()
        op.eng = eng
        op.fn = fn
        op.dma = dma
        op.sig = dma
        op.cnt = 0
        op.gidx = self.n
        self.n += 1
        deps = {}

        def need(p):
            if p is None:
                return
            if (not p.dma) and p.eng == eng and eng == "pe":
                return
            if p.dma:
                deps[("d", p.gidx)] = p
            else:
                k = ("c", p.eng)
                if k not in deps or deps[k].gidx < p.gidx:
                    deps[k] = p

        for b in reads:
            need(b.w)
        for b in writes:
            need(b.w)
            for p in b.r.values():
                need(p)
            for p in b.rd:
                need(p)
        op.deps = list(deps.values())
        for p in op.deps:
            p.sig = True
        for b in reads:
            if dma:
                b.rd.append(op)
            else:
                b.r[eng] = op
        for b in writes:
            b.w = op
            b.r = {}
            b.rd = []
        self.q[eng].append(op)
        return op

    def barrier(self):
        lasts = []
        for e in self.ENGS:
            ops = [o for o in self.q[e] if not o.dma and o.fn is not None]
            if ops:
                lasts.append(ops[-1])
            dops = [o for o in self.q[e] if o.dma]
            lasts.extend(dops[-self.n_dma_sems:])
        for e in self.ENGS:
            op = Op()
            op.eng, op.fn, op.dma, op.sig, op.cnt, op.gidx = e, None, False, False, 0, self.n
            self.n += 1
            op.deps = [p for p in lasts if p.dma or p.eng != e]
            for p in op.deps:
                p.sig = True
            self.q[e].append(op)

    def emit(self):
        nc = self.nc
        from contextlib import ExitStack
        with ExitStack() as es:
            sems = {e: es.enter_context(nc.semaphore("s_" + e)) for e in self.ENGS}
            dsems = {}
            for e in self.ENGS:
                if any(o.dma for o in self.q[e]):
                    dsems[e] = [es.enter_context(nc.semaphore("d_%s%d" % (e, i))) for i in range(self.n_dma_sems)]
            for e in self.ENGS:
                c = 0
                k = 0
                for o in self.q[e]:
                    if o.dma:
                        o.dsem = dsems[e][k % self.n_dma_sems]
                        o.dval = 16 * (k // self.n_dma_sems + 1)
                        k += 1
                    elif o.sig:
                        c += 1
                        o.cnt = c
            block = es.enter_context(nc.Block())
            handles = {"pe": block.tensor, "act": block.scalar, "dve": block.vector, "pool": block.gpsimd, "sp": block.sync}

            def make(e):
                def body(eng):
                    known = {}

                    def wait(sem, val):
                        if known.get(sem.num, 0) >= val:
                            return
                        eng.wait_ge(sem, val)
                        known[sem.num] = val

                    for o in self.q[e]:
                        for p in o.deps:
                            if p.dma:
                                wait(p.dsem, p.dval)
                            else:
                                wait(sems[p.eng], p.cnt)
                        if o.dma and o.dval > 16:
                            wait(o.dsem, o.dval - 16)
                        if o.fn is None:
                            continue
                        ins = o.fn(eng)
                        if o.dma:
                            ins.then_inc(o.dsem, 16)
                        elif o.sig:
                            ins.then_inc(sems[e], 1)
                    if e == "sp":
                        for p in [q_ for q_ in self.final_waits if q_ is not None]:
                            wait(p.dsem, p.dval)
                return body

            for e in self.ENGS:
                if self.q[e] or e == "sp":
                    handles[e](make(e))


def _const_tables():
    t = {}
    t["ident"] = np.eye(128, dtype=np.float32).astype(ml_dtypes.bfloat16)
    h = np.arange(4, dtype=np.float64)
    gam = 1.0 - np.exp2(-5.0 - h)
    lg = np.log(gam)
    pos = np.arange(128, dtype=np.float64)
    qd = np.exp((pos[None, :] - 127.0) * lg[:, None])
    kd = np.exp((127.0 - pos[None, :]) * lg[:, None]) * (128.0 ** -0.5)
    t["qdec"] = np.broadcast_to(qd[None], (128, 4, 128)).astype(np.float32).copy()
    t["kdec"] = np.broadcast_to(kd[None], (128, 4, 128)).astype(np.float32).copy()
    a = np.exp(128.0 * lg)
    t["adec"] = np.broadcast_to(np.repeat(a, 128)[None], (128, 512)).astype(np.float32).copy()
    j = np.arange(128)[:, None]
    i = np.arange(128)[None, :]
    cm = (i >= j).astype(np.float32)
    t["cmask"] = np.broadcast_to(cm[:, None, :], (128, 4, 128)).astype(np.float32).copy()
    slopes = np.exp2(-(np.arange(8, dtype=np.float64) + 1.0))
    s = np.arange(128, dtype=np.float64)[:, None]
    q = np.arange(128, dtype=np.float64)[None, :]
    relp = q + 128.0 - s
    relc = q - s
    bp = np.where((relp >= 0) & (relp < 128), 0.0, 1.0)
    bc = np.where((relc >= 0) & (relc < 128), 0.0, 1.0)
    sbp = np.empty((128, 8, 128), np.float32)
    sbc = np.empty((128, 8, 128), np.float32)
    for hh in range(8):
        sbp[:, hh, :] = np.where(bp > 0, NEG, -slopes[hh] * relp)
        sbc[:, hh, :] = np.where(bc > 0, NEG, -slopes[hh] * relc)
    def hl(a):
        hi = a.astype(ml_dtypes.bfloat16)
        lo = (a - hi.astype(np.float32)).astype(ml_dtypes.bfloat16)
        return np.ascontiguousarray(np.stack([hi, lo], axis=1))
    t["swab_prev"] = hl(sbp)
    t["swab_cur"] = hl(sbc)
    t["swab_neg"] = hl(np.full((128, 8, 128), NEG, np.float32))
    bo = np.zeros((128, 128), np.float32)
    bo[:64, :64] = 1.0 / 64
    bo[64:, 64:] = 1.0 / 64
    t["blockones"] = bo.astype(ml_dtypes.bfloat16)
    sel = np.zeros((64, 32, 128), np.float32)
    for e in range(32):
        sel[e, e, :] = 1.0
        sel[32 + e, e, :] = 1.0
    t["sel"] = sel.astype(ml_dtypes.bfloat16)
    return t


_CT = None


def consts():
    global _CT
    if _CT is None:
        _CT = _const_tables()
    return _CT


class T:
    def __init__(self, h, name):
        self.h = h
        self.b = Buf(name)

    def __getitem__(self, k):
        return self.h[k]


class Builder:
    def __init__(self):
        self.nc = bass.Bass("TRN2", target_bir_lowering=False)
        self.S = Sched(self.nc)
        self.dbg = []

    def din(self, name, shape, dt=F32):
        return T(self.nc.dram_tensor(name, list(shape), dt, kind="ExternalInput").ap(), name)

    def dout(self, name, shape, dt=F32):
        return T(self.nc.dram_tensor(name, list(shape), dt, kind="ExternalOutput").ap(), name)

    def dscratch(self, name, shape, dt=F32):
        return T(self.nc.dram_tensor(name, list(shape), dt).ap(), name)

    def sb(self, name, shape, dt):
        esz = 2 if dt == BF16 else 4
        n = 1
        for v in shape[1:]:
            n *= v
        nbytes = (n * esz + 31) // 32 * 32
        if not hasattr(self, "sp"):
            self.sp = (self.nc.sbuf_base + 63) // 64 * 64
            self.uid = 0
        off = self.sp
        self.sp += nbytes
        assert self.sp <= self.nc.sbuf_top, "SBUF overflow at %s: %d > %d" % (name, self.sp, self.nc.sbuf_top)
        self.hw = max(getattr(self, "hw", 0), self.sp)
        self.uid += 1
        return T(self.nc.alloc_sbuf_tensor_at("%s_%d" % (name, self.uid), list(shape), dt, offset=off), name)

    def mark(self):
        return self.sp

    def release(self, mk):
        if os.environ.get("PROG_SBUF"):
            print("SBUF high-water before release: %d of %d" % (self.hw, self.nc.sbuf_top))
        self.hw = 0
        self.sp = mk

    def ps(self, name):
        t = T(self.nc.alloc_psum_tensor(name, [128, 512], F32), name)
        t.psum = True
        return t

    def _bufs(self, ts):
        return [t.b for t in ts]

    def _rw(self, rd, wr):
        wr = list(wr) + [t for t in rd if getattr(t, "psum", False)]
        return [t.b for t in rd], [t.b for t in wr]

    def dma(self, out, in_, rd, wr, q="sp"):
        return self.S.add(q, lambda e: e.dma_start(out=out, in_=in_), *self._rw(rd, wr), dma=True)

    def mm(self, out, lhsT, rhs, start, stop, rd, wr):
        return self.S.add("pe", lambda e: e.matmul(out, lhsT, rhs, start=start, stop=stop), *self._rw(rd, wr))

    def tr(self, out, in_, ident, rd, wr):
        return self.S.add("pe", lambda e: e.transpose(out, in_, ident), *self._rw(rd, wr))

    def act(self, out, in_, func, rd, wr, bias=None, scale=None, accum_out=None):
        kw = {}
        if bias is not None:
            kw["bias"] = bias
        if scale is not None:
            kw["scale"] = scale
        if accum_out is not None:
            kw["accum_out"] = accum_out
        return self.S.add("act", lambda e: e.activation(out, in_, func, **kw), *self._rw(rd, wr))

    def tt(self, eng, out, in0, in1, op, rd, wr):
        return self.S.add(eng, lambda e: e.tensor_tensor(out, in0, in1, op), *self._rw(rd, wr))

    def ts(self, eng, out, in0, s1, s2, op0, op1, rd, wr):
        if op1 is None:
            return self.S.add(eng, lambda e: e.tensor_scalar(out, in0, s1, None, op0), *self._rw(rd, wr))
        return self.S.add(eng, lambda e: e.tensor_scalar(out, in0, s1, s2, op0, op1), *self._rw(rd, wr))

    def stt(self, eng, out, in0, scalar, in1, op0, op1, rd, wr):
        return self.S.add(eng, lambda e: e.scalar_tensor_tensor(out, in0, scalar, in1, op0, op1), *self._rw(rd, wr))

    def cp(self, eng, out, in_, rd, wr):
        if eng == "act":
            return self.S.add("act", lambda e: e.copy(out, in_), *self._rw(rd, wr))
        return self.S.add(eng, lambda e: e.tensor_copy(out, in_), *self._rw(rd, wr))

    def memset(self, eng, ap, val, wr):
        return self.S.add(eng, lambda e: e.memset(ap, val), [], self._bufs(wr))

    def dump(self, name, t, ap, shape, dt):
        o = self.dout("dbg_" + name, shape, dt)
        op = self.S.add("sp", lambda e: e.dma_start(out=o[:], in_=ap), [t.b], [o.b], dma=True, force=True)
        self.S.final_waits.append(op)
        self.dbg.append("dbg_" + name)


def interleave(*gens):
    gens = [g for g in gens if g is not None]
    while gens:
        for g in list(gens):
            try:
                next(g)
            except StopIteration:
                gens.remove(g)


class Prog:
    def __init__(self, n_prev=16, n_own=16, do_moe=True, do_ple=True, n_groups=16, dbg=None):
        self.B = B = Builder()
        self.n_prev, self.n_own = n_prev, n_own
        self.dbg = dbg or set()
        d = self.d = {}
        for nm, shp, dt in [
            ("x", [TOK, D], F32), ("xprev", [TOK, D], F32), ("p", [TOK, 256], F32),
            ("w_in", [D, IN_W], F32), ("w_ret_o", [512, D], F32), ("w_swa_o", [512, D], F32),
            ("w_out", [D, D], F32), ("w_rt", [D, 36], F32),
            ("w_gate", [32, D, 256], F32), ("w_up", [32, D, 256], F32), ("w_down", [32, 256, D], F32),
            ("w_pg", [D, D], F32), ("w_ple", [256, D], F32),
            ("gmix", [128, 8], F32), ("gffn", [128, 8], F32), ("gpg", [128, 8], F32),
            ("gng", [128, 4], F32), ("gnb", [128, 4], F32), ("gqd", [128, 1], F32), ("gkd", [128, 1], F32),
            ("sinks", [1, 8], F32), ("brt", [1, 36], F32), ("gple", [1, D], F32),
            ("gffn_row", [1, D], F32), ("gpg_row", [1, D], F32),
            ("ident", [128, 128], BF16), ("qdec", [128, 4, 128], F32), ("kdec", [128, 4, 128], F32),
            ("adec", [128, 512], F32), ("cmask", [128, 4, 128], F32),
            ("swab_prev", [128, 2, 8, 128], BF16), ("swab_cur", [128, 2, 8, 128], BF16), ("swab_first", [128, 2, 8, 128], BF16),
            ("blockones", [128, 128], BF16), ("sel", [64, 32, 128], BF16),
        ]:
            d[nm] = B.din(nm, shp, dt)
        self.out = B.dout("out", [TOK, D], F32)
        self.x1s = B.dscratch("x1s", [TOK, D], F32)
        self.psb = [B.ps("bank%d" % i) for i in range(8)]
        self.c = {}
        self.ncast = 0
        self.cast_engs = ("act", "dve")
        self.zbank = 0
        self.lc(["ident", "gmix", "gffn", "gpg"])
        self.mhalf = B.sb("mhalf", [128, 256], F32)
        B.memset("pool", self.mhalf[:], -0.5, [self.mhalf])
        self.h2T = B.sb("h2T", [128, 8, TOK], BF16)
        self.lg_all = B.sb("lg_all", [128, 16, 36], F32)
        mk0 = B.mark()
        self.grT = B.sb("grT", [128, 16, 8, 128], BF16)
        mk = B.mark()
        self.phaseA1()
        if "dumpA1" in self.dbg:
            m = self.m
            for nm, t, shp, dt in [("qT", m["qT"][1], [128, 4, 128], BF16), ("kT", m["kT"][1], [128, 4, 128], BF16),
                                   ("vtok", m["vtok"][1], [128, 512], BF16), ("scT", m["scT"], [128, 4, 128], BF16),
                                   ("yn", m["yn"], [128, 512], BF16), ("YrT", m["YrT"][1], [128, 4, 128], BF16),
                                   ("sgT", m["sgT"][1], [128, 4, 128], BF16), ("sigr", m["sigr"][1], [128, 8, 128], BF16),
                                   ("S", m["S"], [128, 512], F32), ("hT", m["hT"][1], [128, 1024], BF16),
                                   ("mean", m["mean"], [128, 4], F32), ("rstd", m["rstd"], [128, 4], F32),
                                   ("ktok", m["ktok"], [128, 512], BF16), ("t1", m["t1"], [128, 4, 128], BF16)]:
                B.dump(nm, t, t[:], shp, dt)
        if "stopA1" in self.dbg:
            B.dump("grT", self.grT, self.grT[:, 0:n_own], [128, n_own, 8, 128], BF16)
            B.S.emit()
            return
        B.S.barrier()
        B.release(mk)
        self.phaseA2()
        B.S.barrier()
        B.release(mk0)
        if do_moe:
            self.moe(n_groups)
        if do_ple:
            self.ple()
        B.S.emit()

    SHAPES = {"ident": ([128, 128], BF16), "qdec": ([128, 4, 128], F32), "kdec": ([128, 4, 128], F32),
              "adec": ([128, 512], F32), "cmask": ([128, 4, 128], F32), "swab_prev": ([128, 2, 8, 128], BF16),
              "swab_cur": ([128, 2, 8, 128], BF16), "swab_first": ([128, 2, 8, 128], BF16), "blockones": ([128, 128], BF16),
              "gmix": ([128, 8], F32), "gffn": ([128, 8], F32), "gpg": ([128, 8], F32), "gng": ([128, 4], F32),
              "gnb": ([128, 4], F32), "gqd": ([128, 1], F32), "gkd": ([128, 1], F32), "sel": ([64, 32, 128], BF16)}

    def lc(self, names):
        B, d, c = self.B, self.d, self.c
        for nm in names:
            shp, dt = self.SHAPES[nm]
            c[nm] = B.sb("c_" + nm, shp, dt)
            B.dma(c[nm][:], d[nm][:], [d[nm]], [c[nm]])

    def cast(self, out, in_, rd, wr, scale=None):
        B = self.B
        eng = self.cast_engs[self.ncast % len(self.cast_engs)]
        self.ncast += 1
        if scale is None:
            B.cp(eng, out, in_, rd, wr)
        elif eng == "act":
            B.act(out, in_, AF.Copy, rd, wr, scale=scale)
        else:
            B.ts(eng, out, in_, scale, None, ALU.mult, None, rd, wr)

    def rsqrt(self, dst, src, n, scale, rd_t, wr_t):
        B = self.B
        B.ts("pool", dst, src, scale, EPS, ALU.mult, ALU.add, rd_t, wr_t)
        for c0 in range(0, n, 256):
            c1 = min(n, c0 + 256)
            B.tt("pool", dst[:, c0:c1], dst[:, c0:c1], self.mhalf[:, 0:c1 - c0], ALU.pow, wr_t + [self.mhalf], wr_t)

    def new_stage(self, n=6, cols=1024):
        self.stage = [self.B.sb("stage%d" % i, [128, cols], F32) for i in range(n)]
        self.stage_cols = cols
        self.nstage = 0

    def next_stage(self):
        st = self.stage[self.nstage % len(self.stage)]
        self.nstage += 1
        return st

    def load_rows(self, dst, src_name, nk, scale_name=None, const_scale=None):
        B, d, c = self.B, self.d, self.c
        v = d[src_name][:].rearrange("(c p) n -> p c n", p=128)
        n = v.shape[2]
        per = max(1, self.stage_cols // n)
        for kc in range(0, nk, per):
            st = self.next_stage()
            k1 = min(nk, kc + per)
            stv = st[:, 0:(k1 - kc) * n].rearrange("p (c n) -> p c n", c=k1 - kc)
            B.dma(stv, v[:, kc:k1, :], [d[src_name]], [st])
            if scale_name is None and const_scale is not None:
                self.B.ts("dve", dst[:, kc:k1, :], stv, const_scale, None, ALU.mult, None, [st], [dst])
            elif scale_name is None:
                self.cast(dst[:, kc:k1, :], stv, [st], [dst])
            else:
                for k in range(kc, k1):
                    self.cast(dst[:, k, :], stv[:, k - kc, :], [st, c[scale_name]], [dst], scale=c[scale_name][:, k:k + 1])

    def load_win(self, dst, pieces):
        B, d, c = self.B, self.d, self.c
        win_v = d["w_in"][:].rearrange("(c p) n -> p c n", p=128)
        SC = self.stage_cols
        for kc in range(8):
            sc = c["gmix"][:, kc:kc + 1]
            for (c0, c1, dsts) in pieces:
                for p0 in range(c0, c1, SC):
                    p1 = min(c1, p0 + SC)
                    st = self.next_stage()
                    B.dma(st[:, 0:p1 - p0], win_v[:, kc, p0:p1], [d["w_in"]], [st])
                    for (dc0, r0, r1) in dsts:
                        a0, a1 = max(r0, p0 - c0), min(r1, p1 - c0)
                        if a0 >= a1:
                            continue
                        self.cast(dst[:, kc, dc0 + (a0 - r0):dc0 + (a1 - r0)], st[:, a0 - (p0 - c0):a1 - (p0 - c0)], [st, c["gmix"]], [dst], scale=sc)

    def common_bufs(self, nx=2):
        B, m = self.B, self.m
        sb = B.sb
        m["x"] = [sb("xr%d" % i, [128, D], F32) for i in range(nx)]
        m["junk"] = sb("junk", [128, D], BF16)
        m["ss"] = [sb("ss%d" % i, [128, 1], F32) for i in range(2)]
        m["rs"] = [sb("rs%d" % i, [128, 1], F32) for i in range(2)]
        m["hb"] = sb("hb", [128, D], BF16)
        m["hT"] = [sb("hT%d" % i, [128, D], BF16) for i in range(2)]

    def phaseA1(self):
        B, d, c = self.B, self.d, self.c
        sb = B.sb
        self.lc(["qdec", "kdec", "adec", "cmask", "gng", "gnb"])
        W = self.W = {}
        W["in"] = sb("w1_bf", [128, 8, 3072], BF16)
        W["ret_o"] = sb("w_ret_o_bf", [128, 4, D], BF16)
        mk_st = B.mark()
        self.new_stage()
        self.load_win(W["in"], [(0, 2048, [(0, 0, 2048)]), (C_GR, C_GR + 1024, [(2048, 0, 1024)])])
        self.load_rows(W["ret_o"], "w_ret_o", 4, const_scale=0.25)
        B.S.barrier()
        B.release(mk_st)
        m = self.m = {}
        self.common_bufs()
        R2 = lambda nm, shp, dt, n=2: [sb("%s%d" % (nm, i), shp, dt) for i in range(n)]
        m["qT"] = R2("qT", [128, 4, 128], BF16)
        m["kT"] = R2("kT", [128, 4, 128], BF16)
        m["sgT"] = R2("sgT", [128, 4, 128], BF16, 3)
        m["th"] = sb("th", [128, 4, 128], BF16)
        m["sigr"] = R2("sigr", [128, 8, 128], BF16, 4)
        m["vtok"] = R2("vtok", [128, 512], BF16)
        m["ktok"] = sb("ktok", [128, 512], BF16)
        m["S"] = sb("S", [128, 512], F32)
        m["Shat"] = sb("Shat", [128, 512], BF16)
        m["scT"] = sb("scT", [128, 4, 128], BF16)
        m["ysq"] = sb("ysq", [128, 512], F32)
        for nm in ("s1", "s2", "mean", "msq", "var", "rstd"):
            m[nm] = sb("gn_" + nm, [128, 4], F32)
        m["yn"] = sb("yn", [128, 512], BF16)
        m["t1"] = sb("t1", [128, 4, 128], BF16)
        m["YrT"] = R2("YrT", [128, 4, 128], BF16)
        B.memset("pool", m["S"][:], 0.0, [m["S"]])
        tiles = [("prev", 16 - self.n_prev + i) for i in range(self.n_prev)] + [("own", i) for i in range(self.n_own)]
        self.first_state = True

        def front_n(u):
            if u < len(tiles):
                yield from self.gen_N(u, tiles[u])

        def front_z(u):
            if u < len(tiles):
                yield from self.gen_Z1(u, tiles[u])

        def mid(u):
            if 0 <= u < len(tiles):
                yield from self.gen_R(u, tiles[u])

        def mid2(u):
            if 0 <= u < len(tiles) and tiles[u][0] == "own":
                yield from self.gen_R2(u, tiles[u])

        def back(u):
            if 0 <= u < len(tiles) and tiles[u][0] == "own":
                yield from self.gen_O1(u, tiles[u])

        self.cur_tiles = tiles
        self.load_x(0)
        interleave(front_n(0))
        interleave(front_n(1), front_z(0))
        for u in range(len(tiles) + 2):
            interleave(front_n(u + 2), front_z(u + 1), mid(u), mid2(u - 1), back(u - 2))

    def load_x(self, u):
        tiles = self.cur_tiles
        if (u in tiles) if isinstance(tiles, dict) else (0 <= u < len(tiles)):
            kind, ti = tiles[u]
            src = self.d["xprev"] if kind == "prev" else self.d["x"]
            xt = self.m["x"][u % len(self.m["x"])]
            self.B.dma(xt[:], src[ti * 128:(ti + 1) * 128, :], [src], [xt])

    def gen_N(self, u, tile):
        B, d, c, m = self.B, self.d, self.c, self.m
        kind, ti = tile
        xt = m["x"][u % len(m["x"])]
        ss, rs, hb, hT = m["ss"][u % 2], m["rs"][u % 2], m["hb"], m["hT"][u % 2]
        self.load_x(u + 1)
        B.act(m["junk"][:], xt[:], AF.Square, [xt, ss], [m["junk"], ss], accum_out=ss[:])
        self.rsqrt(rs[:], ss[:], 1, 1.0 / D, [ss], [rs])
        B.act(hb[:], xt[:], AF.Copy, [xt, rs], [hb], scale=rs[:, 0:1])
        yield
        pb = self.psb[2]
        pbb = pb[:].bitcast(BF16)
        for cc in range(8):
            B.tr(pbb[:, cc * 128:(cc + 1) * 128], hb[:, cc * 128:(cc + 1) * 128], c["ident"][:], [hb, c["ident"]], [pb])
        B.cp("dve", hT[:], pbb[:, 0:D], [pb], [hT])
        yield

    def zgroup(self, u, c0, nchunk):
        B, W, m = self.B, self.W, self.m
        hT = m["hT"][u % 2]
        pb = self.psb[self.zbank % 2]
        self.zbank += 1
        pv = pb[:].rearrange("p (a q) -> p a q", a=4)
        for a in range(nchunk):
            for kc in range(8):
                B.mm(pv[:, a, :], W["in"][:, kc, c0 + a * 128:c0 + (a + 1) * 128], hT[:, kc * 128:(kc + 1) * 128],
                     kc == 0, kc == 7, [W["in"], hT], [pb])
        return pb, pv

    def ztok(self, u, c0, n):
        B, W, m = self.B, self.W, self.m
        hT = m["hT"][u % 2]
        pb = self.psb[self.zbank % 2]
        self.zbank += 1
        for kc in range(8):
            B.mm(pb[:, 0:n], hT[:, kc * 128:(kc + 1) * 128], W["in"][:, kc, c0:c0 + n], kc == 0, kc == 7, [hT, W["in"]], [pb])
        return pb

    def gen_Z1(self, u, tile):
        B, d, c, m, W = self.B, self.d, self.c, self.m, self.W
        kind, ti = tile
        own = kind == "own"
        s = u % 2
        pb, pv = self.zgroup(u, 512, 4)
        B.tt("dve", m["kT"][s][:], pv, c["kdec"][:], ALU.mult, [pb, c["kdec"]], [m["kT"][s]])
        yield
        pb = self.ztok(u, 1024, 512)
        B.cp("act", m["vtok"][s][:], pb[:], [pb], [m["vtok"][s]])
        yield
        if not own:
            return
        pb, pv = self.zgroup(u, 0, 4)
        B.tt("dve", m["qT"][s][:], pv, c["qdec"][:], ALU.mult, [pb, c["qdec"]], [m["qT"][s]])
        yield
        pb, pv = self.zgroup(u, 1536, 4)
        B.act(m["th"][:], pv, AF.Tanh, [pb], [m["th"]], scale=0.5)
        B.stt("dve", m["sgT"][u % 3][:], m["th"][:], 1.0, pv, ALU.add, ALU.mult, [pb, m["th"]], [m["sgT"][u % 3]])
        yield
        for g2 in range(2):
            pb, pv = self.zgroup(u, 2048 + g2 * 512, 4)
            B.act(m["sigr"][u % 4][:, g2 * 4:(g2 + 1) * 4, :], pv, AF.Tanh, [pb], [m["sigr"][u % 4]], scale=0.5)
            yield

    def gen_R(self, u, tile):
        B, d, c, m, W = self.B, self.d, self.c, self.m, self.W
        kind, ti = tile
        own = kind == "own"
        s = u % 2
        kT, qT, vtok, ktok = m["kT"][s], m["qT"][s], m["vtok"][s], m["ktok"]
        S, Shat = m["S"], m["Shat"]
        if not self.first_state:
            B.tt("dve", S[:], S[:], c["adec"][:], ALU.mult, [S, c["adec"]], [S])
        self.first_state = False
        if own:
            B.cp("act", Shat[:], S[:], [S], [Shat])
        p3 = self.psb[3]
        p3b = p3[:].bitcast(BF16)
        for h in range(4):
            B.tr(p3b[:, h * 128:(h + 1) * 128], kT[:, h, :], c["ident"][:], [kT, c["ident"]], [p3])
        B.cp("act", ktok[:], p3b[:, 0:512], [p3], [ktok])
        yield
        p4 = self.psb[4]
        if own:
            p4v = p4[:].rearrange("p (a q) -> p a q", a=4)
            for h in range(4):
                B.mm(p4v[:, h, :], kT[:, h, :], qT[:, h, :], True, True, [kT, qT], [p4])
            B.tt("dve", m["scT"][:], p4v, c["cmask"][:], ALU.mult, [p4, c["cmask"]], [m["scT"]])
            yield
            p5 = self.psb[5 + s]
            for h in range(4):
                B.mm(p5[:, h * 128:(h + 1) * 128], m["scT"][:, h, :], vtok[:, h * 128:(h + 1) * 128], True, False, [m["scT"], vtok], [p5])
                B.mm(p5[:, h * 128:(h + 1) * 128], qT[:, h, :], Shat[:, h * 128:(h + 1) * 128], False, True, [qT, Shat], [p5])
            yield
        for h in range(4):
            B.mm(p4[:, h * 128:(h + 1) * 128], ktok[:, h * 128:(h + 1) * 128], vtok[:, h * 128:(h + 1) * 128], True, True, [ktok, vtok], [p4])
        B.tt("dve", S[:], S[:], p4[:], ALU.add, [S, p4], [S])
        yield

    def gen_R2(self, u, tile):
        B, d, c, m, W = self.B, self.d, self.c, self.m, self.W
        kind, ti = tile
        s = u % 2
        p5 = self.psb[5 + s]
        p5v = p5[:].rearrange("p (a q) -> p a q", a=4)
        B.act(m["ysq"][:], p5[:], AF.Square, [p5], [m["ysq"]])
        B.S.add("dve", lambda e: e.tensor_reduce(m["s1"][:], p5v, AX.X, ALU.add), [p5.b], [m["s1"].b, p5.b])
        B.S.add("dve", lambda e: e.tensor_reduce(m["s2"][:], m["ysq"][:].rearrange("p (a q) -> p a q", a=4), AX.X, ALU.add), [m["ysq"].b], [m["s2"].b])
        B.ts("dve", m["mean"][:], m["s1"][:], 1.0 / 128, None, ALU.mult, None, [m["s1"]], [m["mean"]])
        B.tt("dve", m["msq"][:], m["mean"][:], m["mean"][:], ALU.mult, [m["mean"]], [m["msq"]])
        B.stt("dve", m["var"][:], m["s2"][:], 1.0 / 128, m["msq"][:], ALU.mult, ALU.subtract, [m["s2"], m["msq"]], [m["var"]])
        self.rsqrt(m["rstd"][:], m["var"][:], 4, 1.0, [m["var"]], [m["rstd"]])
        yield
        ynv = m["yn"][:].rearrange("p (a q) -> p a q", a=4)
        B.tt("dve", m["ysq"][:].rearrange("p (a q) -> p a q", a=4), p5v, m["mean"][:].unsqueeze(2).to_broadcast([128, 4, 128]), ALU.subtract,
             [p5, m["mean"]], [m["ysq"]])
        B.tt("dve", ynv, m["ysq"][:].rearrange("p (a q) -> p a q", a=4), m["rstd"][:].unsqueeze(2).to_broadcast([128, 4, 128]), ALU.mult,
             [m["ysq"], m["rstd"]], [m["yn"]])
        yield
        p3 = self.psb[3]
        p3b = p3[:].bitcast(BF16)
        for h in range(4):
            B.tr(p3b[:, h * 128:(h + 1) * 128], m["yn"][:, h * 128:(h + 1) * 128], c["ident"][:], [m["yn"], c["ident"]], [p3])
        for h in range(4):
            B.act(m["t1"][:, h, :], p3b[:, h * 128:(h + 1) * 128], AF.Identity, [p3, c["gng"], c["gnb"]], [m["t1"]],
                  bias=c["gnb"][:, h:h + 1], scale=c["gng"][:, h:h + 1])
        yield
        sgT = m["sgT"][u % 3]
        B.tt("pool", m["YrT"][s][:], m["t1"][:], sgT[:], ALU.mult, [m["t1"], sgT], [m["YrT"][s]])
        yield

    def gen_O1(self, u, tile):
        B, d, c, m, W = self.B, self.d, self.c, self.m, self.W
        kind, ti = tile
        s = u % 2
        sigr = m["sigr"][u % 4]
        YrT = m["YrT"][s]
        for g2 in range(2):
            pb = self.psb[7]
            pv = pb[:].rearrange("p (a q) -> p a q", a=4)
            for a in range(4):
                oc = g2 * 4 + a
                for kc in range(4):
                    B.mm(pv[:, a, :], W["ret_o"][:, kc, oc * 128:(oc + 1) * 128], YrT[:, kc, :], kc == 0, kc == 3, [W["ret_o"], YrT], [pb])
            B.stt("dve", self.grT[:, ti, g2 * 4:(g2 + 1) * 4, :], sigr[:, g2 * 4:(g2 + 1) * 4, :], 1.0, pv, ALU.add, ALU.mult, [pb, sigr], [self.grT])
            yield

    def phaseA2(self):
        B, d, c = self.B, self.d, self.c
        sb = B.sb
        self.lc(["swab_prev", "swab_cur", "swab_first", "blockones", "gqd", "gkd"])
        c["esink"] = sb("c_esink", [128, 8], F32)
        B.dma(c["esink"][:], d["sinks"][:].partition_broadcast(128), [d["sinks"]], [c["esink"]])
        B.act(c["esink"][:], c["esink"][:], AF.Exp, [c["esink"]], [c["esink"]])
        c["gqk"] = sb("c_gqk", [128, 1], F32)
        B.stt("dve", c["gqk"][:], c["gqd"][:], 0.125, c["gkd"][:], ALU.mult, ALU.mult, [c["gqd"], c["gkd"]], [c["gqk"]])
        W = self.W = {}
        W["in"] = sb("w2_bf", [128, 8, 1920], BF16)
        W["swa_o"] = sb("w_swa_o_bf", [128, 4, D], BF16)
        W["out"] = sb("w_out_bf", [128, 8, D], BF16)
        W["rt"] = sb("wrt_bf", [128, 8, 36], BF16)
        W["rt_lo"] = sb("wrt_lo_bf", [128, 8, 36], BF16)
        wrt_f = sb("wrt_f", [128, 8, 36], F32)
        mk_st = B.mark()
        self.new_stage()
        self.load_win(W["in"], [
            (C_SQ, C_SV + 128, [(0, 0, 512), (512, 512, 576), (576, 512, 576), (640, 576, 640), (704, 576, 640), (768, 640, 768)]),
            (C_GS, C_GS + 1024, [(896, 0, 1024)])])
        self.load_rows(W["swa_o"], "w_swa_o", 4, const_scale=0.5)
        self.load_rows(W["out"], "w_out", 8)
        st = self.next_stage()
        stv = st[:, 0:288].rearrange("p (c n) -> p c n", c=8)
        B.dma(stv, d["w_rt"][:].rearrange("(c p) n -> p c n", p=128), [d["w_rt"]], [st])
        B.cp("dve", W["rt"][:], stv, [st], [W["rt"]])
        B.cp("dve", wrt_f[:], W["rt"][:], [W["rt"]], [wrt_f])
        B.tt("dve", W["rt_lo"][:], stv, wrt_f[:], ALU.subtract, [st, wrt_f], [W["rt_lo"]])
        B.S.barrier()
        B.release(mk_st)
        c["gffn_row"] = sb("gffn_row", [128, D], F32)
        B.dma(c["gffn_row"][:], d["gffn_row"][:].partition_broadcast(128), [d["gffn_row"]], [c["gffn_row"]])
        c["brt"] = sb("brt_b", [128, 36], F32)
        B.dma(c["brt"][:], d["brt"][:].partition_broadcast(128), [d["brt"]], [c["brt"]])
        m = self.m = {}
        self.common_bufs(2)
        m["hb2"] = sb("hb2", [128, D], BF16)
        m["hf2"] = sb("hf2", [128, D], F32)
        m["hl2"] = sb("hl2", [128, D], BF16)
        m["loT"] = sb("loT", [128, 8, 128], BF16)
        m["ss2"] = sb("ss2", [128, 1], F32)
        m["rs2"] = sb("rs2", [128, 1], F32)
        R2 = lambda nm, shp, dt, n=2: [sb("%s%d" % (nm, i), shp, dt) for i in range(n)]
        m["sigs"] = R2("sigs", [128, 8, 128], BF16, 3)
        m["sqT"] = R2("sqT", [128, 4, 128], BF16)
        m["skT"] = R2("skT", [128, 2, 128], BF16, 3)
        m["vaug"] = R2("vaug", [128, 2, 65], BF16, 3)
        m["sqf"] = sb("sqf", [128, 512], F32)
        m["sq2"] = sb("sq2", [128, 512], BF16)
        m["rq"] = sb("rq", [128, 512], F32)
        m["skf"] = sb("skf", [128, 256], F32)
        m["sk2"] = sb("sk2", [128, 256], BF16)
        m["rk"] = sb("rk", [128, 256], F32)
        m["pT"] = R2("pT", [128, 4, 128], BF16, 4)
        m["den"] = sb("den", [128, 8], F32)
        m["rden"] = sb("rden", [128, 8], F32)
        m["ystok"] = sb("ystok", [128, 512], BF16)
        m["ysT"] = R2("ysT", [128, 4, 128], BF16)
        m["gs"] = R2("gs", [128, 4, 128], BF16)
        m["mrg"] = sb("mrg", [128, 8, 128], BF16)
        m["x1t"] = R2("x1t", [128, D], F32, 2)
        for i in range(3):
            B.memset("pool", m["vaug"][i][:], 1.0, [m["vaug"][i]])
        tiles = {15: ("prev", 15)}
        for i in range(self.n_own):
            tiles[16 + i] = ("own", i)
        us = sorted(tiles)

        def front_n(u):
            if u in tiles:
                yield from self.gen_N(u, tiles[u])

        def front_z(u):
            if u in tiles:
                yield from self.gen_Z2(u, tiles[u])

        def mid(u):
            if u in tiles and tiles[u][0] == "own":
                yield from self.gen_W(u, tiles[u])

        def back(u):
            if u in tiles and tiles[u][0] == "own":
                yield from self.gen_O2(u, tiles[u])

        def back2(u):
            if u in tiles and tiles[u][0] == "own":
                yield from self.gen_O2b(u, tiles[u])

        self.cur_tiles = tiles
        self.load_x(us[0])
        interleave(front_n(us[0]))
        interleave(front_n(us[0] + 1), front_z(us[0]))
        for u in range(us[0], us[-1] + 3):
            interleave(front_n(u + 2), front_z(u + 1), mid(u), back(u - 1), back2(u - 2))

    def gen_Z2(self, u, tile):
        B, d, c, m, W = self.B, self.d, self.c, self.m, self.W
        kind, ti = tile
        own = kind == "own"
        s = u % 2
        pb, pv = self.zgroup(u, 512, 2)
        B.cp("dve", m["skf"][:], pb[:, 0:256], [pb], [m["skf"]])
        B.act(m["sk2"][:], pb[:, 0:256], AF.Square, [pb], [m["sk2"]])
        p7 = self.psb[2]
        B.mm(p7[:, 0:256], c["blockones"][:], m["sk2"][:], True, True, [c["blockones"], m["sk2"]], [p7])
        B.act(m["rk"][:], p7[:, 0:256], AF.Sqrt, [p7], [m["rk"]], bias=EPS, scale=1.0)
        B.S.add("dve", lambda e: e.reciprocal(m["rk"][:], m["rk"][:]), [m["rk"].b], [m["rk"].b])
        B.stt("dve", m["skT"][u % 3][:], m["skf"][:].rearrange("p (a q) -> p a q", a=2), c["gqk"][:, 0:1],
              m["rk"][:].rearrange("p (a q) -> p a q", a=2), ALU.mult, ALU.mult, [m["skf"], m["rk"], c["gqk"]], [m["skT"][u % 3]])
        yield
        pb = self.ztok(u, 768, 128)
        B.cp("dve", m["vaug"][u % 3][:, :, 0:64], pb[:, 0:128].rearrange("p (a q) -> p a q", a=2), [pb], [m["vaug"][u % 3]])
        yield
        if not own:
            return
        pb, pv = self.zgroup(u, 0, 4)
        B.cp("dve", m["sqf"][:], pb[:], [pb], [m["sqf"]])
        B.act(m["sq2"][:], pb[:], AF.Square, [pb], [m["sq2"]])
        p7 = self.psb[2]
        B.mm(p7[:], c["blockones"][:], m["sq2"][:], True, True, [c["blockones"], m["sq2"]], [p7])
        B.act(m["rq"][:], p7[:], AF.Sqrt, [p7], [m["rq"]], bias=EPS, scale=1.0)
        B.S.add("dve", lambda e: e.reciprocal(m["rq"][:], m["rq"][:]), [m["rq"].b], [m["rq"].b])
        B.tt("pool", m["sqT"][s][:], m["sqf"][:].rearrange("p (a q) -> p a q", a=4),
             m["rq"][:].rearrange("p (a q) -> p a q", a=4), ALU.mult, [m["sqf"], m["rq"]], [m["sqT"][s]])
        yield
        for g2 in range(2):
            pb, pv = self.zgroup(u, 896 + g2 * 512, 4)
            B.act(m["sigs"][u % 3][:, g2 * 4:(g2 + 1) * 4, :], pv, AF.Tanh, [pb], [m["sigs"][u % 3]], scale=0.5)
            yield

    def gen_W(self, u, tile):
        B, d, c, m, W = self.B, self.d, self.c, self.m, self.W
        kind, ti = tile
        s = u % 2
        sp_ = (u - 1) % 2
        sqT = m["sqT"][s]
        first = (ti == 0)
        k = 0
        pvb = [self.psb[6], self.psb[6]]
        for kh in range(2):
            pts = []
            for kt in range(2):
                slot = (u - 1) % 3 if kt == 0 else u % 3
                skT = m["skT"][slot]
                bias = c["swab_cur"] if kt == 1 else (c["swab_first"] if first else c["swab_prev"])
                pT = m["pT"][kh * 2 + kt]
                pT4 = pT[:].rearrange("p (a b) q -> p a b q", a=2)
                for par in range(2):
                    po = par * 64
                    pb = self.psb[4 + par]
                    pv2 = pb[:, 0:256].rearrange("p (a q) -> p a q", a=2)
                    B.mm(pv2, skT[po:po + 64, kh, :], sqT[po:po + 64, kh * 2:kh * 2 + 2, :], True, False, [skT, sqT], [pb])
                    for hl_ in range(2):
                        b4 = bias[:, hl_, kh * 4:(kh + 1) * 4, :].rearrange("p (a b) q -> p a b q", a=2)
                        B.mm(pv2, c["ident"][:], b4[:, :, par, :], False, hl_ == 1, [c["ident"], bias], [pb])
                    B.act(pT4[:, :, par, :], pv2, AF.Exp, [pb], [pT])
                pts.append((pT, m["vaug"][slot]))
                yield
            po_ = pvb[kh]
            pov = po_[:, 0:260].rearrange("p (a q) -> p a q", a=4)
            for g in range(4):
                for kt in range(2):
                    pT, va = pts[kt]
                    B.mm(pov[:, g, :], pT[:, g, :], va[:, kh, :], kt == 0, kt == 1, [pT, va], [po_])
            B.tt("dve", m["den"][:, kh * 4:(kh + 1) * 4], pov[:, :, 64], c["esink"][:, kh * 4:(kh + 1) * 4], ALU.add, [po_, c["esink"]], [m["den"]])
            B.S.add("dve", lambda e, kh=kh: e.reciprocal(m["rden"][:, kh * 4:(kh + 1) * 4], m["den"][:, kh * 4:(kh + 1) * 4]), [m["den"].b], [m["rden"].b])
            B.tt("dve", m["ystok"][:, kh * 256:(kh + 1) * 256].rearrange("p (a q) -> p a q", a=4), pov[:, :, 0:64],
                 m["rden"][:, kh * 4:(kh + 1) * 4].unsqueeze(2).to_broadcast([128, 4, 64]), ALU.mult, [po_, m["rden"]], [m["ystok"]])
            yield
        p3 = self.psb[3]
        p3b = p3[:].bitcast(BF16)
        for h in range(4):
            B.tr(p3b[:, h * 128:(h + 1) * 128], m["ystok"][:, h * 128:(h + 1) * 128], c["ident"][:], [m["ystok"], c["ident"]], [p3])
        B.cp("dve", m["ysT"][s][:], p3b[:, 0:512].rearrange("p (a q) -> p a q", a=4), [p3], [m["ysT"][s]])
        yield

    def gen_O2(self, u, tile):
        B, d, c, m, W = self.B, self.d, self.c, self.m, self.W
        kind, ti = tile
        s = u % 2
        sigs = m["sigs"][u % 3]
        ysT = m["ysT"][s]
        x1t = m["x1t"][s]
        B.dma(x1t[:], d["x"][ti * 128:(ti + 1) * 128, :], [d["x"]], [x1t])
        for g2 in range(2):
            pb = self.psb[7]
            pv = pb[:].rearrange("p (a q) -> p a q", a=4)
            for a in range(4):
                oc = g2 * 4 + a
                for kc in range(4):
                    B.mm(pv[:, a, :], W["swa_o"][:, kc, oc * 128:(oc + 1) * 128], ysT[:, kc, :], kc == 0, kc == 3, [W["swa_o"], ysT], [pb])
            B.stt("dve", m["gs"][g2][:], sigs[:, g2 * 4:(g2 + 1) * 4, :], 1.0, pv, ALU.add, ALU.mult, [pb, sigs], [m["gs"][g2]])
            B.tt("pool", m["mrg"][:, g2 * 4:(g2 + 1) * 4, :], self.grT[:, ti, g2 * 4:(g2 + 1) * 4, :], m["gs"][g2][:], ALU.add,
                 [self.grT, m["gs"][g2]], [m["mrg"]])
            yield
        for half in range(2):
            pb = self.psb[7]
            for kc in range(8):
                B.mm(pb[:], m["mrg"][:, kc, :], W["out"][:, kc, half * 512:(half + 1) * 512], kc == 0, kc == 7, [m["mrg"], W["out"]], [pb])
            B.tt("dve", x1t[:, half * 512:(half + 1) * 512], pb[:], x1t[:, half * 512:(half + 1) * 512], ALU.add, [pb, x1t], [x1t])
            yield
        if "x1" in self.dbg:
            op = B.dma(self.out[ti * 128:(ti + 1) * 128, :], x1t[:], [x1t], [self.out])
            B.S.final_waits.append(op)
        else:
            B.dma(self.x1s[ti * 128:(ti + 1) * 128, :], x1t[:], [x1t], [self.x1s])
        yield

    def gen_O2b(self, u, tile):
        B, d, c, m, W = self.B, self.d, self.c, self.m, self.W
        kind, ti = tile
        x1t = m["x1t"][u % 2]
        ss, rs, hb = m["ss2"], m["rs2"], m["hb2"]
        B.act(m["junk"][:], x1t[:], AF.Square, [x1t, ss], [m["junk"], ss], accum_out=ss[:])
        self.rsqrt(rs[:], ss[:], 1, 1.0 / D, [ss], [rs])
        hf, hl, loT = m["hf2"], m["hl2"], m["loT"]
        B.stt("dve", hf[:], x1t[:], rs[:, 0:1], c["gffn_row"][:], ALU.mult, ALU.mult, [x1t, rs, c["gffn_row"]], [hf])
        B.cp("act", hb[:], hf[:], [hf], [hb])
        B.tt("pool", hl[:], hf[:], hb[:], ALU.subtract, [hf, hb], [hl])
        yield
        p3 = self.psb[3]
        p3b = p3[:].bitcast(BF16)
        for cc in range(8):
            B.tr(p3b[:, cc * 128:(cc + 1) * 128], hb[:, cc * 128:(cc + 1) * 128], c["ident"][:], [hb, c["ident"]], [p3])
        h2v = self.h2T[:, :, ti * 128:(ti + 1) * 128]
        B.cp("act", h2v, p3b[:, 0:D].rearrange("p (a q) -> p a q", a=8), [p3], [self.h2T])
        yield
        for cc in range(8):
            B.tr(p3b[:, cc * 128:(cc + 1) * 128], hl[:, cc * 128:(cc + 1) * 128], c["ident"][:], [hl, c["ident"]], [p3])
        B.cp("dve", loT[:], p3b[:, 0:D].rearrange("p (a q) -> p a q", a=8), [p3], [loT])
        yield
        p6 = self.psb[3]
        k = 0
        for (lhs_t, lhs_ap, wt) in [(self.h2T, None, W["rt"]), (loT, loT, W["rt"]), (self.h2T, None, W["rt_lo"])]:
            for kc in range(8):
                lhsT = self.h2T[:, kc, ti * 128:(ti + 1) * 128] if lhs_ap is None else loT[:, kc, :]
                B.mm(p6[:, 0:36], lhsT, wt[:, kc, :], k == 0, k == 23, [lhs_t, wt], [p6])
                k += 1
        B.tt("dve", self.lg_all[:, ti, :], p6[:, 0:36], c["brt"][:], ALU.add, [p6, c["brt"]], [self.lg_all])
        yield

    def moe(self, n_groups):
        B, d, c = self.B, self.d, self.c
        sb = B.sb
        m = self.m = {}
        self.acc = sb("acc", [128, 16, D], F32)
        self.mk_after_acc = B.mark()
        self.lc(["sel"])
        self.new_stage(3, 2048)
        h2T = self.h2T
        NTL = self.n_own
        cT = sb("cT", [64, TOK], BF16)
        F = lambda nm, shp: sb("r_" + nm, shp, F32)
        gmax, gd, ge, gsum, gw = F("gmax", [128, 16]), F("gd", [128, 16, 4]), F("ge", [128, 16, 4]), F("gsum", [128, 16]), F("gw", [128, 16])
        oh, pen = F("oh", [128, 16, 4]), F("pen", [128, 16, 4])
        elm, m1, mask1 = F("elm", [128, 16, 32]), F("m1", [128, 16]), F("mask1", [128, 16, 32])
        elm2, m2, mask2 = F("elm2", [128, 16, 32]), F("m2", [128, 16]), F("mask2", [128, 16, 32])
        dm, ed, w1, w2 = F("dm", [128, 16]), F("ed", [128, 16]), F("w1", [128, 16]), F("w2", [128, 16])
        c1, cc, chf, clo = mask1, mask2, elm, elm2
        chl = sb("chl", [128, 16, 64], BF16)
        lg = self.lg_all
        L = lambda fn, rd, wr: B.S.add("dve", fn, [t.b for t in rd], [t.b for t in wr])
        bc = lambda t, shp: t[:].unsqueeze(2).to_broadcast(shp)
        L(lambda e: e.tensor_reduce(gmax[:], lg[:, :, 0:4], AX.X, ALU.max), [lg], [gmax])
        B.tt("dve", gd[:], lg[:, :, 0:4], bc(gmax, [128, 16, 4]), ALU.subtract, [lg, gmax], [gd])
        B.act(ge[:], gd[:], AF.Exp, [gd], [ge])
        L(lambda e: e.tensor_reduce(gsum[:], ge[:], AX.X, ALU.add), [ge], [gsum])
        L(lambda e: e.reciprocal(gw[:], gsum[:]), [gsum], [gw])
        B.tt("dve", oh[:], lg[:, :, 0:4], bc(gmax, [128, 16, 4]), ALU.is_equal, [lg, gmax], [oh])
        B.ts("dve", pen[:], oh[:], 1e30, -1e30, ALU.mult, ALU.add, [oh], [pen])
        B.tt("dve", elm[:].rearrange("p t (a q) -> p t a q", a=4), lg[:, :, 4:36].rearrange("p t (a q) -> p t a q", a=4),
             pen[:].unsqueeze(3).to_broadcast([128, 16, 4, 8]), ALU.add, [lg, pen], [elm])
        L(lambda e: e.tensor_reduce(m1[:], elm[:], AX.X, ALU.max), [elm], [m1])
        B.tt("dve", mask1[:], elm[:], bc(m1, [128, 16, 32]), ALU.is_equal, [elm, m1], [mask1])
        B.stt("dve", elm2[:], mask1[:], -1e30, elm[:], ALU.mult, ALU.add, [mask1, elm], [elm2])
        L(lambda e: e.tensor_reduce(m2[:], elm2[:], AX.X, ALU.max), [elm2], [m2])
        B.tt("dve", mask2[:], elm2[:], bc(m2, [128, 16, 32]), ALU.is_equal, [elm2, m2], [mask2])
        B.tt("dve", dm[:], m2[:], m1[:], ALU.subtract, [m2, m1], [dm])
        B.act(ed[:], dm[:], AF.Exp, [dm], [ed])
        B.ts("dve", w1[:], ed[:], 1.0, None, ALU.add, None, [ed], [w1])
        L(lambda e: e.reciprocal(w1[:], w1[:]), [w1], [w1])
        B.tt("dve", w1[:], w1[:], gw[:], ALU.mult, [w1, gw], [w1])
        B.tt("dve", w2[:], w1[:], ed[:], ALU.mult, [w1, ed], [w2])
        B.tt("dve", c1[:], mask1[:], bc(w1, [128, 16, 32]), ALU.mult, [mask1, w1], [c1])
        B.tt("dve", cc[:], mask2[:], bc(w2, [128, 16, 32]), ALU.mult, [mask2, w2], [cc])
        B.tt("dve", cc[:], cc[:], c1[:], ALU.add, [cc, c1], [cc])
        B.cp("dve", chl[:, :, 0:32], cc[:], [cc], [chl])
        B.cp("dve", chf[:], chl[:, :, 0:32], [chl], [chf])
        B.tt("dve", clo[:], cc[:], chf[:], ALU.subtract, [cc, chf], [clo])
        B.cp("dve", chl[:, :, 32:64], clo[:], [clo], [chl])
        for hf in range(2):
            p5 = self.psb[5 + hf]
            p5b = p5[:].bitcast(BF16)
            nt = min(8, NTL - hf * 8)
            if nt <= 0:
                break
            for k in range(nt):
                B.tr(p5b[0:64, k * 128:(k + 1) * 128], chl[:, hf * 8 + k, :], c["ident"][:], [chl, c["ident"]], [p5])
            B.cp("act", cT[:, hf * 1024:hf * 1024 + nt * 128], p5b[0:64, 0:nt * 128], [p5], [cT])
        if "comb" in self.dbg:
            B.dump("cT", cT, cT[:], [64, TOK], BF16)
        NG = n_groups
        slots = [[{"g": sb("wg%d%d" % (i, j), [128, 8, 256], BF16), "u": sb("wu%d%d" % (i, j), [128, 8, 256], BF16),
                   "d": sb("wd%d%d" % (i, j), [128, 2, D], BF16)} for j in range(2)] for i in range(2)]
        sg = [sb("sg%d" % i, [128, 512], BF16) for i in range(2)]
        tu = [sb("tu%d" % i, [128, 512], BF16) for i in range(2)]
        hid = [[[sb("hid%d%d%d" % (i, j, k), [128, 512], BF16) for k in range(2)] for j in range(2)] for i in range(2)]

        def gen_load(gi):
            work = []
            for j in range(2):
                e = gi * 2 + j
                sl = slots[gi % 2][j]
                work.append((sl["g"], d["w_gate"], e, 8))
                work.append((sl["u"], d["w_up"], e, 8))
                work.append((sl["d"], d["w_down"], e, 2))

            def issue(k):
                dst, src, e, cdim = work[k]
                st = self.next_stage()
                stv = st[:].rearrange("p (c n) -> p c n", c=cdim)
                B.dma(stv, src[e].rearrange("(c p) n -> p c n", p=128), [src], [st])
                return st, stv

            q = [issue(k) for k in range(min(3, len(work)))]
            for k in range(len(work)):
                st, stv = q[k]
                self.cast(work[k][0][:], stv, [st], [work[k][0]])
                if k + 3 < len(work):
                    q.append(issue(k + 3))
                yield

        def load_group(gi):
            interleave(gen_load(gi))

        self.cast_engs = ("act",)
        load_group(0)
        for t in range(NTL):
            B.dma(self.acc[:, t, :], self.x1s[t * 128:(t + 1) * 128, :], [self.x1s], [self.acc])
        cnt = {"nb": 0, "nd": 0, "npc": 0}
        nchunk = self.n_own // 4

        def gen_gu(gi, ch):
            cs = slice(ch * 512, (ch + 1) * 512)
            hs = hid[ch % 2]
            for j in range(2):
                e = gi * 2 + j
                sl = slots[gi % 2][j]
                pc = self.psb[4 + 3 * (cnt["npc"] % 2)]
                cnt["npc"] += 1
                for fc in range(2):
                    nb = cnt["nb"]
                    pa, pu = self.psb[nb % 2], self.psb[2 + nb % 2]
                    cnt["nb"] += 1
                    nb += 1
                    for kc in range(8):
                        B.mm(pa[:], sl["g"][:, kc, fc * 128:(fc + 1) * 128], h2T[:, kc, cs], kc == 0, kc == 7, [sl["g"], h2T], [pa])
                    B.act(sg[nb % 2][:], pa[:], AF.Silu, [pa], [sg[nb % 2]])
                    yield
                    for kc in range(8):
                        B.mm(pu[:], sl["u"][:, kc, fc * 128:(fc + 1) * 128], h2T[:, kc, cs], kc == 0, kc == 7, [sl["u"], h2T], [pu])
                    if fc == 0:
                        B.mm(pc[:], c["sel"][:, e, :], cT[:, cs], True, True, [c["sel"], cT], [pc])
                    B.tt("dve", tu[nb % 2][:], pu[:], sg[nb % 2][:], ALU.mult, [pu, sg[nb % 2]], [tu[nb % 2]])
                    B.tt("dve", hs[j][fc][:], pc[:], tu[nb % 2][:], ALU.mult, [pc, tu[nb % 2]], [hs[j][fc]])
                    yield

        def gen_down(gi, ch):
            hs = hid[ch % 2]
            for tl in range(4):
                t = ch * 4 + tl
                for half in range(2):
                    pd = self.psb[5 + cnt["nd"] % 2]
                    cnt["nd"] += 1
                    k = 0
                    for j in range(2):
                        sl = slots[gi % 2][j]
                        for fc in range(2):
                            B.mm(pd[:], hs[j][fc][:, tl * 128:(tl + 1) * 128], sl["d"][:, fc, half * 512:(half + 1) * 512],
                                 k == 0, k == 3, [hs[j][fc], sl["d"]], [pd])
                            k += 1
                    a = self.acc[:, t, half * 512:(half + 1) * 512]
                    B.tt("dve", a, a, pd[:], ALU.add, [self.acc, pd], [self.acc])
                    yield

        items = [(gi, ch) for gi in range(NG) for ch in range(nchunk)]
        prev = None
        pending = None
        for (gi, ch) in items:
            interleave(gen_gu(gi, ch), gen_down(*prev) if prev is not None else None, pending)
            pending = None
            prev = (gi, ch)
            if ch == 0 and gi + 1 < NG:
                if nchunk >= 2:
                    pending = gen_load(gi + 1)
                else:
                    load_group(gi + 1)
        interleave(gen_down(*prev))

    def ple(self):
        B, d, c = self.B, self.d, self.c
        sb = B.sb
        B.S.barrier()
        B.release(self.mk_after_acc)
        m = self.m = {}
        self.cast_engs = ("act", "dve")
        self.new_stage()
        wpg = sb("wpg_bf", [128, 8, D], BF16)
        wple = sb("wple_bf", [128, 2, D], BF16)
        self.load_rows(wpg, "w_pg", 8)
        self.load_rows(wple, "w_ple", 2)
        grow = sb("gpg_row", [128, D], F32)
        B.dma(grow[:], d["gpg_row"][:].partition_broadcast(128), [d["gpg_row"]], [grow])
        gple = sb("gple_b", [128, D], F32)
        B.dma(gple[:], d["gple"][:].partition_broadcast(128), [d["gple"]], [gple])
        B.ts("pool", gple[:], gple[:], 0.5, None, ALU.mult, None, [gple], [gple])
        m["junk"] = sb("junk", [128, D], BF16)
        junk2 = sb("junk2", [128, 512], BF16)
        R2 = lambda nm, shp, dt, n=2: [sb("%s%d" % (nm, i), shp, dt) for i in range(n)]
        hb = R2("hb3", [128, D], BF16)
        h3T = R2("h3T", [128, 8, 128], BF16)
        ss, rs = R2("ss", [128, 1], F32), R2("rs", [128, 1], F32)
        ssp, rsp = R2("ssp", [128, 2], F32), R2("rsp", [128, 1], F32)
        pt = R2("pt", [128, 256], F32)
        pbf = R2("pbf", [128, 256], BF16)
        pT = R2("pT", [128, 2, 128], BF16)
        sgate = R2("sgate", [128, D], F32)
        ple = R2("ple", [128, D], F32)
        ot = R2("ot", [128, D], F32)
        NTL = self.n_own

        def front(t):
            if t >= NTL:
                return
            i = t % 2
            xa = self.acc[:, t, :]
            if t + 1 < NTL:
                B.dma(pt[(t + 1) % 2][:], d["p"][(t + 1) * 128:(t + 2) * 128, :], [d["p"]], [pt[(t + 1) % 2]])
            B.act(m["junk"][:], xa, AF.Square, [self.acc, ss[i]], [m["junk"], ss[i]], accum_out=ss[i][:])
            self.rsqrt(rs[i][:], ss[i][:], 1, 1.0 / D, [ss[i]], [rs[i]])
            B.stt("dve", hb[i][:], xa, rs[i][:, 0:1], grow[:], ALU.mult, ALU.mult, [self.acc, rs[i], grow], [hb[i]])
            B.cp("dve", pbf[i][:], pt[i][:], [pt[i]], [pbf[i]])
            yield

        def front_b(t):
            if t >= NTL:
                return
            i = t % 2
            pb = self.psb[7]
            pbb = pb[:].bitcast(BF16)
            for cc in range(8):
                B.tr(pbb[:, cc * 128:(cc + 1) * 128], hb[i][:, cc * 128:(cc + 1) * 128], c["ident"][:], [hb[i], c["ident"]], [pb])
            B.cp("act", h3T[i][:], pbb[:, 0:D].rearrange("p (a q) -> p a q", a=8), [pb], [h3T[i]])
            yield
            p4 = self.psb[6]
            p4b = p4[:].bitcast(BF16)
            for kc in range(2):
                B.tr(p4b[:, kc * 128:(kc + 1) * 128], pbf[i][:, kc * 128:(kc + 1) * 128], c["ident"][:], [pbf[i], c["ident"]], [p4])
            B.cp("dve", pT[i][:], p4b[:, 0:256].rearrange("p (a q) -> p a q", a=2), [p4], [pT[i]])
            yield

        def mid(t):
            if not (0 <= t < NTL):
                return
            i = t % 2
            for half in range(2):
                pb = self.psb[2 + 2 * i + half]
                for kc in range(2):
                    B.mm(pb[:], pT[i][:, kc, :], wple[:, kc, half * 512:(half + 1) * 512], kc == 0, kc == 1, [pT[i], wple], [pb])
                B.act(junk2[:], pb[:], AF.Square, [pb, ssp[i]], [junk2, ssp[i]], accum_out=ssp[i][:, half:half + 1])
            yield
            for half in range(2):
                pb = self.psb[half]
                for kc in range(8):
                    B.mm(pb[:], h3T[i][:, kc, :], wpg[:, kc, half * 512:(half + 1) * 512], kc == 0, kc == 7, [h3T[i], wpg], [pb])
                B.act(sgate[i][:, half * 512:(half + 1) * 512], pb[:], AF.Tanh, [pb], [sgate[i]], scale=0.5)
                yield

        def back(t):
            if not (0 <= t < NTL):
                return
            i = t % 2
            xa = self.acc[:, t, :]
            B.tt("dve", rsp[i][:], ssp[i][:, 0:1], ssp[i][:, 1:2], ALU.add, [ssp[i]], [rsp[i]])
            self.rsqrt(rsp[i][:], rsp[i][:], 1, 1.0 / D, [rsp[i]], [rsp[i]])
            yield
            for half in range(2):
                pb = self.psb[2 + 2 * i + half]
                hsl = slice(half * 512, (half + 1) * 512)
                B.stt("dve", ple[i][:, hsl], pb[:], rsp[i][:, 0:1], gple[:, hsl], ALU.mult, ALU.mult, [pb, rsp[i], gple], [ple[i]])
            yield
            o = ot[i]
            B.stt("dve", o[:], sgate[i][:], 1.0, ple[i][:], ALU.add, ALU.mult, [sgate[i], ple[i]], [o])
            B.tt("dve", o[:], o[:], xa, ALU.add, [o, self.acc], [o])
            op = B.dma(self.out[t * 128:(t + 1) * 128, :], o[:], [o], [self.out])
            B.S.final_waits.append(op)
            yield

        B.dma(pt[0][:], d["p"][0:128, :], [d["p"]], [pt[0]])
        interleave(front(0))
        interleave(front(1), front_b(0))
        interleave(front(2), front_b(1), mid(0))
        for t in range(NTL):
            interleave(front(t + 3), front_b(t + 2), mid(t + 1), back(t))


def _pc(v, n):
    return np.ascontiguousarray(np.asarray(v, np.float32).reshape(n, 128).T)


def prep_inputs(inp):
    ct = consts()
    f = lambda a: np.ascontiguousarray(np.asarray(a, np.float32))
    x = f(inp["x"])
    p = f(inp["p"])[0]
    shared = dict(
        w_in=f(inp["w_in"])[0], w_ret_o=f(inp["w_ret_o"])[0], w_swa_o=f(inp["w_swa_o"])[0], w_out=f(inp["w_out"])[0],
        w_rt=np.ascontiguousarray(np.concatenate([f(inp["w_router_group"])[0], f(inp["w_router_expert"])[0]], axis=1)),
        w_gate=f(inp["w_exp_gate"])[0], w_up=f(inp["w_exp_up"])[0], w_down=f(inp["w_exp_down"])[0],
        w_pg=f(inp["w_ple_gate"])[0], w_ple=f(inp["w_ple"])[0],
        gmix=_pc(inp["mix_norm_g"][0], 8), gffn=_pc(inp["ffn_norm_g"][0], 8), gpg=_pc(inp["ple_gate_norm_g"][0], 8),
        gng=_pc(inp["ret_gn_g"][0], 4), gnb=_pc(inp["ret_gn_b"][0], 4),
        gqd=np.ascontiguousarray(np.tile(f(inp["q_norm_g"])[0], 2).reshape(128, 1)),
        gkd=np.ascontiguousarray(np.tile(f(inp["k_norm_g"])[0], 2).reshape(128, 1)),
        sinks=f(inp["attn_sinks"]).reshape(1, 8),
        brt=np.ascontiguousarray(np.concatenate([f(inp["b_router_group"])[0], f(inp["b_router_expert"])[0]]).reshape(1, 36)),
        gple=f(inp["ple_norm_g"]).reshape(1, D),
        gffn_row=f(inp["ffn_norm_g"]).reshape(1, D), gpg_row=f(inp["ple_gate_norm_g"]).reshape(1, D),
        ident=ct["ident"], qdec=ct["qdec"], kdec=ct["kdec"], adec=ct["adec"], cmask=ct["cmask"],
        swab_prev=ct["swab_prev"], swab_cur=ct["swab_cur"], blockones=ct["blockones"], sel=ct["sel"],
    )
    maps = []
    zeros = np.zeros((TOK, D), np.float32)
    allneg = ct["swab_neg"]
    for cid in range(NCORES):
        b, half = cid // 2, cid % 2
        mp = dict(shared)
        mp["x"] = np.ascontiguousarray(x[b, half * TOK:(half + 1) * TOK])
        mp["xprev"] = np.ascontiguousarray(x[b, 0:TOK]) if half == 1 else zeros
        mp["p"] = np.ascontiguousarray(p[b, half * TOK:(half + 1) * TOK])
        mp["swab_first"] = ct["swab_prev"] if half == 1 else allneg
        maps.append(mp)
    return maps


_PROG = None


def kernel(**inputs):
    global _PROG
    if _PROG is None:
        _PROG = Prog()
    maps = prep_inputs(inputs)
    res = run_bass_kernel_spmd(_PROG.B.nc, maps, core_ids=list(range(NCORES)))
    out = np.empty((4, 4096, D), np.float32)
    for cid in range(NCORES):
        b, half = cid // 2, cid % 2
        out[b, half * TOK:(half + 1) * TOK] = res.results[cid]["out"]
    return out
```
